# Optimizing a Trainium2 kernel written in Bass

```python
import jax, jax.numpy as jnp
from jax import lax
import numpy as np

D_MODEL = 1024
BATCH = 2
SEQ = 16384
DEPTH = 1

ATT_HEAD_DIM = 64
ATT_HEADS = (D_MODEL // 2) // ATT_HEAD_DIM
ATT_W = ATT_HEADS * ATT_HEAD_DIM
IDX_HEADS = 4
IDX_DIM = 64
TOPK_MAX = 256
Q_BLOCK = 128
HG_KDIM = 128
HG_VDIM = 128
HG_HEADS = (D_MODEL // 2) // HG_VDIM
HG_W = HG_HEADS * HG_VDIM
HG_CHUNK = 64
MIX_W = ATT_W + HG_W
D_FF = 4 * D_MODEL
ROPE_THETA = 10000.0
ALPHA = (2.0 * DEPTH) ** 0.25
BETA = (8.0 * DEPTH) ** -0.25
LN_EPS = 1e-5
RMS_EPS = 1e-6
SPLIT_SIZES = (ATT_W, ATT_W, ATT_W,
               IDX_HEADS * IDX_DIM, IDX_DIM, IDX_HEADS,
               HG_HEADS * HG_KDIM, HG_HEADS * HG_KDIM,
               HG_W, HG_W)
N_IN_COLS = sum(SPLIT_SIZES)

kernel_name = "hymba_dsa_hgrn2_deepnorm"


def _split_points():
    pts, acc = [], 0
    for s in SPLIT_SIZES[:-1]:
        acc += s
        pts.append(acc)
    return pts


def layer_norm(x, g, b):
    xf = x.astype(jnp.float32)
    mu = jnp.mean(xf, axis=-1, keepdims=True)
    var = jnp.mean(jnp.square(xf - mu), axis=-1, keepdims=True)
    y = (xf - mu) * lax.rsqrt(var + LN_EPS) * g.astype(jnp.float32) + b.astype(jnp.float32)
    return y.astype(x.dtype)


def rope(x, pos):
    half = x.shape[-1] // 2
    inv = jnp.power(ROPE_THETA, -jnp.arange(half, dtype=jnp.float32) / half)
    ang = pos[:, None] * inv[None, :]
    cos = jnp.cos(ang)[None, :, None, :]
    sin = jnp.sin(ang)[None, :, None, :]
    xf = x.astype(jnp.float32)
    x1, x2 = xf[..., :half], xf[..., half:]
    return jnp.concatenate([x1 * cos - x2 * sin, x2 * cos + x1 * sin], axis=-1).astype(x.dtype)


def dsa_attention(q, k, v, iq, ik, iw):
    B, S, H, D = q.shape
    topk = min(TOPK_MAX, S // 4)
    n_blocks = S // Q_BLOCK
    kpos = jnp.arange(S, dtype=jnp.int32)
    bidx = jnp.arange(B, dtype=jnp.int32)[:, None, None]
    scale = D ** -0.5

    def block(i):
        start = i * Q_BLOCK
        qb = lax.dynamic_slice_in_dim(q, start, Q_BLOCK, axis=1)
        iqb = lax.dynamic_slice_in_dim(iq, start, Q_BLOCK, axis=1)
        iwb = lax.dynamic_slice_in_dim(iw, start, Q_BLOCK, axis=1)
        qpos = start + jnp.arange(Q_BLOCK, dtype=jnp.int32)
        logits = jnp.einsum('bqhd,bsd->bqhs', iqb, ik).astype(jnp.float32)
        score = jnp.einsum('bqh,bqhs->bqs', iwb.astype(jnp.float32), jax.nn.relu(logits))
        causal = kpos[None, :] <= qpos[:, None]
        score = jnp.where(causal[None], score, -jnp.inf)
        _, idx = lax.top_k(score, topk)
        valid = idx <= qpos[None, :, None]
        ks = k[bidx, idx]
        vs = v[bidx, idx]
        s = jnp.einsum('bqhd,bqkhd->bhqk', qb, ks).astype(jnp.float32) * scale
        s = jnp.where(valid[:, None], s, -jnp.inf)
        p = jax.nn.softmax(s, axis=-1).astype(v.dtype)
        return jnp.einsum('bhqk,bqkhd->bqhd', p, vs)

    out = lax.map(block, jnp.arange(n_blocks, dtype=jnp.int32))
    return out.transpose(1, 0, 2, 3, 4).reshape(B, S, H * D)


def hgrn2(q, f_logit, inp, gate, lb, norm_g):
    B, S, Hh, K = q.shape
    V = inp.shape[-1]
    C = HG_CHUNK
    N = S // C
    f = lb[None, None] + (1.0 - lb[None, None]) * jax.nn.sigmoid(f_logit.astype(jnp.float32))
    logf = jnp.log(f)
    kk = 1.0 - f

    def chunks(a):
        return a.reshape(B, N, C, Hh, a.shape[-1]).transpose(1, 0, 3, 2, 4)

    qc, kc, vc, lc = chunks(q.astype(jnp.float32)), chunks(kk), chunks(inp.astype(jnp.float32)), chunks(logf)
    tri = jnp.arange(C)[:, None] >= jnp.arange(C)[None, :]

    def step(state, xs):
        qh, kh, vh, lh = xs
        b = jnp.cumsum(lh, axis=-2)
        o_inter = jnp.einsum('bhck,bhkv->bhcv', qh * jnp.exp(b), state)
        diff = b[:, :, :, None, :] - b[:, :, None, :, :]
        decay = jnp.exp(jnp.where(tri[None, None, :, :, None], diff, -jnp.inf))
        a = jnp.einsum('bhtk,bhtsk,bhsk->bhts', qh, decay, kh)
        o = o_inter + jnp.einsum('bhts,bhsv->bhtv', a, vh)
        b_last = b[:, :, -1:, :]
        new_state = jnp.exp(b_last[:, :, 0, :])[..., None] * state + \
            jnp.einsum('bhsk,bhsv->bhkv', kh * jnp.exp(b_last - b), vh)
        return new_state, o

    s0 = jnp.zeros((B, Hh, K, V), jnp.float32)
    _, o = lax.scan(step, s0, (qc, kc, vc, lc))
    o = o.transpose(1, 0, 3, 2, 4).reshape(B, S, Hh, V)
    o = o * lax.rsqrt(jnp.mean(jnp.square(o), axis=-1, keepdims=True) + RMS_EPS) * norm_g.astype(jnp.float32)
    o = o * jax.nn.silu(gate.astype(jnp.float32))
    return o.reshape(B, S, Hh * V).astype(inp.dtype)


def setup_inputs(seed: int = 0) -> dict:
    key = jax.random.key(seed)
    ks = jax.random.split(key, 12)
    f32 = jnp.float32
    x = jax.random.normal(ks[0], (BATCH, SEQ, D_MODEL), f32)
    w_in = jax.random.normal(ks[1], (DEPTH, D_MODEL, N_IN_COLS), f32) * D_MODEL ** -0.5
    w_o = jax.random.normal(ks[2], (DEPTH, MIX_W, D_MODEL), f32) * (MIX_W ** -0.5 * BETA)
    lb_logits = jax.random.normal(ks[3], (DEPTH + 1, HG_HEADS, HG_KDIM), f32) * 0.5
    hg_norm_g = 1.0 + 0.02 * jax.random.normal(ks[4], (DEPTH, HG_HEADS, HG_VDIM), f32)
    ln1_g = 1.0 + 0.02 * jax.random.normal(ks[5], (DEPTH, D_MODEL), f32)
    ln1_b = 0.02 * jax.random.normal(ks[6], (DEPTH, D_MODEL), f32)
    w_up = jax.random.normal(ks[7], (DEPTH, D_MODEL, D_FF), f32) * D_MODEL ** -0.5
    w_down = jax.random.normal(ks[8], (DEPTH, D_FF, D_MODEL), f32) * (D_FF ** -0.5 * BETA)
    ln2_g = 1.0 + 0.02 * jax.random.normal(ks[9], (DEPTH, D_MODEL), f32)
    ln2_b = 0.02 * jax.random.normal(ks[10], (DEPTH, D_MODEL), f32)
    return {"x": x, "w_in": w_in, "w_o": w_o, "lb_logits": lb_logits, "hg_norm_g": hg_norm_g,
            "ln1_g": ln1_g, "ln1_b": ln1_b, "w_up": w_up, "w_down": w_down,
            "ln2_g": ln2_g, "ln2_b": ln2_b}


def reference(x, w_in, w_o, lb_logits, hg_norm_g, ln1_g, ln1_b, w_up, w_down, ln2_g, ln2_b):
    B, S, _ = x.shape
    pos = jnp.arange(S, dtype=jnp.float32)
    lower_bounds = jnp.cumsum(jax.nn.softmax(lb_logits.astype(jnp.float32), axis=0), axis=0)
    idx_w_scale = (IDX_HEADS ** -0.5) * (IDX_DIM ** -0.5)
    for l in range(DEPTH):
        proj = x @ w_in[l]
        q, k, v, iq, ik, iw, hq, hf, hi, hg = jnp.split(proj, _split_points(), axis=-1)
        q = rope(q.reshape(B, S, ATT_HEADS, ATT_HEAD_DIM), pos)
        k = rope(k.reshape(B, S, ATT_HEADS, ATT_HEAD_DIM), pos)
        v = v.reshape(B, S, ATT_HEADS, ATT_HEAD_DIM)
        iq = rope(iq.reshape(B, S, IDX_HEADS, IDX_DIM), pos)
        ik = rope(ik.reshape(B, S, 1, IDX_DIM), pos)[:, :, 0, :]
        att = dsa_attention(q, k, v, iq, ik, iw * idx_w_scale)
        hgo = hgrn2(hq.reshape(B, S, HG_HEADS, HG_KDIM),
                    hf.reshape(B, S, HG_HEADS, HG_KDIM),
                    hi.reshape(B, S, HG_HEADS, HG_VDIM),
                    hg.reshape(B, S, HG_HEADS, HG_VDIM),
                    lower_bounds[l], hg_norm_g[l])
        mix = jnp.concatenate([att, hgo], axis=-1) @ w_o[l]
        x = layer_norm(ALPHA * x + mix, ln1_g[l], ln1_b[l])
        h = jnp.square(jax.nn.relu(x @ w_up[l])) @ w_down[l]
        x = layer_norm(ALPHA * x + h, ln2_g[l], ln2_b[l])
    return x
```

```python
import math
from contextlib import ExitStack
import numpy as np
import concourse.bass as bass
import concourse.mybir as mybir
from concourse.bass_utils import run_bass_kernel_spmd

F32 = mybir.dt.float32
BF16 = mybir.dt.bfloat16
I32 = mybir.dt.int32
AF = mybir.ActivationFunctionType
ALU = mybir.AluOpType
AX = mybir.AxisListType

D = 1024
SEQ = 16384
NCOL = 3908
DFF = 4096
ALPHA = 2.0 ** 0.25
LN_EPS = 1e-5
RMS_EPS = 1e-6
IDX_SCALE = (4 ** -0.5) * (64 ** -0.5)
TOPK = 256.0
NEG = -1.0e30
NIT = 18

C_Q, C_K, C_V, C_IQ, C_IK, C_IW, C_HQ, C_HF, C_HI, C_HG = 0, 512, 1024, 1536, 1792, 1856, 1860, 2372, 2884, 3396


class T:
    __slots__ = ("w", "r", "name", "excl")

    def __init__(self, name="", excl=False):
        self.w = None
        self.r = {}
        self.name = name
        self.excl = excl


class DSem:
    def __init__(self, h):
        self.h = h
        self.count = 0


class Sched:
    def __init__(self, nc, es):
        self.nc = nc
        self.es = es
        self.eng = {"pe": nc.tensor, "act": nc.scalar, "dve": nc.vector, "pool": nc.gpsimd, "sp": nc.sync}
        self.sem = {k: es.enter_context(nc.semaphore("s_" + k)) for k in ("pe", "act", "dve", "pool")}
        self.cnt = {k: 0 for k in self.sem}
        self.seen = {k: {} for k in self.eng}
        self.n_ins = 0

    def dsem(self, name):
        return DSem(self.es.enter_context(self.nc.semaphore(name)))

    def _deps(self, reads, writes, eng=None):
        deps = {}
        for b in reads:
            if b.w is not None:
                k, v = b.w
                if deps.get(k, 0) < v:
                    deps[k] = v
            if b.excl:
                for k, v in b.r.items():
                    if k != eng and deps.get(k, 0) < v:
                        deps[k] = v
        for b in writes:
            if b.w is not None:
                k, v = b.w
                if deps.get(k, 0) < v:
                    deps[k] = v
            for k, v in b.r.items():
                if deps.get(k, 0) < v:
                    deps[k] = v
        return deps

    def _wait(self, eng, deps):
        seen = self.seen[eng]
        e = self.eng[eng]
        for k, v in deps.items():
            if seen.get(k, 0) >= v:
                continue
            if k == "pe" and eng == "pe":
                continue
            h = self.sem[k] if isinstance(k, str) else k.h
            e.wait_ge(h, v)
            seen[k] = v
            self.n_ins += 1

    def op(self, eng, fn, reads=(), writes=()):
        self._wait(eng, self._deps(reads, writes, eng))
        ins = fn(self.eng[eng])
        self.cnt[eng] += 1
        c = self.cnt[eng]
        ins.then_inc(self.sem[eng], 1)
        self.n_ins += 1
        for b in reads:
            b.r[eng] = c
        for b in writes:
            b.w = (eng, c)
            b.r = {}
        return ins

    def group(self, eng, fns, reads=(), writes=()):
        self._wait(eng, self._deps(reads, writes, eng))
        e = self.eng[eng]
        ins = None
        for fn in fns:
            ins = fn(e)
            self.n_ins += 1
        self.cnt[eng] += 1
        c = self.cnt[eng]
        ins.then_inc(self.sem[eng], 1)
        for b in reads:
            b.r[eng] = c
        for b in writes:
            b.w = (eng, c)
            b.r = {}

    def dma(self, q, ds, out, in_, reads=(), writes=()):
        deps = self._deps(reads, writes)
        deps.pop(ds, None)
        self._wait(q, deps)
        ins = self.eng[q].dma_start(out=out, in_=in_)
        ds.count += 16
        ins.then_inc(ds.h, 16)
        self.n_ins += 1
        for b in reads:
            b.r[ds] = ds.count
        for b in writes:
            b.w = (ds, ds.count)
            b.r = {}

    def finish(self, q, bufs):
        self._wait(q, self._deps(bufs, bufs))


def build_nc(S=SEQ, dbg=False):
    NB = S // 128
    NOWN = NB // 4
    SO = NOWN * 128
    NG = NB // 4
    nc = bass.Bass("TRN2", target_bir_lowering=False)
    dram = lambda n, s, d, k: nc.dram_tensor(n, s, d, kind=k).ap()
    xT_d = dram("xT", [D, S], F32, "ExternalInput")
    xo_d = dram("xo", [SO, D], F32, "ExternalInput")
    pos_d = dram("pos", [128, NB], F32, "ExternalInput")
    kb0_d = dram("kb0", [1, 512], F32, "ExternalInput")
    win_d = dram("w_in", [D, NCOL], F32, "ExternalInput")
    wo_d = dram("w_o", [D, D], F32, "ExternalInput")
    wup_d = dram("w_up", [D, DFF], F32, "ExternalInput")
    wdn_d = dram("w_down", [DFF, D], F32, "ExternalInput")
    lb0_d = dram("lb0", [1, 512], F32, "ExternalInput")
    lb1_d = dram("lb1", [1, 512], F32, "ExternalInput")
    hgn_d = dram("hgn", [1, 512], F32, "ExternalInput")
    lnp_ds = [dram("lnp%d" % i, [1, D], F32, "ExternalInput") for i in range(4)]
    y_d = dram("y", [SO, D], F32, "ExternalOutput")
    SK = "ExternalOutput" if dbg else "Internal"
    KT_d = dram("KT_s", [128, 4, S], BF16, SK)
    V_d = dram("V_s", [128, NB, 520], BF16, SK)
    IKT_d = dram("IKT_s", [128, S], BF16, SK)
    QT_d = dram("QT_s", [128, 4, SO], BF16, SK)
    IQT_d = dram("IQT_s", [128, NOWN, 256], BF16, SK)
    SEL_d = dram("SEL_s", [128, NOWN, 512], BF16, SK)
    HGT_d = dram("HGT_s", [128, 4, SO], BF16, SK)
    ATT_d = dram("ATT_s", [128, 4, SO], BF16, SK)
    dbg_outs = {}

    with ExitStack() as es:
        sc = Sched(nc, es)
        op, grp, dma = sc.op, sc.group, sc.dma

        def sb(stack, name, shape, dt):
            return stack.enter_context(nc.sbuf_tensor(name, shape, dt))

        Fb = [es.enter_context(nc.psum_tensor("F%d" % i, [128, 512], F32)) for i in range(6)]
        Hb = [es.enter_context(nc.psum_tensor("H%d" % i, [128, 1024], BF16)) for i in range(2)]
        tF = [T("F%d" % i, excl=True) for i in range(6)]
        tH = [T("H%d" % i, excl=True) for i in range(2)]
        tF2s = tF[2]

        IDN = sb(es, "IDN", [128, 128], BF16)
        tC = T("consts")
        op("pool", lambda e: e.memset(IDN[:], 1.0), writes=[tC])
        op("pool", lambda e: e.affine_select(out=IDN[:], in_=IDN[:], pattern=[[-1, 128]], compare_op=ALU.is_equal,
                                             fill=0.0, base=0, channel_multiplier=1), writes=[tC])
        TRIB = sb(es, "TRIB", [128, 128], F32)
        op("pool", lambda e: e.memset(TRIB[:], 0.0), writes=[tC])
        op("pool", lambda e: e.affine_select(out=TRIB[:], in_=TRIB[:], pattern=[[-1, 128]], compare_op=ALU.is_ge,
                                             fill=NEG, base=0, channel_multiplier=1), writes=[tC])

        fin_list = []

        with ExitStack() as p1:
            Wb = sb(p1, "Wb", [128, 8, NCOL], BF16)
            tW = T("Wb")
            dW = sc.dsem("dW")
            win_v = win_d.rearrange("(c p) n -> p c n", p=128)
            for c in range(8):
                dma("pool", dW, Wb[:, c, :], win_v[:, c, :], writes=[tW])
            XG = [sb(p1, "XG%d" % i, [128, 8, 512], BF16) for i in range(2)]
            tXG = [T("XG0"), T("XG1")]
            dXG = [sc.dsem("dXG0"), sc.dsem("dXG1")]
            xT_v = xT_d.rearrange("(c p) t -> p c t", p=128)
            POS = sb(p1, "POS", [128, NB], F32)
            dMisc = sc.dsem("dMisc")
            dPOS = sc.dsem("dPOS")
            tPOS = T("POS")
            dma("sp", dPOS, POS[:], pos_d[:, :], writes=[tPOS])
            COS = sb(p1, "COS", [128, NB, 32], F32)
            SIN = sb(p1, "SIN", [128, NB, 32], F32)
            tTab = T("tab")
            with ExitStack() as pt:
                INV = sb(pt, "INV", [128, 32], F32)
                ANG = sb(pt, "ANG", [128, NB, 32], F32)
                KQ = sb(pt, "KQ", [128, NB, 32], F32)
                RR = sb(pt, "RR", [128, NB, 32], F32)
                tI, tA, tK, tR = T(), T(), T(), T()
                for dd_ in range(32):
                    op("pool", lambda e, dd_=dd_: e.memset(INV[:, dd_:dd_ + 1], float(np.float32(10000.0 ** (-dd_ / 32.0)))), writes=[tI])
                op("dve", lambda e: e.tensor_tensor(out=ANG[:], in0=POS[:].unsqueeze(2).broadcast_to([128, NB, 32]),
                                                    in1=INV[:].unsqueeze(1).broadcast_to([128, NB, 32]), op=ALU.mult),
                   reads=[tPOS, tI], writes=[tA])
                TWO_PI = 2.0 * math.pi
                C1 = 6.28125
                C2 = TWO_PI - C1
                MAGIC = 12582912.0
                for which, off, TAB in (("sin", 0.0, SIN), ("cos", 0.25, COS)):
                    op("dve", lambda e: e.tensor_scalar(out=KQ[:], in0=ANG[:], scalar1=1.0 / TWO_PI, scalar2=off,
                                                        op0=ALU.mult, op1=ALU.add), reads=[tA], writes=[tK])
                    op("dve", lambda e: e.tensor_scalar(out=KQ[:], in0=KQ[:], scalar1=MAGIC, scalar2=None, op0=ALU.add),
                       reads=[tK], writes=[tK])
                    op("dve", lambda e: e.tensor_scalar(out=KQ[:], in0=KQ[:], scalar1=-MAGIC, scalar2=None, op0=ALU.add),
                       reads=[tK], writes=[tK])
                    op("dve", lambda e: e.scalar_tensor_tensor(out=RR[:], in0=KQ[:], scalar=-C1, in1=ANG[:],
                                                               op0=ALU.mult, op1=ALU.add), reads=[tK, tA], writes=[tR])
                    op("dve", lambda e: e.scalar_tensor_tensor(out=RR[:], in0=KQ[:], scalar=-C2, in1=RR[:],
                                                               op0=ALU.mult, op1=ALU.add), reads=[tK, tR], writes=[tR])
                    if which == "cos":
                        op("dve", lambda e: e.tensor_scalar(out=RR[:], in0=RR[:], scalar1=math.pi / 2.0, scalar2=None,
                                                            op0=ALU.add), reads=[tR], writes=[tR])
                    op("dve", lambda e: e.tensor_scalar(out=RR[:], in0=RR[:], scalar1=3.14159, scalar2=-3.14159,
                                                        op0=ALU.min, op1=ALU.max), reads=[tR], writes=[tR])
                    op("act", lambda e, TAB=TAB: e.activation(out=TAB[:], in_=RR[:], func=AF.Sin), reads=[tR], writes=[tTab])
                for q in ("pe", "act", "dve", "pool", "sp"):
                    sc.finish(q, [tI, tA, tK, tR, tTab, tPOS])

            LB = sb(p1, "LB", [128, 512], F32)
            OML = sb(p1, "OML", [128, 512], F32)
            NGB = sb(p1, "NGB", [128, 512], F32)
            L1 = sb(p1, "L1", [128, 512], F32)
            tLB = T("LB")
            dma("sp", dMisc, LB[:], lb0_d[0:1, :].partition_broadcast(128), writes=[tLB])
            tL1 = T("L1")
            dL1 = sc.dsem("dL1")
            dma("sp", dL1, L1[:], lb1_d[0:1, :].partition_broadcast(128), writes=[tL1])
            tNG = T("NGB")
            dNG = sc.dsem("dNG")
            dma("sp", dNG, NGB[:], hgn_d[0:1, :].partition_broadcast(128), writes=[tNG])
            op("dve", lambda e: e.tensor_tensor(out=L1[:], in0=L1[:], in1=LB[:], op=ALU.subtract), reads=[tL1, tLB], writes=[tL1])
            op("act", lambda e: e.activation(out=L1[:], in_=L1[:], func=AF.Exp), reads=[tL1], writes=[tL1])
            op("dve", lambda e: e.tensor_scalar(out=L1[:], in0=L1[:], scalar1=1.0, scalar2=None, op0=ALU.add), reads=[tL1], writes=[tL1])
            op("dve", lambda e: e.reciprocal(out=LB[:], in_=L1[:]), reads=[tL1], writes=[tLB])
            op("dve", lambda e: e.tensor_scalar(out=OML[:], in0=LB[:], scalar1=-1.0, scalar2=1.0, op0=ALU.mult, op1=ALU.add),
               reads=[tLB], writes=[tLB])
            LT_U128 = sb(p1, "LT_U128", [128, 128], F32)
            LT_I64 = sb(p1, "LT_I64", [128, 128], F32)
            LT_U64 = sb(p1, "LT_U64", [128, 128], F32)
            IND2 = sb(p1, "IND2", [128, 2], F32)
            MASKBD = sb(p1, "MASKBD", [128, 128], F32)
            op("pool", lambda e: e.memset(LT_U128[:], 1.0), writes=[tC])
            op("pool", lambda e: e.affine_select(out=LT_U128[:], in_=LT_U128[:], pattern=[[-1, 128]], compare_op=ALU.is_gt,
                                                 fill=0.0, base=0, channel_multiplier=1), writes=[tC])
            op("pool", lambda e: e.memset(LT_I64[:], 0.0), writes=[tC])
            op("pool", lambda e: e.memset(LT_U64[:], 0.0), writes=[tC])
            for cblk in range(2):
                sl = slice(64 * cblk, 64 * cblk + 64)
                op("pool", lambda e, sl=sl: e.memset(LT_I64[sl, sl], 1.0), writes=[tC])
                op("pool", lambda e, sl=sl: e.affine_select(out=LT_I64[sl, sl], in_=LT_I64[sl, sl], pattern=[[1, 64]],
                                                            compare_op=ALU.is_ge, fill=0.0, base=0, channel_multiplier=-1), writes=[tC])
                op("pool", lambda e, sl=sl: e.memset(LT_U64[sl, sl], 1.0), writes=[tC])
                op("pool", lambda e, sl=sl: e.affine_select(out=LT_U64[sl, sl], in_=LT_U64[sl, sl], pattern=[[-1, 64]],
                                                            compare_op=ALU.is_gt, fill=0.0, base=0, channel_multiplier=1), writes=[tC])
            op("pool", lambda e: e.tensor_copy(out=MASKBD[:], in_=LT_I64[:]), writes=[tC])
            op("pool", lambda e: e.memset(IND2[:], 1.0), writes=[tC])
            op("pool", lambda e: e.memset(IND2[64:128, 0:1], 0.0), writes=[tC])
            E_u = [sb(p1, "E_u%d" % u, [128, 128], BF16) for u in range(2)]
            ETA = [sb(p1, "ETA%d" % u, [128, 128], BF16) for u in range(2)]
            ETB = [sb(p1, "ETB%d" % u, [128, 128], BF16) for u in range(2)]
            for u in range(2):
                op("pool", lambda e, u=u: e.memset(E_u[u][:], 1.0), writes=[tC])
                for hf_ in range(2):
                    ps = slice(64 * hf_, 64 * hf_ + 64)
                    op("pool", lambda e, u=u, ps=ps: e.affine_select(out=E_u[u][ps, :], in_=E_u[u][ps, :], pattern=[[1, 128]],
                                                                      compare_op=ALU.is_equal, fill=0.0, base=-64 * u,
                                                                      channel_multiplier=-1), writes=[tC])
                op("pool", lambda e, u=u: e.memset(ETA[u][:], 0.0), writes=[tC])
                op("pool", lambda e, u=u: e.memset(ETB[u][:], 0.0), writes=[tC])
                op("pool", lambda e, u=u: e.memset(ETA[u][:, 0:64], 1.0), writes=[tC])
                op("pool", lambda e, u=u: e.memset(ETB[u][:, 64:128], 1.0), writes=[tC])
                op("pool", lambda e, u=u: e.affine_select(out=ETA[u][:, 0:64], in_=ETA[u][:, 0:64], pattern=[[1, 64]],
                                                          compare_op=ALU.is_equal, fill=0.0, base=64 * u, channel_multiplier=-1), writes=[tC])
                op("pool", lambda e, u=u: e.affine_select(out=ETB[u][:, 64:128], in_=ETB[u][:, 64:128], pattern=[[1, 64]],
                                                          compare_op=ALU.is_equal, fill=0.0, base=64 * u, channel_multiplier=-1), writes=[tC])

            KTS = [sb(p1, "KTS%d" % i, [128, 4, 512], BF16) for i in range(2)]
            VS = [sb(p1, "VS%d" % i, [128, 4, 520], BF16) for i in range(2)]
            IKS = [sb(p1, "IKS%d" % i, [128, 512], BF16) for i in range(2)]
            tKTS = [T(), T()]; tVS = [T(), T()]; tIKS = [T(), T()]
            dKTS = [sc.dsem("dKTS0"), sc.dsem("dKTS1")]
            dVS = [sc.dsem("dVS0"), sc.dsem("dVS1")]
            dIKS = [sc.dsem("dIKS0"), sc.dsem("dIKS1")]
            for i in range(2):
                op("pool", lambda e, i=i: e.memset(VS[i][:], 1.0), writes=[tVS[i]])
            ST_ = sb(p1, "STATE", [128, 4, 128], F32)
            tST = T("state")
            op("pool", lambda e: e.memset(ST_[:], 0.0), writes=[tST])

            def tmp(name, shape, dt):
                return sb(p1, name, shape, dt), T(name)
            KR, tKR = tmp("KR", [128, 512], BF16)
            TA, tTA = tmp("TA", [128, 512], F32)
            TB_, tTB = tmp("TB", [128, 256], F32)
            TC_, tTC = tmp("TC", [128, 256], F32)
            XR, tXR = tmp("XR", [128, 320], F32)
            IKR, tIKR = tmp("IKR", [128, 128], BF16)
            EZ, tEZ = tmp("EZ", [128, 512], F32)
            FG, tFG = tmp("FG", [128, 512], F32)
            LF, tLF = tmp("LF", [128, 512], F32)
            KK, tKK = tmp("KK", [128, 512], F32)
            DP, tDP = tmp("DP", [128, 512], F32)
            DK, tDK = tmp("DK", [128, 512], BF16)
            VH, tVH = tmp("VH", [128, 512], BF16)
            DCOL, tDCOL = tmp("DCOL", [128, 8], F32)
            SB16, tSB16 = tmp("SB16", [128, 4, 128], BF16)
            AW, tAW = tmp("AW", [128, 4], F32)
            SGN, tSGN = tmp("SGN", [128, 4], BF16)
            IQS, tIQS = tmp("IQS", [128, 256], BF16)
            AQ, tAQ = tmp("AQ", [128, 512], F32)
            BN, tBN = tmp("BN", [128, 512], F32)
            D64, tD64 = tmp("D64", [128, 512], F32)
            QA, tQA = tmp("QA", [128, 512], BF16)
            KBm, tKBm = tmp("KBm", [128, 512], BF16)
            KD, tKD = tmp("KD", [128, 512], BF16)
            QAT, tQAT = tmp("QAT", [128, 4, 128], BF16)
            QAI, tQAI = tmp("QAI", [128, 4, 128], BF16)
            KBT, tKBT = tmp("KBT", [128, 4, 128], BF16)
            KDT, tKDT = tmp("KDT", [128, 4, 64], BF16)
            AT, tAT = tmp("AT", [128, 4, 128], BF16)
            MS, tMS = tmp("MS", [128, 4], F32)
            RS, tRS = tmp("RS", [128, 4], F32)
            JK, tJK = tmp("JK", [128, 128], F32)
            GS, tGS = tmp("GS", [128, 512], F32)
            OG, tOG = tmp("OG", [128, 512], F32)
            OGB, tOGB = tmp("OGB", [128, 512], BF16)
            QTS = [sb(p1, "QTS%d" % i, [128, 4, 128], BF16) for i in range(2)]
            IQTS = [sb(p1, "IQTS%d" % i, [128, 2, 2, 64], BF16) for i in range(2)]
            SELS = [sb(p1, "SELS%d" % i, [128, 4, 128], BF16) for i in range(2)]
            HGTS = [sb(p1, "HGTS%d" % i, [128, 4, 128], BF16) for i in range(2)]
            tQTS = [T(), T()]; tIQTS = [T(), T()]; tSELS = [T(), T()]; tHGTS = [T(), T()]
            dQTS = [sc.dsem("dQTS0"), sc.dsem("dQTS1")]
            dIQTS = [sc.dsem("dIQTS0"), sc.dsem("dIQTS1")]
            dSELS = [sc.dsem("dSELS0"), sc.dsem("dSELS1")]
            dHGTS = [sc.dsem("dHGTS0"), sc.dsem("dHGTS1")]
            tKTd = [T() for _ in range(NG)]
            tVd = [T() for _ in range(NG)]
            tIKTd = [T() for _ in range(NG)]
            tQTd = [T() for _ in range(NOWN)]
            tIQTd = [T() for _ in range(NOWN)]
            tSELd = [T() for _ in range(NOWN)]
            tHGTd = [T() for _ in range(NOWN)]
            tATTd = [T() for _ in range(NOWN)]

            def proj(bank, tb, xs, r, c0, w, extra_reads=()):
                grp("pe", [(lambda e, c=c: e.matmul(bank[:, 0:w], lhsT=xs[:, c, 128 * r:128 * r + 128], rhs=Wb[:, c, c0:c0 + w],
                                                     start=(c == 0), stop=(c == 7))) for c in range(8)],
                    reads=[tW] + list(extra_reads), writes=[tb])

            def rope(src, tsrc, nh, n, dst, tdst):
                cosb = COS[:, n, :].unsqueeze(1).broadcast_to([128, 2 * nh, 32])
                sinb = SIN[:, n, :].unsqueeze(1).broadcast_to([128, nh, 32])
                s4 = src.rearrange("p (h two d) -> p h two d", two=2, d=32)
                d4 = dst.rearrange("p (h two d) -> p h two d", two=2, d=32)
                ta = TA[:, 0:nh * 64]
                tb = TB_[:, 0:nh * 32].rearrange("p (h d) -> p h d", d=32)
                tc_ = TC_[:, 0:nh * 32].rearrange("p (h d) -> p h d", d=32)
                ta4 = ta.rearrange("p (h two d) -> p h two d", two=2, d=32)
                op("dve", lambda e: e.tensor_tensor(out=ta.rearrange("p (g d) -> p g d", d=32),
                                                    in0=src.rearrange("p (g d) -> p g d", d=32), in1=cosb, op=ALU.mult),
                   reads=[tsrc, tTab], writes=[tTA])
                op("dve", lambda e: e.tensor_tensor(out=tb, in0=s4[:, :, 1, :], in1=sinb, op=ALU.mult), reads=[tsrc, tTab], writes=[tTB])
                op("dve", lambda e: e.tensor_tensor(out=tc_, in0=s4[:, :, 0, :], in1=sinb, op=ALU.mult), reads=[tsrc, tTab], writes=[tTC])
                op("dve", lambda e: e.tensor_tensor(out=d4[:, :, 0, :], in0=ta4[:, :, 0, :], in1=tb, op=ALU.subtract),
                   reads=[tTA, tTB], writes=[tdst])
                op("dve", lambda e: e.tensor_tensor(out=d4[:, :, 1, :], in0=ta4[:, :, 1, :], in1=tc_, op=ALU.add),
                   reads=[tTA, tTC], writes=[tdst])

            def transposes(src, tsrc, nblk, hb, out_cols=128, in_rows=slice(0, 128)):
                grp("pe", [(lambda e, i=i: e.transpose(out=Hb[hb][:, i * out_cols:(i + 1) * out_cols],
                                                       in_=src[in_rows, 128 * i:128 * i + 128], identity=IDN[in_rows, in_rows]))
                           for i in range(nblk)], reads=[tsrc, tC], writes=[tH[hb]])

            for n in range(NB):
                g, r = divmod(n, 4)
                own = (r == 3)
                m = g
                xs = XG[g % 2]
                if r == 0:
                    dma("pool", dXG[g % 2], xs[:], xT_v[:, :, 512 * g:512 * g + 512], writes=[tXG[g % 2]])
                txs = tXG[g % 2]
                proj(Fb[0], tF[0], xs, r, C_K, 512, [txs])
                proj(Fb[1], tF[1], xs, r, C_V, 512, [txs])
                proj(Fb[2], tF[2], xs, r, C_IQ, 324, [txs])
                proj(Fb[3], tF[3], xs, r, C_HF, 512, [txs])
                proj(Fb[4], tF[4], xs, r, C_HI, 512, [txs])
                rope(Fb[0][:, :], tF[0], 8, n, KR[:, :], tKR)
                transposes(KR, tKR, 4, 0)
                op("act", lambda e: e.activation(out=KTS[g % 2][:, :, 128 * r:128 * r + 128],
                                                 in_=Hb[0][:, 0:512].rearrange("p (a t) -> p a t", a=4), func=AF.Copy),
                   reads=[tH[0]], writes=[tKTS[g % 2]])
                op("act", lambda e: e.activation(out=VS[g % 2][:, r, :].rearrange("p (h e) -> p h e", e=65)[:, :, 0:64],
                                                 in_=Fb[1][:, :].rearrange("p (h d) -> p h d", d=64), func=AF.Copy),
                   reads=[tF[1]], writes=[tVS[g % 2]])
                rope(Fb[2][:, 0:320], tF[2], 5, n, XR[:, :], tXR)
                op("dve", lambda e: e.tensor_copy(out=IKR[:, :].rearrange("p (a d) -> p a d", a=2),
                                                  in_=XR[:, 256:320].unsqueeze(1).broadcast_to([128, 2, 64])),
                   reads=[tXR], writes=[tIKR])
                transposes(IKR, tIKR, 1, 1)
                op("act", lambda e: e.activation(out=IKS[g % 2][:, 128 * r:128 * r + 128], in_=Hb[1][:, 0:128], func=AF.Copy),
                   reads=[tH[1]], writes=[tIKS[g % 2]])
                op("act", lambda e: e.activation(out=EZ[:], in_=Fb[3][:, :], func=AF.Exp, scale=-1.0), reads=[tF[3]], writes=[tEZ])
                op("pool", lambda e: e.tensor_scalar(out=EZ[:], in0=EZ[:], scalar1=1.0, scalar2=None, op0=ALU.add), reads=[tEZ], writes=[tEZ])
                op("dve", lambda e: e.reciprocal(out=FG[:], in_=EZ[:]), reads=[tEZ], writes=[tFG])
                op("pool", lambda e: e.tensor_tensor(out=FG[:], in0=FG[:], in1=OML[:], op=ALU.mult), reads=[tFG, tLB], writes=[tFG])
                op("pool", lambda e: e.tensor_tensor(out=FG[:], in0=FG[:], in1=LB[:], op=ALU.add), reads=[tFG, tLB], writes=[tFG])
                op("act", lambda e: e.activation(out=LF[:], in_=FG[:], func=AF.Ln), reads=[tFG], writes=[tLF])
                op("pool", lambda e: e.tensor_scalar(out=KK[:], in0=FG[:], scalar1=-1.0, scalar2=1.0, op0=ALU.mult, op1=ALU.add),
                   reads=[tFG], writes=[tKK])
                grp("pe", [lambda e: e.matmul(Fb[3][:, :], lhsT=LT_U128[:], rhs=LF[:], start=True, stop=True)],
                    reads=[tLF, tC], writes=[tF[3]])
                op("act", lambda e: e.activation(out=DP[:], in_=Fb[3][:, :], func=AF.Exp), reads=[tF[3]], writes=[tDP])
                op("pool", lambda e: e.tensor_tensor(out=DK[:], in0=KK[:], in1=DP[:], op=ALU.mult), reads=[tKK, tDP], writes=[tDK])
                op("act", lambda e: e.activation(out=VH[:], in_=Fb[4][:, :], func=AF.Copy), reads=[tF[4]], writes=[tVH])
                grp("pe", [(lambda e, h=h: e.matmul(Fb[2][:, 384 + 2 * h:386 + 2 * h], lhsT=LF[:, 128 * h:128 * h + 128], rhs=IND2[:],
                                                    start=True, stop=True)) for h in range(4)],
                    reads=[tLF, tC], writes=[tF2s])
                op("act", lambda e: e.activation(out=DCOL[:], in_=Fb[2][:, 384:392], func=AF.Exp), reads=[tF2s], writes=[tDCOL])
                if own:
                    op("act", lambda e: e.activation(out=SB16[:], in_=ST_[:], func=AF.Copy), reads=[tST], writes=[tSB16])
                grp("pe", [(lambda e, h=h: e.matmul(Fb[5][:, 128 * h:128 * h + 128], lhsT=DK[:, 128 * h:128 * h + 128],
                                                    rhs=VH[:, 128 * h:128 * h + 128], start=True, stop=True)) for h in range(4)],
                    reads=[tDK, tVH], writes=[tF[5]])
                op("dve", lambda e: e.tensor_tensor(out=ST_[:], in0=ST_[:],
                                                    in1=DCOL[:, :].rearrange("p (h c) -> p h c", c=2)[:, :, 1:2].broadcast_to([128, 4, 128]),
                                                    op=ALU.mult), reads=[tST, tDCOL], writes=[tST])
                op("dve", lambda e: e.tensor_tensor(out=ST_[:], in0=ST_[:], in1=Fb[5][:, :].rearrange("p (h v) -> p h v", h=4), op=ALU.add),
                   reads=[tST, tF[5]], writes=[tST])

                if own:
                    sl = m % 2
                    proj(Fb[0], tF[0], xs, r, C_Q, 512, [txs])
                    rope(Fb[0][:, :], tF[0], 8, n, KR[:, :], tKR)
                    transposes(KR, tKR, 4, 0)
                    op("act", lambda e: e.activation(out=QTS[sl][:], in_=Hb[0][:, 0:512].rearrange("p (a t) -> p a t", a=4), func=AF.Copy),
                       reads=[tH[0]], writes=[tQTS[sl]])
                    dma("sp", dQTS[sl], QT_d[:, :, 128 * m:128 * m + 128], QTS[sl][:], reads=[tQTS[sl]], writes=[tQTd[m]])
                    op("dve", lambda e: e.tensor_scalar(out=TC_[:, 0:4], in0=Fb[2][:, 320:324], scalar1=0.0, scalar2=2.0,
                                                        op0=ALU.is_ge, op1=ALU.mult), reads=[tF[2]], writes=[tTC])
                    op("dve", lambda e: e.tensor_scalar(out=TC_[:, 4:8], in0=TC_[:, 0:4], scalar1=-1.0, scalar2=None, op0=ALU.add),
                       reads=[tTC], writes=[tTC])
                    op("dve", lambda e: e.tensor_copy(out=SGN[:], in_=TC_[:, 4:8]), reads=[tTC], writes=[tSGN])
                    op("dve", lambda e: e.scalar_tensor_tensor(out=AW[:], in0=Fb[2][:, 320:324], scalar=IDX_SCALE, in1=TC_[:, 4:8],
                                                               op0=ALU.mult, op1=ALU.mult), reads=[tF[2], tTC], writes=[tAW])
                    op("dve", lambda e: e.tensor_tensor(out=IQS[:, :].rearrange("p (h d) -> p h d", d=64),
                                                        in0=XR[:, 0:256].rearrange("p (h d) -> p h d", d=64),
                                                        in1=AW[:, :].unsqueeze(2).broadcast_to([128, 4, 64]), op=ALU.mult),
                       reads=[tXR, tAW], writes=[tIQS])
                    transposes(IQS, tIQS, 2, 1)
                    op("act", lambda e: e.activation(out=IQTS[sl][:].rearrange("p u a t -> p a u t"), in_=Hb[1][:, 0:256].rearrange("p (a u t) -> p a u t", a=2, u=2), func=AF.Copy),
                       reads=[tH[1]], writes=[tIQTS[sl]])
                    dma("sp", dIQTS[sl], IQT_d[:, m, :], IQTS[sl][:].rearrange("p u a t -> p (u a t)"), reads=[tIQTS[sl]], writes=[tIQTd[m]])
                    fns = []
                    for u in range(2):
                        fns.append(lambda e, u=u: e.matmul(Fb[2][:, 400 + 2 * u:402 + 2 * u], lhsT=ETA[u][:], rhs=SGN[:, 0:2], start=True, stop=False))
                        fns.append(lambda e, u=u: e.matmul(Fb[2][:, 400 + 2 * u:402 + 2 * u], lhsT=ETB[u][:], rhs=SGN[:, 2:4], start=False, stop=True))
                    grp("pe", fns, reads=[tSGN, tC], writes=[tF2s])
                    for hp in range(2):
                        for u in range(2):
                            op("dve", lambda e, hp=hp, u=u: e.tensor_scalar(out=SELS[sl][:, 2 * hp + u, :], in0=E_u[u][:],
                                                                            scalar1=Fb[2][:, 400 + 2 * u + hp:401 + 2 * u + hp],
                                                                            scalar2=None, op0=ALU.mult),
                               reads=[tF2s, tC], writes=[tSELS[sl]])
                    dma("sp", dSELS[sl], SEL_d[:, m, :], SELS[sl][:].rearrange("p a t -> p (a t)"), reads=[tSELS[sl]], writes=[tSELd[m]])
                    proj(Fb[1], tF[1], xs, r, C_HQ, 512, [txs])
                    proj(Fb[4], tF[4], xs, r, C_HG, 512, [txs])
                    grp("pe", [lambda e: e.matmul(Fb[3][:, :], lhsT=LT_I64[:], rhs=LF[:], start=True, stop=True)], reads=[tLF, tC], writes=[tF[3]])
                    op("act", lambda e: e.activation(out=AQ[:], in_=Fb[3][:, :], func=AF.Exp), reads=[tF[3]], writes=[tAQ])
                    op("act", lambda e: e.activation(out=BN[:], in_=Fb[3][:, :], func=AF.Exp, scale=-1.0), reads=[tF[3]], writes=[tBN])
                    grp("pe", [lambda e: e.matmul(Fb[3][:, :], lhsT=LT_U64[:], rhs=LF[:], start=True, stop=True)], reads=[tLF, tC], writes=[tF[3]])
                    op("act", lambda e: e.activation(out=D64[:], in_=Fb[3][:, :], func=AF.Exp), reads=[tF[3]], writes=[tD64])
                    op("dve", lambda e: e.tensor_tensor(out=QA[:], in0=Fb[1][:, :], in1=AQ[:], op=ALU.mult), reads=[tF[1], tAQ], writes=[tQA])
                    op("pool", lambda e: e.tensor_tensor(out=KBm[:], in0=KK[:], in1=BN[:], op=ALU.mult), reads=[tKK, tBN], writes=[tKBm])
                    op("pool", lambda e: e.tensor_tensor(out=KD[:], in0=KK[:], in1=D64[:], op=ALU.mult), reads=[tKK, tD64], writes=[tKD])
                    transposes(QA, tQA, 4, 0)
                    h0v = Hb[0][:, 0:512].rearrange("p (a t) -> p a t", a=4)
                    op("act", lambda e: e.activation(out=QAT[:], in_=h0v, func=AF.Copy), reads=[tH[0]], writes=[tQAT])
                    op("act", lambda e: e.activation(out=QAI[:, :, 0:64], in_=h0v[:, :, 0:64], func=AF.Copy), reads=[tH[0]], writes=[tQAI])
                    op("dve", lambda e: e.tensor_tensor(out=QAI[:, :, 64:128], in0=h0v[:, :, 64:128],
                                                        in1=DCOL[:, :].rearrange("p (h c) -> p h c", c=2)[:, :, 0:1].broadcast_to([128, 4, 64]),
                                                        op=ALU.mult), reads=[tH[0], tDCOL], writes=[tQAI])
                    transposes(KBm, tKBm, 4, 1)
                    op("act", lambda e: e.activation(out=KBT[:], in_=Hb[1][:, 0:512].rearrange("p (a t) -> p a t", a=4), func=AF.Copy),
                       reads=[tH[1]], writes=[tKBT])
                    transposes(KD, tKD, 4, 0, out_cols=64, in_rows=slice(0, 64))
                    op("act", lambda e: e.activation(out=KDT[:], in_=Hb[0][:, 0:256].rearrange("p (a t) -> p a t", a=4), func=AF.Copy),
                       reads=[tH[0]], writes=[tKDT])
                    grp("pe", [(lambda e, h=h: e.matmul(Fb[5][:, 128 * h:128 * h + 128], lhsT=KBT[:, h, :], rhs=QAT[:, h, :], start=True, stop=True))
                               for h in range(4)], reads=[tKBT, tQAT], writes=[tF[5]])
                    op("dve", lambda e: e.tensor_tensor(out=AT[:], in0=Fb[5][:, :].rearrange("p (h t) -> p h t", h=4),
                                                        in1=MASKBD[:, :].unsqueeze(1).broadcast_to([128, 4, 128]), op=ALU.mult),
                       reads=[tF[5], tC], writes=[tAT])
                    grp("pe", [(lambda e, h=h: e.matmul(Fb[0][0:64, 64 * h:64 * h + 64], lhsT=KDT[:, h, :], rhs=QAT[:, h, 64:128], start=True, stop=True))
                               for h in range(4)], reads=[tKDT, tQAT], writes=[tF[0]])
                    op("dve", lambda e: e.tensor_copy(out=AT[0:64, :, 64:128], in_=Fb[0][0:64, 0:256].rearrange("p (h t) -> p h t", h=4)),
                       reads=[tF[0]], writes=[tAT])
                    fns = []
                    for h in range(4):
                        fns.append(lambda e, h=h: e.matmul(Fb[5][:, 128 * h:128 * h + 128], lhsT=AT[:, h, :], rhs=VH[:, 128 * h:128 * h + 128], start=True, stop=False))
                        fns.append(lambda e, h=h: e.matmul(Fb[5][:, 128 * h:128 * h + 128], lhsT=QAI[:, h, :], rhs=SB16[:, h, :], start=False, stop=True))
                    grp("pe", fns, reads=[tAT, tVH, tQAI, tSB16], writes=[tF[5]])
                    for h in range(4):
                        op("act", lambda e, h=h: e.activation(out=JK[:], in_=Fb[5][:, 128 * h:128 * h + 128], func=AF.Square, accum_out=MS[:, h:h + 1]),
                           reads=[tF[5]], writes=[tMS, tJK])
                    op("dve", lambda e: e.tensor_scalar(out=MS[:], in0=MS[:], scalar1=1.0 / 128.0, scalar2=RMS_EPS, op0=ALU.mult, op1=ALU.add),
                       reads=[tMS], writes=[tMS])
                    op("act", lambda e: e.activation(out=RS[:], in_=MS[:], func=AF.Ln), reads=[tMS], writes=[tRS])
                    op("act", lambda e: e.activation(out=RS[:], in_=RS[:], func=AF.Exp, scale=-0.5), reads=[tRS], writes=[tRS])
                    op("act", lambda e: e.activation(out=GS[:], in_=Fb[4][:, :], func=AF.Exp, scale=-1.0), reads=[tF[4]], writes=[tGS])
                    op("pool", lambda e: e.tensor_scalar(out=GS[:], in0=GS[:], scalar1=1.0, scalar2=None, op0=ALU.add), reads=[tGS], writes=[tGS])
                    op("dve", lambda e: e.reciprocal(out=GS[:], in_=GS[:]), reads=[tGS], writes=[tGS])
                    op("dve", lambda e: e.tensor_tensor(out=GS[:], in0=Fb[4][:, :], in1=GS[:], op=ALU.mult), reads=[tF[4], tGS], writes=[tGS])
                    op("dve", lambda e: e.tensor_tensor(out=OG[:, :].rearrange("p (h v) -> p h v", h=4),
                                                        in0=Fb[5][:, :].rearrange("p (h v) -> p h v", h=4),
                                                        in1=RS[:, :].unsqueeze(2).broadcast_to([128, 4, 128]), op=ALU.mult),
                       reads=[tF[5], tRS], writes=[tOG])
                    op("pool", lambda e: e.tensor_tensor(out=OG[:], in0=OG[:], in1=NGB[:], op=ALU.mult), reads=[tOG, tNG], writes=[tOG])
                    op("pool", lambda e: e.tensor_tensor(out=OGB[:], in0=OG[:], in1=GS[:], op=ALU.mult), reads=[tOG, tGS], writes=[tOGB])
                    transposes(OGB, tOGB, 4, 1)
                    op("act", lambda e: e.activation(out=HGTS[sl][:], in_=Hb[1][:, 0:512].rearrange("p (a t) -> p a t", a=4), func=AF.Copy),
                       reads=[tH[1]], writes=[tHGTS[sl]])
                    dma("sp", dHGTS[sl], HGT_d[:, :, 128 * m:128 * m + 128], HGTS[sl][:], reads=[tHGTS[sl]], writes=[tHGTd[m]])
                    dma("sp", dKTS[g % 2], KT_d[:, :, 512 * g:512 * g + 512], KTS[g % 2][:], reads=[tKTS[g % 2]], writes=[tKTd[g]])
                    dma("sp", dVS[g % 2], V_d[:, 4 * g:4 * g + 4, :], VS[g % 2][:], reads=[tVS[g % 2]], writes=[tVd[g]])
                    dma("sp", dIKS[g % 2], IKT_d[:, 512 * g:512 * g + 512], IKS[g % 2][:], reads=[tIKS[g % 2]], writes=[tIKTd[g]])
            if dbg:
                for nm, tl, tt in [("FG", FG, tFG), ("LF", LF, tLF), ("KK", KK, tKK), ("DP", DP, tDP), ("DCOL", DCOL, tDCOL),
                                   ("ST", ST_, tST), ("AQ", AQ, tAQ), ("BN", BN, tBN), ("D64", D64, tD64), ("QAT", QAT, tQAT),
                                   ("AT", AT, tAT), ("OG", OG, tOG), ("GS", GS, tGS), ("RS", RS, tRS), ("SB16", SB16, tSB16),
                                   ("COS", COS, tTab), ("SIN", SIN, tTab), ("VH", VH, tVH), ("QAI", QAI, tQAI), ("KBT", KBT, tKBT)]:
                    shp = list(tl[:].shape)
                    od = dram("dbg_" + nm, shp, tl[:].dtype, "ExternalOutput")
                    dsx = sc.dsem("dd_" + nm)
                    dma("sp", dsx, od, tl[:], reads=[tt])
                    sc._wait("sp", {dsx: dsx.count})
            p1_bufs = [tW, tXG[0], tXG[1], tTab, tLB, tL1, tNG, tC, tST, tKR, tTA, tTB, tTC, tXR, tIKR, tEZ, tFG, tLF, tKK, tDP, tDK, tVH,
                       tDCOL, tSB16, tAW, tSGN, tIQS, tAQ, tBN, tD64, tQA, tKBm, tKD, tQAT, tQAI, tKBT, tKDT, tAT, tMS, tRS, tJK, tGS, tOG,
                       tOGB, tPOS] + tKTS + tVS + tIKS + tQTS + tIQTS + tSELS + tHGTS
            for q in ("pe", "act", "dve", "pool", "sp"):
                sc.finish(q, p1_bufs)

        with ExitStack() as p2:
            IKT = sb(p2, "IKT", [128, S], BF16)
            tIKT = T("IKT")
            dIKT = sc.dsem("dIKT")
            for g in range(NG):
                dma("sp", dIKT, IKT[:, 512 * g:512 * g + 512], IKT_d[:, 512 * g:512 * g + 512], reads=[tIKTd[g]], writes=[tIKT])
            SCR = sb(p2, "SCR", [128, S], F32)
            tSCR = T("SCR")
            KB0 = sb(p2, "KB0", [128, 512], F32)
            KB0D = sb(p2, "KB0D", [128, 512], F32)
            tKB0 = T("KB0")
            dKB0 = sc.dsem("dKB0")
            dma("sp", dKB0, KB0[:], kb0_d[0:1, :].partition_broadcast(128), writes=[tKB0])
            op("dve", lambda e: e.tensor_copy(out=KB0D[:], in_=KB0[:]), reads=[tKB0], writes=[tKB0])
            op("dve", lambda e: e.tensor_tensor(out=KB0D[:, 384:512], in0=KB0D[:, 384:512], in1=TRIB[:], op=ALU.add), reads=[tKB0, tC], writes=[tKB0])
            HALF = sb(p2, "HALF", [128, 1], F32)
            op("dve", lambda e: e.memset(HALF[:], 0.5), writes=[tKB0])
            QTB = [sb(p2, "QTB%d" % i, [128, 4, 128], BF16) for i in range(2)]
            IQTB = [sb(p2, "IQTB%d" % i, [128, 256], BF16) for i in range(2)]
            SELB = [sb(p2, "SELB%d" % i, [128, 4, 128], BF16) for i in range(2)]
            tQL = [T(), T()]
            dQL = [sc.dsem("dQL0"), sc.dsem("dQL1")]
            RB = [sb(p2, "RB%d" % i, [128, 512], BF16) for i in range(8)]
            tRB = [T() for _ in range(8)]
            MX = sb(p2, "MX", [128, 32], F32); tMX = T()
            MNt = sb(p2, "MN", [128, 32], F32); tMN = T()
            HI = sb(p2, "HI", [128, 1], F32); LO = sb(p2, "LO", [128, 1], F32); MID = sb(p2, "MID", [128, 1], F32)
            CH = sb(p2, "CH", [128, 1], F32); CNT = [sb(p2, "CNT%d" % i, [128, 1], F32) for i in range(2)]
            DD = sb(p2, "DD", [128, 1], F32); MNEED = sb(p2, "MNEED", [128, 1], F32); CARRY = sb(p2, "CARRY", [128, 1], F32)
            SELI = sb(p2, "SELI", [128, 1], I32); NSEL = sb(p2, "NSEL", [128, 1], I32)
            tBS = T("bisect")
            JUNK = sb(p2, "JUNK", [128, 2048], BF16); tJUNK = T()
            AB = [sb(p2, "AB%d" % i, [128, 2048], BF16) for i in range(2)]; tAB = [T(), T()]
            PF = sb(p2, "PF", [128, 2048], F32); tPF = T()
            TM = sb(p2, "TM", [128, 2048], BF16); tTM = T()
            MK = sb(p2, "MK", [128, 2048], BF16); tMK = T()
            MT = sb(p2, "MT", [128, NB, 128], BF16); tMT = T()
            KTB = [sb(p2, "KTB%d" % i, [128, 4, 512], BF16) for i in range(2)]
            VB = [sb(p2, "VB%d" % i, [128, 4, 520], BF16) for i in range(2)]
            tKTB = [T(), T()]; tVB = [T(), T()]
            dKTB = [sc.dsem("dKTB0"), sc.dsem("dKTB1")]; dVB = [sc.dsem("dVB0"), sc.dsem("dVB1")]
            EX = [sb(p2, "EX%d" % i, [128, 4, 128], BF16) for i in range(4)]; tEX = [T() for _ in range(4)]
            PM = [sb(p2, "PM%d" % i, [128, 4, 128], BF16) for i in range(4)]; tPM = [T() for _ in range(4)]
            RC = sb(p2, "RC", [128, 8], F32); tRC = T()
            ATo = sb(p2, "ATo", [128, 512], BF16); tATo = T()
            ATS = [sb(p2, "ATS%d" % i, [128, 4, 128], BF16) for i in range(2)]; tATS = [T(), T()]
            dATS = [sc.dsem("dATS0"), sc.dsem("dATS1")]
            kchunk = 0

            for m in range(NOWN):
                sl = m % 2
                ng = m + 1
                nk = 512 * ng
                nkb = 4 * ng
                dma("sp", dQL[sl], QTB[sl][:], QT_d[:, :, 128 * m:128 * m + 128], reads=[tQTd[m]], writes=[tQL[sl]])
                dma("sp", dQL[sl], IQTB[sl][:], IQT_d[:, m, :], reads=[tIQTd[m]], writes=[tQL[sl]])
                dma("sp", dQL[sl], SELB[sl][:].rearrange("p a t -> p (a t)"), SEL_d[:, m, :], reads=[tSELd[m]], writes=[tQL[sl]])
                for G in range(ng):
                    rb0 = 4 * (G % 2)
                    for hp in range(2):
                        for u in range(2):
                            i4 = 2 * hp + u
                            ps = slice(64 * hp, 64 * hp + 64)
                            grp("pe", [lambda e, ps=ps, u=u, i4=i4: e.matmul(Fb[i4][:, :], lhsT=IQTB[sl][ps, 128 * u:128 * u + 128],
                                                                             rhs=IKT[ps, 512 * G:512 * G + 512], start=True, stop=True)],
                                reads=[tQL[sl], tIKT], writes=[tF[i4]])
                            op("act", lambda e, i4=i4: e.activation(out=RB[rb0 + i4][:], in_=Fb[i4][:, :], func=AF.Relu),
                               reads=[tF[i4]], writes=[tRB[rb0 + i4]])
                    fb = 4 + (G % 2)
                    grp("pe", [(lambda e, i4=i4: e.matmul(Fb[fb][:, :], lhsT=SELB[sl][:, i4, :], rhs=RB[rb0 + i4][:], start=(i4 == 0), stop=(i4 == 3)))
                               for i4 in range(4)], reads=[tQL[sl]] + [tRB[rb0 + i] for i in range(4)], writes=[tF[fb]])
                    op("dve", lambda e: e.tensor_reduce(out=MNt[:, G:G + 1], in_=Fb[fb][:, :], axis=AX.X, op=ALU.min),
                       reads=[tF[fb]], writes=[tMN])
                    csl = slice(512 * G, 512 * G + 512)
                    if G == 0:
                        kb = KB0D if m == 0 else KB0
                        op("dve", lambda e, kb=kb: e.tensor_tensor(out=SCR[:, csl], in0=Fb[fb][:, :], in1=kb[:], op=ALU.add),
                           reads=[tF[fb], tKB0], writes=[tSCR])
                    elif G == m:
                        op("dve", lambda e: e.tensor_copy(out=SCR[:, 512 * G:512 * G + 384], in_=Fb[fb][:, 0:384]), reads=[tF[fb]], writes=[tSCR])
                        op("dve", lambda e: e.tensor_tensor(out=SCR[:, 512 * G + 384:512 * G + 512], in0=Fb[fb][:, 384:512], in1=TRIB[:], op=ALU.add),
                           reads=[tF[fb], tC], writes=[tSCR])
                    else:
                        op("act", lambda e: e.activation(out=SCR[:, csl], in_=Fb[fb][:, :], func=AF.Copy), reads=[tF[fb]], writes=[tSCR])
                    op("dve", lambda e: e.tensor_reduce(out=MX[:, G:G + 1], in_=SCR[:, csl], axis=AX.X, op=ALU.max), reads=[tSCR], writes=[tMX])
                op("dve", lambda e: e.tensor_reduce(out=HI[:], in_=MX[:, 0:ng], axis=AX.X, op=ALU.max), reads=[tMX], writes=[tBS])
                op("dve", lambda e: e.tensor_reduce(out=LO[:], in_=MNt[:, 0:ng], axis=AX.X, op=ALU.min), reads=[tMN, tBS], writes=[tBS])
                op("dve", lambda e: e.tensor_tensor(out=DD[:], in0=HI[:], in1=LO[:], op=ALU.subtract), reads=[tBS], writes=[tBS])
                op("dve", lambda e: e.scalar_tensor_tensor(out=HI[:], in0=DD[:], scalar=1e-3, in1=HI[:], op0=ALU.mult, op1=ALU.add), reads=[tBS], writes=[tBS])
                op("dve", lambda e: e.tensor_scalar(out=HI[:], in0=HI[:], scalar1=1e-6, scalar2=None, op0=ALU.add), reads=[tBS], writes=[tBS])
                op("dve", lambda e: e.memset(CH[:], 0.0), reads=[tBS], writes=[tBS])
                chunks = [(c0, min(2048, nk - c0)) for c0 in range(0, nk, 2048)]
                for it in range(NIT):
                    op("dve", lambda e: e.scalar_tensor_tensor(out=MID[:], in0=LO[:], scalar=HI[:, 0:1], in1=HALF[:], op0=ALU.add, op1=ALU.mult),
                       reads=[tBS, tKB0], writes=[tBS])
                    for ci, (c0, w) in enumerate(chunks):
                        cprev = CNT[(ci + 1) % 2]
                        ccur = CNT[ci % 2]
                        op("dve", lambda e, c0=c0, w=w, ci=ci, cprev=cprev, ccur=ccur: e.tensor_scalar(
                            out=JUNK[:, 0:w], in0=SCR[:, c0:c0 + w], scalar1=MID[:, 0:1], scalar2=(None if ci == 0 else cprev[:, 0:1]),
                            op0=ALU.is_ge, op1=ALU.add, accum_out=ccur[:, 0:1]), reads=[tSCR, tBS], writes=[tBS, tJUNK])
                    cfin = CNT[(len(chunks) - 1) % 2]
                    op("dve", lambda e: e.tensor_scalar(out=SELI[:], in0=cfin[:], scalar1=TOPK, scalar2=None, op0=ALU.is_ge), reads=[tBS], writes=[tBS])
                    op("dve", lambda e: e.tensor_scalar(out=NSEL[:], in0=cfin[:], scalar1=TOPK, scalar2=None, op0=ALU.is_lt), reads=[tBS], writes=[tBS])
                    op("dve", lambda e: e.copy_predicated(out=LO[:], mask=SELI[:], data=MID[:]), reads=[tBS], writes=[tBS])
                    op("dve", lambda e: e.copy_predicated(out=HI[:], mask=NSEL[:], data=MID[:]), reads=[tBS], writes=[tBS])
                    op("dve", lambda e: e.copy_predicated(out=CH[:], mask=NSEL[:], data=cfin[:]), reads=[tBS], writes=[tBS])
                op("dve", lambda e: e.tensor_scalar(out=MNEED[:], in0=CH[:], scalar1=-1.0, scalar2=TOPK, op0=ALU.mult, op1=ALU.add), reads=[tBS], writes=[tBS])
                for ci, (c0, w) in enumerate(chunks):
                    op("dve", lambda e: e.tensor_scalar(out=AB[0][:, 0:w], in0=SCR[:, c0:c0 + w], scalar1=LO[:, 0:1], scalar2=None, op0=ALU.is_ge),
                       reads=[tSCR, tBS], writes=[tAB[0]])
                    op("dve", lambda e: e.tensor_scalar(out=AB[1][:, 0:w], in0=SCR[:, c0:c0 + w], scalar1=HI[:, 0:1], scalar2=None, op0=ALU.is_ge),
                       reads=[tSCR, tBS], writes=[tAB[1]])
                    if ci > 0:
                        op("dve", lambda e: e.tensor_copy(out=CARRY[:], in_=PF[:, 2047:2048]), reads=[tPF, tBS], writes=[tBS])
                    op("dve", lambda e: e.tensor_tensor_scan(out=PF[:, 0:w], data0=AB[0][:, 0:w], data1=AB[1][:, 0:w],
                                                             initial=(0.0 if ci == 0 else CARRY[:, 0:1]), op0=ALU.add, op1=ALU.subtract),
                       reads=[tAB[0], tAB[1], tBS], writes=[tPF])
                    op("dve", lambda e: e.scalar_tensor_tensor(out=TM[:, 0:w], in0=PF[:, 0:w], scalar=MNEED[:, 0:1], in1=AB[1][:, 0:w],
                                                               op0=ALU.is_le, op1=ALU.max), reads=[tPF, tAB[1], tBS], writes=[tTM])
                    op("pool", lambda e: e.tensor_tensor(out=MK[:, 0:w], in0=TM[:, 0:w], in1=AB[0][:, 0:w], op=ALU.mult), reads=[tTM, tAB[0]], writes=[tMK])
                    nb_ = w // 128
                    for j0 in range(0, nb_, 8):
                        j1 = min(nb_, j0 + 8)
                        hb = (j0 // 8) % 2
                        grp("pe", [(lambda e, j=j: e.transpose(out=Hb[hb][:, 128 * (j - j0):128 * (j - j0) + 128], in_=MK[:, 128 * j:128 * j + 128], identity=IDN[:]))
                                   for j in range(j0, j1)], reads=[tMK, tC], writes=[tH[hb]])
                        kb0_ = c0 // 128 + j0
                        op("act", lambda e: e.activation(out=MT[:, kb0_:kb0_ + (j1 - j0), :],
                                                         in_=Hb[hb][:, 0:128 * (j1 - j0)].rearrange("p (a t) -> p a t", t=128), func=AF.Copy),
                           reads=[tH[hb]], writes=[tMT])
                first = [True, True]
                for k0 in range(0, nkb, 4):
                    cs = kchunk % 2
                    kchunk += 1
                    gi = k0 // 4
                    dma("sp", dKTB[cs], KTB[cs][:], KT_d[:, :, 512 * gi:512 * gi + 512], reads=[tKTd[gi]], writes=[tKTB[cs]])
                    dma("sp", dVB[cs], VB[cs][:], V_d[:, 4 * gi:4 * gi + 4, :], reads=[tVd[gi]], writes=[tVB[cs]])
                    for j in range(4):
                        kb = k0 + j
                        last_kb = (kb == nkb - 1)
                        for par in range(2):
                            bi = 2 * (kb % 2) + par
                            ps = slice(64 * par, 64 * par + 64)
                            grp("pe", [(lambda e, pr=pr: e.matmul(Fb[bi][:, 128 * pr:128 * pr + 128], lhsT=KTB[cs][ps, pr, 128 * j:128 * j + 128],
                                                                  rhs=QTB[sl][ps, pr, :], start=True, stop=True)) for pr in range(4)],
                                reads=[tKTB[cs], tQL[sl]], writes=[tF[bi]])
                            op("act", lambda e: e.activation(out=EX[bi][:], in_=Fb[bi][:, :].rearrange("p (a t) -> p a t", a=4), func=AF.Exp, scale=0.125),
                               reads=[tF[bi]], writes=[tEX[bi]])
                            op("pool", lambda e: e.tensor_tensor(out=PM[bi][:], in0=EX[bi][:], in1=MT[:, kb, :].unsqueeze(1).broadcast_to([128, 4, 128]),
                                                                 op=ALU.mult), reads=[tEX[bi], tMT], writes=[tPM[bi]])
                            fns = []
                            for pr in range(4):
                                head = 2 * pr + par
                                ob = 4 + head // 4
                                st_ = first[head // 4]
                                first[head // 4] = False
                                sp_ = last_kb and par == 1 and (head % 4 == 3)
                                fns.append(lambda e, pr=pr, head=head, ob=ob, st_=st_, sp_=sp_: e.matmul(
                                    Fb[ob][:, 65 * (head % 4):65 * (head % 4) + 65], lhsT=PM[bi][:, pr, :],
                                    rhs=VB[cs][:, j, 65 * head:65 * head + 65], start=st_, stop=sp_, skip_group_check=True))
                            grp("pe", fns, reads=[tPM[bi], tVB[cs]], writes=[tF[4], tF[5]])
                for ob in range(2):
                    ov = Fb[4 + ob][:, 0:260].rearrange("p (h e) -> p h e", e=65)
                    op("dve", lambda e, ov=ov, ob=ob: e.reciprocal(out=RC[:, 4 * ob:4 * ob + 4], in_=ov[:, :, 64]), reads=[tF[4 + ob]], writes=[tRC])
                    op("dve", lambda e, ov=ov, ob=ob: e.tensor_tensor(out=ATo[:, 256 * ob:256 * ob + 256].rearrange("p (h d) -> p h d", d=64),
                                                                      in0=ov[:, :, 0:64], in1=RC[:, 4 * ob:4 * ob + 4].unsqueeze(2).broadcast_to([128, 4, 64]),
                                                                      op=ALU.mult), reads=[tF[4 + ob], tRC], writes=[tATo])
                grp("pe", [(lambda e, i=i: e.transpose(out=Hb[0][:, 128 * i:128 * i + 128], in_=ATo[:, 128 * i:128 * i + 128], identity=IDN[:]))
                           for i in range(4)], reads=[tATo, tC], writes=[tH[0]])
                op("act", lambda e: e.activation(out=ATS[sl][:], in_=Hb[0][:, 0:512].rearrange("p (a t) -> p a t", a=4), func=AF.Copy),
                   reads=[tH[0]], writes=[tATS[sl]])
                dma("sp", dATS[sl], ATT_d[:, :, 128 * m:128 * m + 128], ATS[sl][:], reads=[tATS[sl]], writes=[tATTd[m]])
            p2_bufs = [tIKT, tSCR, tKB0, tMX, tMN, tBS, tJUNK, tPF, tTM, tMK, tMT, tRC, tATo] + tQL + tRB + tAB + tKTB + tVB + tEX + tPM + tATS
            for q in ("pe", "act", "dve", "pool", "sp"):
                sc.finish(q, p2_bufs)

        with ExitStack() as p3:
            WO = sb(p3, "WO", [128, 8, D], BF16)
            WU = sb(p3, "WU", [128, 8, DFF], BF16)
            WD = sb(p3, "WD", [128, 32, D], BF16)
            tWO, tWU, tWD = T(), T(), T()
            dW3 = [sc.dsem("dW3_%d" % i) for i in range(3)]
            wo_v = wo_d.rearrange("(c p) n -> p c n", p=128)
            wu_v = wup_d.rearrange("(c p) n -> p c n", p=128)
            wd_v = wdn_d.rearrange("(c p) n -> p c n", p=128)
            for c in range(8):
                dma("pool", dW3[0], WO[:, c, :], wo_v[:, c, :], writes=[tWO])
            for c in range(8):
                dma("pool", dW3[1], WU[:, c, :], wu_v[:, c, :], writes=[tWU])
            for c in range(32):
                dma("pool", dW3[2], WD[:, c, :], wd_v[:, c, :], writes=[tWD])
            LNP = [sb(p3, "LNP%d" % i, [128, D], F32) for i in range(4)]
            tLNP = T()
            dLNP = [sc.dsem("dLNP%d" % i) for i in range(4)]
            for i in range(4):
                dma("sp", dLNP[i], LNP[i][:], lnp_ds[i][0:1, :].partition_broadcast(128), writes=[tLNP])
            ATB = [sb(p3, "ATB%d" % i, [128, 4, 128], BF16) for i in range(2)]
            HGB = [sb(p3, "HGB%d" % i, [128, 4, 128], BF16) for i in range(2)]
            XO = [sb(p3, "XO%d" % i, [128, D], F32) for i in range(2)]
            tL3 = [T(), T()]
            dL3 = [sc.dsem("dL3_0"), sc.dsem("dL3_1")]
            Y = sb(p3, "Y", [128, D], F32); tY = T()
            STT = sb(p3, "STT", [128, 2, 6], F32); MV = sb(p3, "MV", [128, 2], F32); RSD = sb(p3, "RSD", [128, 1], F32); tLN = T()
            X1 = sb(p3, "X1", [128, D], F32); tX1 = T()
            X1B = sb(p3, "X1B", [128, D], BF16); tX1B = T()
            X1T = sb(p3, "X1T", [128, 8, 128], BF16); tX1T = T()
            RT = [sb(p3, "RT%d" % i, [128, 128], F32) for i in range(2)]; tRT = [T(), T()]
            HT = sb(p3, "HT", [128, 32, 128], BF16); tHT = T()
            OB = [sb(p3, "OB%d" % i, [128, D], F32) for i in range(2)]; tOB = [T(), T()]
            dOB = [sc.dsem("dOB0"), sc.dsem("dOB1")]

            def layer_norm(src, tsrc, gi, dst, tdst):
                for hh in range(2):
                    op("dve", lambda e, hh=hh: e.bn_stats(out=STT[:, hh, :], in_=src[:, 512 * hh:512 * hh + 512]), reads=[tsrc], writes=[tLN])
                op("dve", lambda e: e.bn_aggr(out=MV[:], in_=STT[:].rearrange("p a b -> p (a b)")), reads=[tLN], writes=[tLN])
                op("dve", lambda e: e.tensor_scalar(out=RSD[:], in0=MV[:, 1:2], scalar1=LN_EPS, scalar2=None, op0=ALU.add), reads=[tLN], writes=[tLN])
                op("act", lambda e: e.activation(out=RSD[:], in_=RSD[:], func=AF.Ln), reads=[tLN], writes=[tLN])
                op("act", lambda e: e.activation(out=RSD[:], in_=RSD[:], func=AF.Exp, scale=-0.5), reads=[tLN], writes=[tLN])
                op("dve", lambda e: e.tensor_scalar(out=dst[:], in0=src[:], scalar1=MV[:, 0:1], scalar2=RSD[:, 0:1], op0=ALU.subtract, op1=ALU.mult),
                   reads=[tsrc, tLN], writes=[tdst])
                op("pool", lambda e: e.tensor_tensor(out=dst[:], in0=dst[:], in1=LNP[gi][:], op=ALU.mult), reads=[tdst, tLNP], writes=[tdst])
                op("pool", lambda e: e.tensor_tensor(out=dst[:], in0=dst[:], in1=LNP[gi + 1][:], op=ALU.add), reads=[tdst, tLNP], writes=[tdst])

            for m in range(NOWN):
                sl = m % 2
                dma("sp", dL3[sl], ATB[sl][:], ATT_d[:, :, 128 * m:128 * m + 128], reads=[tATTd[m]], writes=[tL3[sl]])
                dma("sp", dL3[sl], HGB[sl][:], HGT_d[:, :, 128 * m:128 * m + 128], reads=[tHGTd[m]], writes=[tL3[sl]])
                dma("sp", dL3[sl], XO[sl][:], xo_d[128 * m:128 * m + 128, :], writes=[tL3[sl]])
                for hh in range(2):
                    grp("pe", [(lambda e, c=c: e.matmul(Fb[hh][:, :], lhsT=(ATB[sl][:, c, :] if c < 4 else HGB[sl][:, c - 4, :]),
                                                        rhs=WO[:, c, 512 * hh:512 * hh + 512], start=(c == 0), stop=(c == 7))) for c in range(8)],
                        reads=[tL3[sl], tWO], writes=[tF[hh]])
                    op("dve", lambda e, hh=hh: e.scalar_tensor_tensor(out=Y[:, 512 * hh:512 * hh + 512], in0=XO[sl][:, 512 * hh:512 * hh + 512], scalar=ALPHA,
                                                                      in1=Fb[hh][:, :], op0=ALU.mult, op1=ALU.add), reads=[tL3[sl], tF[hh]], writes=[tY])
                layer_norm(Y, tY, 0, X1, tX1)
                op("act", lambda e: e.activation(out=X1B[:], in_=X1[:], func=AF.Copy), reads=[tX1], writes=[tX1B])
                grp("pe", [(lambda e, i=i: e.transpose(out=Hb[0][:, 128 * i:128 * i + 128], in_=X1B[:, 128 * i:128 * i + 128], identity=IDN[:]))
                           for i in range(8)], reads=[tX1B, tC], writes=[tH[0]])
                op("act", lambda e: e.activation(out=X1T[:], in_=Hb[0][:, :].rearrange("p (a t) -> p a t", a=8), func=AF.Copy), reads=[tH[0]], writes=[tX1T])
                for fc in range(32):
                    bi = 2 + fc % 4
                    grp("pe", [(lambda e, c=c: e.matmul(Fb[bi][:, 0:128], lhsT=WU[:, c, 128 * fc:128 * fc + 128], rhs=X1T[:, c, :],
                                                        start=(c == 0), stop=(c == 7))) for c in range(8)], reads=[tWU, tX1T], writes=[tF[bi]])
                    op("act", lambda e: e.activation(out=RT[fc % 2][:], in_=Fb[bi][:, 0:128], func=AF.Relu), reads=[tF[bi]], writes=[tRT[fc % 2]])
                    op("pool", lambda e: e.tensor_tensor(out=HT[:, fc, :], in0=RT[fc % 2][:], in1=RT[fc % 2][:], op=ALU.mult), reads=[tRT[fc % 2]], writes=[tHT])
                for hh in range(2):
                    grp("pe", [(lambda e, fc=fc: e.matmul(Fb[hh][:, :], lhsT=HT[:, fc, :], rhs=WD[:, fc, 512 * hh:512 * hh + 512],
                                                          start=(fc == 0), stop=(fc == 31))) for fc in range(32)], reads=[tHT, tWD], writes=[tF[hh]])
                    op("dve", lambda e, hh=hh: e.scalar_tensor_tensor(out=Y[:, 512 * hh:512 * hh + 512], in0=X1[:, 512 * hh:512 * hh + 512], scalar=ALPHA,
                                                                      in1=Fb[hh][:, :], op0=ALU.mult, op1=ALU.add), reads=[tX1, tF[hh]], writes=[tY])
                layer_norm(Y, tY, 2, OB[sl], tOB[sl])
                dma("sp", dOB[sl], y_d[128 * m:128 * m + 128, :], OB[sl][:], reads=[tOB[sl]])
            p3_bufs = [tWO, tWU, tWD, tLNP, tY, tLN, tX1, tX1B, tX1T, tHT] + tL3 + tRT + tOB
            for q in ("pe", "act", "dve", "pool", "sp"):
                sc.finish(q, p3_bufs)
        for q in ("pe", "act", "dve", "pool", "sp"):
            sc.finish(q, [tC] + tF + tH + [tF2s])
    return nc


def make_inputs(x, w_in, w_o, lb_logits, hg_norm_g, ln1_g, ln1_b, w_up, w_down, ln2_g, ln2_b, S=SEQ):
    NB = S // 128
    NOWN = NB // 4
    x = np.asarray(x, np.float32)
    B = x.shape[0]
    in_maps = []
    shared = {
        "w_in": np.ascontiguousarray(np.asarray(w_in, np.float32)[0]),
        "w_o": np.ascontiguousarray(np.asarray(w_o, np.float32)[0]),
        "w_up": np.ascontiguousarray(np.asarray(w_up, np.float32)[0]),
        "w_down": np.ascontiguousarray(np.asarray(w_down, np.float32)[0]),
        "lb0": np.ascontiguousarray(np.asarray(lb_logits, np.float32).reshape(2, 512)[0:1]),
        "lb1": np.ascontiguousarray(np.asarray(lb_logits, np.float32).reshape(2, 512)[1:2]),
        "hgn": np.ascontiguousarray(np.asarray(hg_norm_g, np.float32).reshape(1, 512)),
        "lnp0": np.ascontiguousarray(np.asarray(ln1_g, np.float32).reshape(1, D)),
        "lnp1": np.ascontiguousarray(np.asarray(ln1_b, np.float32).reshape(1, D)),
        "lnp2": np.ascontiguousarray(np.asarray(ln2_g, np.float32).reshape(1, D)),
        "lnp3": np.ascontiguousarray(np.asarray(ln2_b, np.float32).reshape(1, D)),
    }
    for c in range(4 * B):
        b, j = divmod(c, 4)
        npad = 3 - j
        nreal = NB - npad
        xp = np.zeros((S, D), np.float32)
        xp[128 * npad:] = x[b, :128 * nreal]
        own_blocks = [4 * m + j for m in range(NOWN)]
        xo = np.concatenate([x[b, 128 * r:128 * r + 128] for r in own_blocks], 0)
        pidx = np.arange(S, dtype=np.float32).reshape(NB, 128).T - 128.0 * npad
        pos = np.maximum(pidx, 0.0).astype(np.float32)
        kb0 = np.where(pidx.T.reshape(-1)[:512] < 0, NEG, 0.0).astype(np.float32).reshape(1, 512)
        d = dict(shared)
        d.update({"xT": np.ascontiguousarray(xp.T), "xo": np.ascontiguousarray(xo), "pos": np.ascontiguousarray(pos), "kb0": kb0})
        in_maps.append(d)
    return in_maps


def assemble(results, B, S=SEQ):
    NB = S // 128
    NOWN = NB // 4
    out = np.zeros((B, S, D), np.float32)
    for c in range(4 * B):
        b, j = divmod(c, 4)
        y = results[c]["y"]
        for m in range(NOWN):
            r = 4 * m + j
            out[b, 128 * r:128 * r + 128] = y[128 * m:128 * m + 128]
    return out


def kernel(x, w_in, w_o, lb_logits, hg_norm_g, ln1_g, ln1_b, w_up, w_down, ln2_g, ln2_b):
    x = np.asarray(x)
    B, S, _ = x.shape
    nc = build_nc(S)
    in_maps = make_inputs(x, w_in, w_o, lb_logits, hg_norm_g, ln1_g, ln1_b, w_up, w_down, ln2_g, ln2_b, S)
    res = run_bass_kernel_spmd(nc, in_maps, core_ids=list(range(4 * B)))
    return assemble(res.results, B, S)
```

```python
import math
from contextlib import ExitStack
import numpy as np
import concourse.bass as bass
import concourse.mybir as mybir
from concourse.bass_utils import run_bass_kernel_spmd

F32 = mybir.dt.float32
BF16 = mybir.dt.bfloat16
I32 = mybir.dt.int32
AF = mybir.ActivationFunctionType
ALU = mybir.AluOpType
AX = mybir.AxisListType

D = 1024
SEQ = 16384
NCOL = 3908
DFF = 4096
ALPHA = 2.0 ** 0.25
LN_EPS = 1e-5
RMS_EPS = 1e-6
IDX_SCALE = (4 ** -0.5) * (64 ** -0.5)
TOPK = 256.0
NEG = -1.0e30
NIT = 16

C_Q, C_K, C_V, C_IQ, C_IK, C_IW, C_HQ, C_HF, C_HI, C_HG = 0, 512, 1024, 1536, 1792, 1856, 1860, 2372, 2884, 3396


class T:
    __slots__ = ("w", "r", "name", "excl")

    def __init__(self, name="", excl=False):
        self.w = None
        self.r = {}
        self.name = name
        self.excl = excl


class DSem:
    def __init__(self, h):
        self.h = h
        self.count = 0


class Sched:
    def __init__(self, nc, es):
        self.nc = nc
        self.es = es
        self.eng = {"pe": nc.tensor, "act": nc.scalar, "dve": nc.vector, "pool": nc.gpsimd, "sp": nc.sync}
        self.sem = {k: es.enter_context(nc.semaphore("s_" + k)) for k in ("pe", "act", "dve", "pool")}
        self.cnt = {k: 0 for k in self.sem}
        self.seen = {k: {} for k in self.eng}
        self.n_ins = 0

    def dsem(self, name):
        return DSem(self.es.enter_context(self.nc.semaphore(name)))

    def _deps(self, reads, writes, eng=None):
        deps = {}
        for b in reads:
            if b.w is not None:
                k, v = b.w
                if deps.get(k, 0) < v:
                    deps[k] = v
            if b.excl:
                for k, v in b.r.items():
                    if k != eng and deps.get(k, 0) < v:
                        deps[k] = v
        for b in writes:
            if b.w is not None:
                k, v = b.w
                if deps.get(k, 0) < v:
                    deps[k] = v
            for k, v in b.r.items():
                if deps.get(k, 0) < v:
                    deps[k] = v
        return deps

    def _wait(self, eng, deps):
        seen = self.seen[eng]
        e = self.eng[eng]
        for k, v in deps.items():
            if seen.get(k, 0) >= v:
                continue
            if k == "pe" and eng == "pe":
                continue
            h = self.sem[k] if isinstance(k, str) else k.h
            e.wait_ge(h, v)
            seen[k] = v
            self.n_ins += 1

    def op(self, eng, fn, reads=(), writes=()):
        self._wait(eng, self._deps(reads, writes, eng))
        ins = fn(self.eng[eng])
        self.cnt[eng] += 1
        c = self.cnt[eng]
        ins.then_inc(self.sem[eng], 1)
        self.n_ins += 1
        for b in reads:
            b.r[eng] = c
        for b in writes:
            b.w = (eng, c)
            b.r = {}
        return ins

    def group(self, eng, fns, reads=(), writes=()):
        self._wait(eng, self._deps(reads, writes, eng))
        e = self.eng[eng]
        ins = None
        for fn in fns:
            ins = fn(e)
            self.n_ins += 1
        self.cnt[eng] += 1
        c = self.cnt[eng]
        ins.then_inc(self.sem[eng], 1)
        for b in reads:
            b.r[eng] = c
        for b in writes:
            b.w = (eng, c)
            b.r = {}

    def dma(self, q, ds, out, in_, reads=(), writes=()):
        deps = self._deps(reads, writes)
        deps.pop(ds, None)
        self._wait(q, deps)
        ins = self.eng[q].dma_start(out=out, in_=in_)
        ds.count += 16
        ins.then_inc(ds.h, 16)
        self.n_ins += 1
        for b in reads:
            b.r[ds] = ds.count
        for b in writes:
            b.w = (ds, ds.count)
            b.r = {}

    def finish(self, q, bufs):
        self._wait(q, self._deps(bufs, bufs))


def build_nc(S=SEQ, dbg=False):
    NB = S // 128
    NOWN = NB // 4
    SO = NOWN * 128
    NG = NB // 4
    nc = bass.Bass("TRN2", target_bir_lowering=False)
    dram = lambda n, s, d, k: nc.dram_tensor(n, s, d, kind=k).ap()
    xT_d = dram("xT", [D, S], F32, "ExternalInput")
    xo_d = dram("xo", [SO, D], F32, "ExternalInput")
    pos_d = dram("pos", [128, NB], F32, "ExternalInput")
    kb0_d = dram("kb0", [1, 512], F32, "ExternalInput")
    win_d = dram("w_in", [D, NCOL], F32, "ExternalInput")
    wo_d = dram("w_o", [D, D], F32, "ExternalInput")
    wup_d = dram("w_up", [D, DFF], F32, "ExternalInput")
    wdn_d = dram("w_down", [DFF, D], F32, "ExternalInput")
    lb0_d = dram("lb0", [1, 512], F32, "ExternalInput")
    lb1_d = dram("lb1", [1, 512], F32, "ExternalInput")
    hgn_d = dram("hgn", [1, 512], F32, "ExternalInput")
    lnp_ds = [dram("lnp%d" % i, [1, D], F32, "ExternalInput") for i in range(4)]
    y_d = dram("y", [SO, D], F32, "ExternalOutput")
    SK = "ExternalOutput" if dbg else "Internal"
    KT_d = dram("KT_s", [128, 4, S], BF16, SK)
    V_d = dram("V_s", [128, NB, 520], BF16, SK)
    IKT_d = dram("IKT_s", [128, S], BF16, SK)
    QT_d = dram("QT_s", [128, 4, SO], BF16, SK)
    IQT_d = dram("IQT_s", [128, NOWN, 256], BF16, SK)
    SEL_d = dram("SEL_s", [128, NOWN, 512], BF16, SK)
    HGT_d = dram("HGT_s", [128, 4, SO], BF16, SK)
    ATT_d = dram("ATT_s", [128, 4, SO], BF16, SK)
    dbg_outs = {}

    with ExitStack() as es:
        sc = Sched(nc, es)
        op, grp, dma = sc.op, sc.group, sc.dma

        def sb(stack, name, shape, dt):
            return stack.enter_context(nc.sbuf_tensor(name, shape, dt))

        Fb = [es.enter_context(nc.psum_tensor("F%d" % i, [128, 512], F32)) for i in range(6)]
        Hb = [es.enter_context(nc.psum_tensor("H%d" % i, [128, 1024], BF16)) for i in range(2)]
        tF = [T("F%d" % i, excl=True) for i in range(6)]
        tH = [T("H%d" % i, excl=True) for i in range(2)]
        tF2s = tF[2]

        IDN = sb(es, "IDN", [128, 128], BF16)
        tC = T("consts")
        op("pool", lambda e: e.memset(IDN[:], 1.0), writes=[tC])
        op("pool", lambda e: e.affine_select(out=IDN[:], in_=IDN[:], pattern=[[-1, 128]], compare_op=ALU.is_equal,
                                             fill=0.0, base=0, channel_multiplier=1), writes=[tC])
        TRIB = sb(es, "TRIB", [128, 128], F32)
        op("pool", lambda e: e.memset(TRIB[:], 0.0), writes=[tC])
        op("pool", lambda e: e.affine_select(out=TRIB[:], in_=TRIB[:], pattern=[[-1, 128]], compare_op=ALU.is_ge,
                                             fill=NEG, base=0, channel_multiplier=1), writes=[tC])

        fin_list = []

        with ExitStack() as p1:
            Wb = sb(p1, "Wb", [128, 8, NCOL], BF16)
            tW = T("Wb")
            dW = sc.dsem("dW")
            win_v = win_d.rearrange("(c p) n -> p c n", p=128)
            for c in range(8):
                dma("pool", dW, Wb[:, c, :], win_v[:, c, :], writes=[tW])
            XG = [sb(p1, "XG%d" % i, [128, 8, 512], BF16) for i in range(2)]
            tXG = [T("XG0"), T("XG1")]
            dXG = [sc.dsem("dXG0"), sc.dsem("dXG1")]
            xT_v = xT_d.rearrange("(c p) t -> p c t", p=128)
            POS = sb(p1, "POS", [128, NB], F32)
            dMisc = sc.dsem("dMisc")
            dPOS = sc.dsem("dPOS")
            tPOS = T("POS")
            dma("sp", dPOS, POS[:], pos_d[:, :], writes=[tPOS])
            COS = sb(p1, "COS", [128, NB, 32], F32)
            SIN = sb(p1, "SIN", [128, NB, 32], F32)
            tTab = T("tab")
            with ExitStack() as pt:
                INV = sb(pt, "INV", [128, 32], F32)
                ANG = sb(pt, "ANG", [128, NB, 32], F32)
                KQ = sb(pt, "KQ", [128, NB, 32], F32)
                RR = sb(pt, "RR", [128, NB, 32], F32)
                tI, tA, tK, tR = T(), T(), T(), T()
                for dd_ in range(32):
                    op("pool", lambda e, dd_=dd_: e.memset(INV[:, dd_:dd_ + 1], float(np.float32(10000.0 ** (-dd_ / 32.0)))), writes=[tI])
                op("dve", lambda e: e.tensor_tensor(out=ANG[:], in0=POS[:].unsqueeze(2).broadcast_to([128, NB, 32]),
                                                    in1=INV[:].unsqueeze(1).broadcast_to([128, NB, 32]), op=ALU.mult),
                   reads=[tPOS, tI], writes=[tA])
                TWO_PI = 2.0 * math.pi
                C1 = 6.28125
                C2 = TWO_PI - C1
                MAGIC = 12582912.0
                for which, off, TAB in (("sin", 0.0, SIN), ("cos", 0.25, COS)):
                    op("dve", lambda e: e.tensor_scalar(out=KQ[:], in0=ANG[:], scalar1=1.0 / TWO_PI, scalar2=off,
                                                        op0=ALU.mult, op1=ALU.add), reads=[tA], writes=[tK])
                    op("dve", lambda e: e.tensor_scalar(out=KQ[:], in0=KQ[:], scalar1=MAGIC, scalar2=None, op0=ALU.add),
                       reads=[tK], writes=[tK])
                    op("dve", lambda e: e.tensor_scalar(out=KQ[:], in0=KQ[:], scalar1=-MAGIC, scalar2=None, op0=ALU.add),
                       reads=[tK], writes=[tK])
                    op("dve", lambda e: e.scalar_tensor_tensor(out=RR[:], in0=KQ[:], scalar=-C1, in1=ANG[:],
                                                               op0=ALU.mult, op1=ALU.add), reads=[tK, tA], writes=[tR])
                    op("dve", lambda e: e.scalar_tensor_tensor(out=RR[:], in0=KQ[:], scalar=-C2, in1=RR[:],
                                                               op0=ALU.mult, op1=ALU.add), reads=[tK, tR], writes=[tR])
                    if which == "cos":
                        op("dve", lambda e: e.tensor_scalar(out=RR[:], in0=RR[:], scalar1=math.pi / 2.0, scalar2=None,
                                                            op0=ALU.add), reads=[tR], writes=[tR])
                    op("dve", lambda e: e.tensor_scalar(out=RR[:], in0=RR[:], scalar1=3.14159, scalar2=-3.14159,
                                                        op0=ALU.min, op1=ALU.max), reads=[tR], writes=[tR])
                    op("act", lambda e, TAB=TAB: e.activation(out=TAB[:], in_=RR[:], func=AF.Sin), reads=[tR], writes=[tTab])
                for q in ("pe", "act", "dve", "pool", "sp"):
                    sc.finish(q, [tI, tA, tK, tR, tTab, tPOS])

            LB = sb(p1, "LB", [128, 512], F32)
            OML = sb(p1, "OML", [128, 512], F32)
            NGB = sb(p1, "NGB", [128, 512], F32)
            L1 = sb(p1, "L1", [128, 512], F32)
            tLB = T("LB")
            dma("sp", dMisc, LB[:], lb0_d[0:1, :].partition_broadcast(128), writes=[tLB])
            tL1 = T("L1")
            dL1 = sc.dsem("dL1")
            dma("sp", dL1, L1[:], lb1_d[0:1, :].partition_broadcast(128), writes=[tL1])
            tNG = T("NGB")
            dNG = sc.dsem("dNG")
            dma("sp", dNG, NGB[:], hgn_d[0:1, :].partition_broadcast(128), writes=[tNG])
            op("dve", lambda e: e.tensor_tensor(out=L1[:], in0=L1[:], in1=LB[:], op=ALU.subtract), reads=[tL1, tLB], writes=[tL1])
            op("act", lambda e: e.activation(out=L1[:], in_=L1[:], func=AF.Exp), reads=[tL1], writes=[tL1])
            op("dve", lambda e: e.tensor_scalar(out=L1[:], in0=L1[:], scalar1=1.0, scalar2=None, op0=ALU.add), reads=[tL1], writes=[tL1])
            op("dve", lambda e: e.reciprocal(out=LB[:], in_=L1[:]), reads=[tL1], writes=[tLB])
            op("dve", lambda e: e.tensor_scalar(out=OML[:], in0=LB[:], scalar1=-1.0, scalar2=1.0, op0=ALU.mult, op1=ALU.add),
               reads=[tLB], writes=[tLB])
            LT_U128 = sb(p1, "LT_U128", [128, 128], F32)
            LT_I64 = sb(p1, "LT_I64", [128, 128], F32)
            LT_U64 = sb(p1, "LT_U64", [128, 128], F32)
            IND2 = sb(p1, "IND2", [128, 2], F32)
            MASKBD = sb(p1, "MASKBD", [128, 128], F32)
            op("pool", lambda e: e.memset(LT_U128[:], 1.0), writes=[tC])
            op("pool", lambda e: e.affine_select(out=LT_U128[:], in_=LT_U128[:], pattern=[[-1, 128]], compare_op=ALU.is_gt,
                                                 fill=0.0, base=0, channel_multiplier=1), writes=[tC])
            op("pool", lambda e: e.memset(LT_I64[:], 0.0), writes=[tC])
            op("pool", lambda e: e.memset(LT_U64[:], 0.0), writes=[tC])
            for cblk in range(2):
                sl = slice(64 * cblk, 64 * cblk + 64)
                op("pool", lambda e, sl=sl: e.memset(LT_I64[sl, sl], 1.0), writes=[tC])
                op("pool", lambda e, sl=sl: e.affine_select(out=LT_I64[sl, sl], in_=LT_I64[sl, sl], pattern=[[1, 64]],
                                                            compare_op=ALU.is_ge, fill=0.0, base=0, channel_multiplier=-1), writes=[tC])
                op("pool", lambda e, sl=sl: e.memset(LT_U64[sl, sl], 1.0), writes=[tC])
                op("pool", lambda e, sl=sl: e.affine_select(out=LT_U64[sl, sl], in_=LT_U64[sl, sl], pattern=[[-1, 64]],
                                                            compare_op=ALU.is_gt, fill=0.0, base=0, channel_multiplier=1), writes=[tC])
            op("pool", lambda e: e.tensor_copy(out=MASKBD[:], in_=LT_I64[:]), writes=[tC])
            op("pool", lambda e: e.memset(IND2[:], 1.0), writes=[tC])
            op("pool", lambda e: e.memset(IND2[64:128, 0:1], 0.0), writes=[tC])
            E_u = [sb(p1, "E_u%d" % u, [128, 128], BF16) for u in range(2)]
            ETA = [sb(p1, "ETA%d" % u, [128, 128], BF16) for u in range(2)]
            ETB = [sb(p1, "ETB%d" % u, [128, 128], BF16) for u in range(2)]
            for u in range(2):
                op("pool", lambda e, u=u: e.memset(E_u[u][:], 1.0), writes=[tC])
                for hf_ in range(2):
                    ps = slice(64 * hf_, 64 * hf_ + 64)
                    op("pool", lambda e, u=u, ps=ps: e.affine_select(out=E_u[u][ps, :], in_=E_u[u][ps, :], pattern=[[1, 128]],
                                                                      compare_op=ALU.is_equal, fill=0.0, base=-64 * u,
                                                                      channel_multiplier=-1), writes=[tC])
                op("pool", lambda e, u=u: e.memset(ETA[u][:], 0.0), writes=[tC])
                op("pool", lambda e, u=u: e.memset(ETB[u][:], 0.0), writes=[tC])
                op("pool", lambda e, u=u: e.memset(ETA[u][:, 0:64], 1.0), writes=[tC])
                op("pool", lambda e, u=u: e.memset(ETB[u][:, 64:128], 1.0), writes=[tC])
                op("pool", lambda e, u=u: e.affine_select(out=ETA[u][:, 0:64], in_=ETA[u][:, 0:64], pattern=[[1, 64]],
                                                          compare_op=ALU.is_equal, fill=0.0, base=64 * u, channel_multiplier=-1), writes=[tC])
                op("pool", lambda e, u=u: e.affine_select(out=ETB[u][:, 64:128], in_=ETB[u][:, 64:128], pattern=[[1, 64]],
                                                          compare_op=ALU.is_equal, fill=0.0, base=64 * u, channel_multiplier=-1), writes=[tC])

            KTS = [sb(p1, "KTS%d" % i, [128, 4, 512], BF16) for i in range(2)]
            VS = [sb(p1, "VS%d" % i, [128, 4, 520], BF16) for i in range(2)]
            IKS = [sb(p1, "IKS%d" % i, [128, 512], BF16) for i in range(2)]
            tKTS = [T(), T()]; tVS = [T(), T()]; tIKS = [T(), T()]
            dKTS = [sc.dsem("dKTS0"), sc.dsem("dKTS1")]
            dVS = [sc.dsem("dVS0"), sc.dsem("dVS1")]
            dIKS = [sc.dsem("dIKS0"), sc.dsem("dIKS1")]
            for i in range(2):
                op("pool", lambda e, i=i: e.memset(VS[i][:], 1.0), writes=[tVS[i]])
            ST_ = sb(p1, "STATE", [128, 4, 128], F32)
            tST = T("state")
            op("pool", lambda e: e.memset(ST_[:], 0.0), writes=[tST])

            def tmp(name, shape, dt):
                return sb(p1, name, shape, dt), T(name)
            KR, tKR = tmp("KR", [128, 512], BF16)
            TA, tTA = tmp("TA", [128, 512], F32)
            TB_, tTB = tmp("TB", [128, 256], F32)
            TC_, tTC = tmp("TC", [128, 256], F32)
            XR, tXR = tmp("XR", [128, 320], F32)
            IKR, tIKR = tmp("IKR", [128, 128], BF16)
            EZ, tEZ = tmp("EZ", [128, 512], F32)
            FG, tFG = tmp("FG", [128, 512], F32)
            LF, tLF = tmp("LF", [128, 512], F32)
            KK, tKK = tmp("KK", [128, 512], F32)
            DP, tDP = tmp("DP", [128, 512], F32)
            DK, tDK = tmp("DK", [128, 512], BF16)
            VH, tVH = tmp("VH", [128, 512], BF16)
            DCOL, tDCOL = tmp("DCOL", [128, 8], F32)
            SB16, tSB16 = tmp("SB16", [128, 4, 128], BF16)
            AW, tAW = tmp("AW", [128, 4], F32)
            SGN, tSGN = tmp("SGN", [128, 4], BF16)
            IQS, tIQS = tmp("IQS", [128, 256], BF16)
            AQ, tAQ = tmp("AQ", [128, 512], F32)
            BN, tBN = tmp("BN", [128, 512], F32)
            D64, tD64 = tmp("D64", [128, 512], F32)
            QA, tQA = tmp("QA", [128, 512], BF16)
            KBm, tKBm = tmp("KBm", [128, 512], BF16)
            KD, tKD = tmp("KD", [128, 512], BF16)
            QAT, tQAT = tmp("QAT", [128, 4, 128], BF16)
            QAI, tQAI = tmp("QAI", [128, 4, 128], BF16)
            KBT, tKBT = tmp("KBT", [128, 4, 128], BF16)
            KDT, tKDT = tmp("KDT", [128, 4, 64], BF16)
            AT, tAT = tmp("AT", [128, 4, 128], BF16)
            MS, tMS = tmp("MS", [128, 4], F32)
            RS, tRS = tmp("RS", [128, 4], F32)
            JK, tJK = tmp("JK", [128, 128], F32)
            GS, tGS = tmp("GS", [128, 512], F32)
            OG, tOG = tmp("OG", [128, 512], F32)
            OGB, tOGB = tmp("OGB", [128, 512], BF16)
            QTS = [sb(p1, "QTS%d" % i, [128, 4, 128], BF16) for i in range(2)]
            IQTS = [sb(p1, "IQTS%d" % i, [128, 2, 2, 64], BF16) for i in range(2)]
            SELS = [sb(p1, "SELS%d" % i, [128, 4, 128], BF16) for i in range(2)]
            HGTS = [sb(p1, "HGTS%d" % i, [128, 4, 128], BF16) for i in range(2)]
            tQTS = [T(), T()]; tIQTS = [T(), T()]; tSELS = [T(), T()]; tHGTS = [T(), T()]
            dQTS = [sc.dsem("dQTS0"), sc.dsem("dQTS1")]
            dIQTS = [sc.dsem("dIQTS0"), sc.dsem("dIQTS1")]
            dSELS = [sc.dsem("dSELS0"), sc.dsem("dSELS1")]
            dHGTS = [sc.dsem("dHGTS0"), sc.dsem("dHGTS1")]
            tKTd = [T() for _ in range(NG)]
            tVd = [T() for _ in range(NG)]
            tIKTd = [T() for _ in range(NG)]
            tQTd = [T() for _ in range(NOWN)]
            tIQTd = [T() for _ in range(NOWN)]
            tSELd = [T() for _ in range(NOWN)]
            tHGTd = [T() for _ in range(NOWN)]
            tATTd = [T() for _ in range(NOWN)]

            def proj(bank, tb, xs, r, c0, w, extra_reads=()):
                grp("pe", [(lambda e, c=c: e.matmul(bank[:, 0:w], lhsT=xs[:, c, 128 * r:128 * r + 128], rhs=Wb[:, c, c0:c0 + w],
                                                     start=(c == 0), stop=(c == 7))) for c in range(8)],
                    reads=[tW] + list(extra_reads), writes=[tb])

            def rope(src, tsrc, nh, n, dst, tdst):
                cosb = COS[:, n, :].unsqueeze(1).broadcast_to([128, 2 * nh, 32])
                sinb = SIN[:, n, :].unsqueeze(1).broadcast_to([128, nh, 32])
                s4 = src.rearrange("p (h two d) -> p h two d", two=2, d=32)
                d4 = dst.rearrange("p (h two d) -> p h two d", two=2, d=32)
                ta = TA[:, 0:nh * 64]
                tb = TB_[:, 0:nh * 32].rearrange("p (h d) -> p h d", d=32)
                tc_ = TC_[:, 0:nh * 32].rearrange("p (h d) -> p h d", d=32)
                ta4 = ta.rearrange("p (h two d) -> p h two d", two=2, d=32)
                op("dve", lambda e: e.tensor_tensor(out=ta.rearrange("p (g d) -> p g d", d=32),
                                                    in0=src.rearrange("p (g d) -> p g d", d=32), in1=cosb, op=ALU.mult),
                   reads=[tsrc, tTab], writes=[tTA])
                op("dve", lambda e: e.tensor_tensor(out=tb, in0=s4[:, :, 1, :], in1=sinb, op=ALU.mult), reads=[tsrc, tTab], writes=[tTB])
                op("dve", lambda e: e.tensor_tensor(out=tc_, in0=s4[:, :, 0, :], in1=sinb, op=ALU.mult), reads=[tsrc, tTab], writes=[tTC])
                op("dve", lambda e: e.tensor_tensor(out=d4[:, :, 0, :], in0=ta4[:, :, 0, :], in1=tb, op=ALU.subtract),
                   reads=[tTA, tTB], writes=[tdst])
                op("dve", lambda e: e.tensor_tensor(out=d4[:, :, 1, :], in0=ta4[:, :, 1, :], in1=tc_, op=ALU.add),
                   reads=[tTA, tTC], writes=[tdst])

            def transposes(src, tsrc, nblk, hb, out_cols=128, in_rows=slice(0, 128)):
                grp("pe", [(lambda e, i=i: e.transpose(out=Hb[hb][:, i * out_cols:(i + 1) * out_cols],
                                                       in_=src[in_rows, 128 * i:128 * i + 128], identity=IDN[in_rows, in_rows]))
                           for i in range(nblk)], reads=[tsrc, tC], writes=[tH[hb]])

            for n in range(NB):
                g, r = divmod(n, 4)
                own = (r == 3)
                m = g
                xs = XG[g % 2]
                if r == 0:
                    dma("pool", dXG[g % 2], xs[:], xT_v[:, :, 512 * g:512 * g + 512], writes=[tXG[g % 2]])
                txs = tXG[g % 2]
                proj(Fb[0], tF[0], xs, r, C_K, 512, [txs])
                proj(Fb[1], tF[1], xs, r, C_V, 512, [txs])
                proj(Fb[2], tF[2], xs, r, C_IQ, 324, [txs])
                proj(Fb[3], tF[3], xs, r, C_HF, 512, [txs])
                proj(Fb[4], tF[4], xs, r, C_HI, 512, [txs])
                rope(Fb[0][:, :], tF[0], 8, n, KR[:, :], tKR)
                transposes(KR, tKR, 4, 0)
                op("act", lambda e: e.activation(out=KTS[g % 2][:, :, 128 * r:128 * r + 128],
                                                 in_=Hb[0][:, 0:512].rearrange("p (a t) -> p a t", a=4), func=AF.Copy),
                   reads=[tH[0]], writes=[tKTS[g % 2]])
                op("act", lambda e: e.activation(out=VS[g % 2][:, r, :].rearrange("p (h e) -> p h e", e=65)[:, :, 0:64],
                                                 in_=Fb[1][:, :].rearrange("p (h d) -> p h d", d=64), func=AF.Copy),
                   reads=[tF[1]], writes=[tVS[g % 2]])
                rope(Fb[2][:, 0:320], tF[2], 5, n, XR[:, :], tXR)
                op("dve", lambda e: e.tensor_copy(out=IKR[:, :].rearrange("p (a d) -> p a d", a=2),
                                                  in_=XR[:, 256:320].unsqueeze(1).broadcast_to([128, 2, 64])),
                   reads=[tXR], writes=[tIKR])
                transposes(IKR, tIKR, 1, 1)
                op("act", lambda e: e.activation(out=IKS[g % 2][:, 128 * r:128 * r + 128], in_=Hb[1][:, 0:128], func=AF.Copy),
                   reads=[tH[1]], writes=[tIKS[g % 2]])
                op("act", lambda e: e.activation(out=EZ[:], in_=Fb[3][:, :], func=AF.Exp, scale=-1.0), reads=[tF[3]], writes=[tEZ])
                op("pool", lambda e: e.tensor_scalar(out=EZ[:], in0=EZ[:], scalar1=1.0, scalar2=None, op0=ALU.add), reads=[tEZ], writes=[tEZ])
                op("dve", lambda e: e.reciprocal(out=FG[:], in_=EZ[:]), reads=[tEZ], writes=[tFG])
                op("pool", lambda e: e.tensor_tensor(out=FG[:], in0=FG[:], in1=OML[:], op=ALU.mult), reads=[tFG, tLB], writes=[tFG])
                op("pool", lambda e: e.tensor_tensor(out=FG[:], in0=FG[:], in1=LB[:], op=ALU.add), reads=[tFG, tLB], writes=[tFG])
                op("act", lambda e: e.activation(out=LF[:], in_=FG[:], func=AF.Ln), reads=[tFG], writes=[tLF])
                op("pool", lambda e: e.tensor_scalar(out=KK[:], in0=FG[:], scalar1=-1.0, scalar2=1.0, op0=ALU.mult, op1=ALU.add),
                   reads=[tFG], writes=[tKK])
                grp("pe", [lambda e: e.matmul(Fb[3][:, :], lhsT=LT_U128[:], rhs=LF[:], start=True, stop=True)],
                    reads=[tLF, tC], writes=[tF[3]])
                op("act", lambda e: e.activation(out=DP[:], in_=Fb[3][:, :], func=AF.Exp), reads=[tF[3]], writes=[tDP])
                op("pool", lambda e: e.tensor_tensor(out=DK[:], in0=KK[:], in1=DP[:], op=ALU.mult), reads=[tKK, tDP], writes=[tDK])
                op("act", lambda e: e.activation(out=VH[:], in_=Fb[4][:, :], func=AF.Copy), reads=[tF[4]], writes=[tVH])
                grp("pe", [(lambda e, h=h: e.matmul(Fb[2][:, 384 + 2 * h:386 + 2 * h], lhsT=LF[:, 128 * h:128 * h + 128], rhs=IND2[:],
                                                    start=True, stop=True)) for h in range(4)],
                    reads=[tLF, tC], writes=[tF2s])
                op("act", lambda e: e.activation(out=DCOL[:], in_=Fb[2][:, 384:392], func=AF.Exp), reads=[tF2s], writes=[tDCOL])
                if own:
                    op("act", lambda e: e.activation(out=SB16[:], in_=ST_[:], func=AF.Copy), reads=[tST], writes=[tSB16])
                grp("pe", [(lambda e, h=h: e.matmul(Fb[5][:, 128 * h:128 * h + 128], lhsT=DK[:, 128 * h:128 * h + 128],
                                                    rhs=VH[:, 128 * h:128 * h + 128], start=True, stop=True)) for h in range(4)],
                    reads=[tDK, tVH], writes=[tF[5]])
                op("dve", lambda e: e.tensor_tensor(out=ST_[:], in0=ST_[:],
                                                    in1=DCOL[:, :].rearrange("p (h c) -> p h c", c=2)[:, :, 1:2].broadcast_to([128, 4, 128]),
                                                    op=ALU.mult), reads=[tST, tDCOL], writes=[tST])
                op("dve", lambda e: e.tensor_tensor(out=ST_[:], in0=ST_[:], in1=Fb[5][:, :].rearrange("p (h v) -> p h v", h=4), op=ALU.add),
                   reads=[tST, tF[5]], writes=[tST])

                if own:
                    sl = m % 2
                    proj(Fb[0], tF[0], xs, r, C_Q, 512, [txs])
                    rope(Fb[0][:, :], tF[0], 8, n, KR[:, :], tKR)
                    transposes(KR, tKR, 4, 0)
                    op("act", lambda e: e.activation(out=QTS[sl][:], in_=Hb[0][:, 0:512].rearrange("p (a t) -> p a t", a=4), func=AF.Copy),
                       reads=[tH[0]], writes=[tQTS[sl]])
                    dma("sp", dQTS[sl], QT_d[:, :, 128 * m:128 * m + 128], QTS[sl][:], reads=[tQTS[sl]], writes=[tQTd[m]])
                    op("dve", lambda e: e.tensor_scalar(out=TC_[:, 0:4], in0=Fb[2][:, 320:324], scalar1=0.0, scalar2=2.0,
                                                        op0=ALU.is_ge, op1=ALU.mult), reads=[tF[2]], writes=[tTC])
                    op("dve", lambda e: e.tensor_scalar(out=TC_[:, 4:8], in0=TC_[:, 0:4], scalar1=-1.0, scalar2=None, op0=ALU.add),
                       reads=[tTC], writes=[tTC])
                    op("dve", lambda e: e.tensor_copy(out=SGN[:], in_=TC_[:, 4:8]), reads=[tTC], writes=[tSGN])
                    op("dve", lambda e: e.scalar_tensor_tensor(out=AW[:], in0=Fb[2][:, 320:324], scalar=IDX_SCALE, in1=TC_[:, 4:8],
                                                               op0=ALU.mult, op1=ALU.mult), reads=[tF[2], tTC], writes=[tAW])
                    op("dve", lambda e: e.tensor_tensor(out=IQS[:, :].rearrange("p (h d) -> p h d", d=64),
                                                        in0=XR[:, 0:256].rearrange("p (h d) -> p h d", d=64),
                                                        in1=AW[:, :].unsqueeze(2).broadcast_to([128, 4, 64]), op=ALU.mult),
                       reads=[tXR, tAW], writes=[tIQS])
                    transposes(IQS, tIQS, 2, 1)
                    op("act", lambda e: e.activation(out=IQTS[sl][:].rearrange("p u a t -> p a u t"), in_=Hb[1][:, 0:256].rearrange("p (a u t) -> p a u t", a=2, u=2), func=AF.Copy),
                       reads=[tH[1]], writes=[tIQTS[sl]])
                    dma("sp", dIQTS[sl], IQT_d[:, m, :], IQTS[sl][:].rearrange("p u a t -> p (u a t)"), reads=[tIQTS[sl]], writes=[tIQTd[m]])
                    fns = []
                    for u in range(2):
                        fns.append(lambda e, u=u: e.matmul(Fb[2][:, 400 + 2 * u:402 + 2 * u], lhsT=ETA[u][:], rhs=SGN[:, 0:2], start=True, stop=False))
                        fns.append(lambda e, u=u: e.matmul(Fb[2][:, 400 + 2 * u:402 + 2 * u], lhsT=ETB[u][:], rhs=SGN[:, 2:4], start=False, stop=True))
                    grp("pe", fns, reads=[tSGN, tC], writes=[tF2s])
                    for hp in range(2):
                        for u in range(2):
                            op("dve", lambda e, hp=hp, u=u: e.tensor_scalar(out=SELS[sl][:, 2 * hp + u, :], in0=E_u[u][:],
                                                                            scalar1=Fb[2][:, 400 + 2 * u + hp:401 + 2 * u + hp],
                                                                            scalar2=None, op0=ALU.mult),
                               reads=[tF2s, tC], writes=[tSELS[sl]])
                    dma("sp", dSELS[sl], SEL_d[:, m, :], SELS[sl][:].rearrange("p a t -> p (a t)"), reads=[tSELS[sl]], writes=[tSELd[m]])
                    proj(Fb[1], tF[1], xs, r, C_HQ, 512, [txs])
                    proj(Fb[4], tF[4], xs, r, C_HG, 512, [txs])
                    grp("pe", [lambda e: e.matmul(Fb[3][:, :], lhsT=LT_I64[:], rhs=LF[:], start=True, stop=True)], reads=[tLF, tC], writes=[tF[3]])
                    op("act", lambda e: e.activation(out=AQ[:], in_=Fb[3][:, :], func=AF.Exp), reads=[tF[3]], writes=[tAQ])
                    op("act", lambda e: e.activation(out=BN[:], in_=Fb[3][:, :], func=AF.Exp, scale=-1.0), reads=[tF[3]], writes=[tBN])
                    grp("pe", [lambda e: e.matmul(Fb[3][:, :], lhsT=LT_U64[:], rhs=LF[:], start=True, stop=True)], reads=[tLF, tC], writes=[tF[3]])
                    op("act", lambda e: e.activation(out=D64[:], in_=Fb[3][:, :], func=AF.Exp), reads=[tF[3]], writes=[tD64])
                    op("dve", lambda e: e.tensor_tensor(out=QA[:], in0=Fb[1][:, :], in1=AQ[:], op=ALU.mult), reads=[tF[1], tAQ], writes=[tQA])
                    op("pool", lambda e: e.tensor_tensor(out=KBm[:], in0=KK[:], in1=BN[:], op=ALU.mult), reads=[tKK, tBN], writes=[tKBm])
                    op("pool", lambda e: e.tensor_tensor(out=KD[:], in0=KK[:], in1=D64[:], op=ALU.mult), reads=[tKK, tD64], writes=[tKD])
                    transposes(QA, tQA, 4, 0)
                    h0v = Hb[0][:, 0:512].rearrange("p (a t) -> p a t", a=4)
                    op("act", lambda e: e.activation(out=QAT[:], in_=h0v, func=AF.Copy), reads=[tH[0]], writes=[tQAT])
                    op("act", lambda e: e.activation(out=QAI[:, :, 0:64], in_=h0v[:, :, 0:64], func=AF.Copy), reads=[tH[0]], writes=[tQAI])
                    op("dve", lambda e: e.tensor_tensor(out=QAI[:, :, 64:128], in0=h0v[:, :, 64:128],
                                                        in1=DCOL[:, :].rearrange("p (h c) -> p h c", c=2)[:, :, 0:1].broadcast_to([128, 4, 64]),
                                                        op=ALU.mult), reads=[tH[0], tDCOL], writes=[tQAI])
                    transposes(KBm, tKBm, 4, 1)
                    op("act", lambda e: e.activation(out=KBT[:], in_=Hb[1][:, 0:512].rearrange("p (a t) -> p a t", a=4), func=AF.Copy),
                       reads=[tH[1]], writes=[tKBT])
                    transposes(KD, tKD, 4, 0, out_cols=64, in_rows=slice(0, 64))
                    op("act", lambda e: e.activation(out=KDT[:], in_=Hb[0][:, 0:256].rearrange("p (a t) -> p a t", a=4), func=AF.Copy),
                       reads=[tH[0]], writes=[tKDT])
                    grp("pe", [(lambda e, h=h: e.matmul(Fb[5][:, 128 * h:128 * h + 128], lhsT=KBT[:, h, :], rhs=QAT[:, h, :], start=True, stop=True))
                               for h in range(4)], reads=[tKBT, tQAT], writes=[tF[5]])
                    op("dve", lambda e: e.tensor_tensor(out=AT[:], in0=Fb[5][:, :].rearrange("p (h t) -> p h t", h=4),
                                                        in1=MASKBD[:, :].unsqueeze(1).broadcast_to([128, 4, 128]), op=ALU.mult),
                       reads=[tF[5], tC], writes=[tAT])
                    grp("pe", [(lambda e, h=h: e.matmul(Fb[0][0:64, 64 * h:64 * h + 64], lhsT=KDT[:, h, :], rhs=QAT[:, h, 64:128], start=True, stop=True))
                               for h in range(4)], reads=[tKDT, tQAT], writes=[tF[0]])
                    op("dve", lambda e: e.tensor_copy(out=AT[0:64, :, 64:128], in_=Fb[0][0:64, 0:256].rearrange("p (h t) -> p h t", h=4)),
                       reads=[tF[0]], writes=[tAT])
                    fns = []
                    for h in range(4):
                        fns.append(lambda e, h=h: e.matmul(Fb[5][:, 128 * h:128 * h + 128], lhsT=AT[:, h, :], rhs=VH[:, 128 * h:128 * h + 128], start=True, stop=False))
                        fns.append(lambda e, h=h: e.matmul(Fb[5][:, 128 * h:128 * h + 128], lhsT=QAI[:, h, :], rhs=SB16[:, h, :], start=False, stop=True))
                    grp("pe", fns, reads=[tAT, tVH, tQAI, tSB16], writes=[tF[5]])
                    for h in range(4):
                        op("act", lambda e, h=h: e.activation(out=JK[:], in_=Fb[5][:, 128 * h:128 * h + 128], func=AF.Square, accum_out=MS[:, h:h + 1]),
                           reads=[tF[5]], writes=[tMS, tJK])
                    op("dve", lambda e: e.tensor_scalar(out=MS[:], in0=MS[:], scalar1=1.0 / 128.0, scalar2=RMS_EPS, op0=ALU.mult, op1=ALU.add),
                       reads=[tMS], writes=[tMS])
                    op("act", lambda e: e.activation(out=RS[:], in_=MS[:], func=AF.Ln), reads=[tMS], writes=[tRS])
                    op("act", lambda e: e.activation(out=RS[:], in_=RS[:], func=AF.Exp, scale=-0.5), reads=[tRS], writes=[tRS])
                    op("act", lambda e: e.activation(out=GS[:], in_=Fb[4][:, :], func=AF.Exp, scale=-1.0), reads=[tF[4]], writes=[tGS])
                    op("pool", lambda e: e.tensor_scalar(out=GS[:], in0=GS[:], scalar1=1.0, scalar2=None, op0=ALU.add), reads=[tGS], writes=[tGS])
                    op("dve", lambda e: e.reciprocal(out=GS[:], in_=GS[:]), reads=[tGS], writes=[tGS])
                    op("dve", lambda e: e.tensor_tensor(out=GS[:], in0=Fb[4][:, :], in1=GS[:], op=ALU.mult), reads=[tF[4], tGS], writes=[tGS])
                    op("dve", lambda e: e.tensor_tensor(out=OG[:, :].rearrange("p (h v) -> p h v", h=4),
                                                        in0=Fb[5][:, :].rearrange("p (h v) -> p h v", h=4),
                                                        in1=RS[:, :].unsqueeze(2).broadcast_to([128, 4, 128]), op=ALU.mult),
                       reads=[tF[5], tRS], writes=[tOG])
                    op("pool", lambda e: e.tensor_tensor(out=OG[:], in0=OG[:], in1=NGB[:], op=ALU.mult), reads=[tOG, tNG], writes=[tOG])
                    op("pool", lambda e: e.tensor_tensor(out=OGB[:], in0=OG[:], in1=GS[:], op=ALU.mult), reads=[tOG, tGS], writes=[tOGB])
                    transposes(OGB, tOGB, 4, 1)
                    op("act", lambda e: e.activation(out=HGTS[sl][:], in_=Hb[1][:, 0:512].rearrange("p (a t) -> p a t", a=4), func=AF.Copy),
                       reads=[tH[1]], writes=[tHGTS[sl]])
                    dma("sp", dHGTS[sl], HGT_d[:, :, 128 * m:128 * m + 128], HGTS[sl][:], reads=[tHGTS[sl]], writes=[tHGTd[m]])
                    dma("sp", dKTS[g % 2], KT_d[:, :, 512 * g:512 * g + 512], KTS[g % 2][:], reads=[tKTS[g % 2]], writes=[tKTd[g]])
                    dma("sp", dVS[g % 2], V_d[:, 4 * g:4 * g + 4, :], VS[g % 2][:], reads=[tVS[g % 2]], writes=[tVd[g]])
                    dma("sp", dIKS[g % 2], IKT_d[:, 512 * g:512 * g + 512], IKS[g % 2][:], reads=[tIKS[g % 2]], writes=[tIKTd[g]])
            if dbg:
                for nm, tl, tt in [("FG", FG, tFG), ("LF", LF, tLF), ("KK", KK, tKK), ("DP", DP, tDP), ("DCOL", DCOL, tDCOL),
                                   ("ST", ST_, tST), ("AQ", AQ, tAQ), ("BN", BN, tBN), ("D64", D64, tD64), ("QAT", QAT, tQAT),
                                   ("AT", AT, tAT), ("OG", OG, tOG), ("GS", GS, tGS), ("RS", RS, tRS), ("SB16", SB16, tSB16),
                                   ("COS", COS, tTab), ("SIN", SIN, tTab), ("VH", VH, tVH), ("QAI", QAI, tQAI), ("KBT", KBT, tKBT)]:
                    shp = list(tl[:].shape)
                    od = dram("dbg_" + nm, shp, tl[:].dtype, "ExternalOutput")
                    dsx = sc.dsem("dd_" + nm)
                    dma("sp", dsx, od, tl[:], reads=[tt])
                    sc._wait("sp", {dsx: dsx.count})
            p1_bufs = [tW, tXG[0], tXG[1], tTab, tLB, tL1, tNG, tC, tST, tKR, tTA, tTB, tTC, tXR, tIKR, tEZ, tFG, tLF, tKK, tDP, tDK, tVH,
                       tDCOL, tSB16, tAW, tSGN, tIQS, tAQ, tBN, tD64, tQA, tKBm, tKD, tQAT, tQAI, tKBT, tKDT, tAT, tMS, tRS, tJK, tGS, tOG,
                       tOGB, tPOS] + tKTS + tVS + tIKS + tQTS + tIQTS + tSELS + tHGTS
            for q in ("pe", "act", "dve", "pool", "sp"):
                sc.finish(q, p1_bufs)

        with ExitStack() as p2:
            IKT = sb(p2, "IKT", [128, S], BF16)
            tIKT = T("IKT")
            dIKT = sc.dsem("dIKT")
            for g in range(NG):
                dma("sp", dIKT, IKT[:, 512 * g:512 * g + 512], IKT_d[:, 512 * g:512 * g + 512], reads=[tIKTd[g]], writes=[tIKT])
            SCR = sb(p2, "SCR", [128, S], F32)
            tSCR = T("SCR")
            KB0 = sb(p2, "KB0", [128, 512], F32)
            KB0D = sb(p2, "KB0D", [128, 512], F32)
            tKB0 = T("KB0")
            dKB0 = sc.dsem("dKB0")
            dma("sp", dKB0, KB0[:], kb0_d[0:1, :].partition_broadcast(128), writes=[tKB0])
            op("dve", lambda e: e.tensor_copy(out=KB0D[:], in_=KB0[:]), reads=[tKB0], writes=[tKB0])
            op("dve", lambda e: e.tensor_tensor(out=KB0D[:, 384:512], in0=KB0D[:, 384:512], in1=TRIB[:], op=ALU.add), reads=[tKB0, tC], writes=[tKB0])
            HALF = sb(p2, "HALF", [128, 1], F32)
            op("dve", lambda e: e.memset(HALF[:], 0.5), writes=[tKB0])
            QTB = [sb(p2, "QTB%d" % i, [128, 4, 128], BF16) for i in range(2)]
            IQTB = [sb(p2, "IQTB%d" % i, [128, 256], BF16) for i in range(2)]
            SELB = [sb(p2, "SELB%d" % i, [128, 4, 128], BF16) for i in range(2)]
            tQL = [T(), T()]
            dQL = [sc.dsem("dQL0"), sc.dsem("dQL1")]
            RB = [sb(p2, "RB%d" % i, [128, 512], BF16) for i in range(8)]
            tRB = [T() for _ in range(8)]
            MX = sb(p2, "MX", [128, 32], F32); tMX = T()
            MNt = sb(p2, "MN", [128, 32], F32); tMN = T()
            HI = sb(p2, "HI", [128, 1], F32); LO = sb(p2, "LO", [128, 1], F32); MID = sb(p2, "MID", [128, 1], F32)
            CH = sb(p2, "CH", [128, 1], F32); CNT = [sb(p2, "CNT%d" % i, [128, 1], F32) for i in range(2)]
            DD = sb(p2, "DD", [128, 1], F32); MNEED = sb(p2, "MNEED", [128, 1], F32); CARRY = sb(p2, "CARRY", [128, 1], F32)
            SELI = sb(p2, "SELI", [128, 1], I32); NSEL = sb(p2, "NSEL", [128, 1], I32)
            tBS = T("bisect")
            JUNK = sb(p2, "JUNK", [128, 2048], BF16); tJUNK = T()
            AB = [sb(p2, "AB%d" % i, [128, 2048], BF16) for i in range(2)]; tAB = [T(), T()]
            PF = sb(p2, "PF", [128, 2048], F32); tPF = T()
            TM = sb(p2, "TM", [128, 2048], BF16); tTM = T()
            MK = sb(p2, "MK", [128, 2048], BF16); tMK = T()
            MT = sb(p2, "MT", [128, NB, 128], BF16); tMT = T()
            KTB = [sb(p2, "KTB%d" % i, [128, 4, 512], BF16) for i in range(2)]
            VB = [sb(p2, "VB%d" % i, [128, 4, 520], BF16) for i in range(2)]
            tKTB = [T(), T()]; tVB = [T(), T()]
            dKTB = [sc.dsem("dKTB0"), sc.dsem("dKTB1")]; dVB = [sc.dsem("dVB0"), sc.dsem("dVB1")]
            EX = [sb(p2, "EX%d" % i, [128, 4, 128], BF16) for i in range(4)]; tEX = [T() for _ in range(4)]
            tPM = []
            RC = sb(p2, "RC", [128, 8], F32); tRC = T()
            ATo = sb(p2, "ATo", [128, 512], BF16); tATo = T()
            ATS = [sb(p2, "ATS%d" % i, [128, 4, 128], BF16) for i in range(2)]; tATS = [T(), T()]
            dATS = [sc.dsem("dATS0"), sc.dsem("dATS1")]
            kchunk = 0

            QBD = [sb(p2, "QBD%d" % i, [128, 4, 256], BF16) for i in range(2)]
            tQBD = [T(), T()]
            for i in range(2):
                op("pool", lambda e, i=i: e.memset(QBD[i][:], 0.0), writes=[tQBD[i]])
            BIGM = 30000.0
            MKB = sb(p2, "MKB", [128, 2048], BF16); tMKB = T()

            def load_q(m):
                sl = m % 2
                dma("sp", dQL[sl], QTB[sl][:], QT_d[:, :, 128 * m:128 * m + 128], reads=[tQTd[m]], writes=[tQL[sl]])
                dma("sp", dQL[sl], IQTB[sl][:], IQT_d[:, m, :], reads=[tIQTd[m]], writes=[tQL[sl]])
                dma("sp", dQL[sl], SELB[sl][:].rearrange("p a t -> p (a t)"), SEL_d[:, m, :], reads=[tSELd[m]], writes=[tQL[sl]])
                op("pool", lambda e: e.tensor_copy(out=QBD[sl][0:64, :, 0:128], in_=QTB[sl][0:64, :, :]), reads=[tQL[sl]], writes=[tQBD[sl]])
                op("pool", lambda e: e.tensor_copy(out=QBD[sl][64:128, :, 128:256], in_=QTB[sl][64:128, :, :]), reads=[tQL[sl]], writes=[tQBD[sl]])

            def scores_bisect(m):
                sl = m % 2
                ng = m + 1
                nk = 512 * ng
                for G in range(ng):
                    rb0 = 4 * (G % 2)
                    for hp in range(2):
                        for u in range(2):
                            i4 = 2 * hp + u
                            ps = slice(64 * hp, 64 * hp + 64)
                            grp("pe", [lambda e, ps=ps, u=u, i4=i4: e.matmul(Fb[i4][:, :], lhsT=IQTB[sl][ps, 128 * u:128 * u + 128],
                                                                             rhs=IKT[ps, 512 * G:512 * G + 512], start=True, stop=True)],
                                reads=[tQL[sl], tIKT], writes=[tF[i4]])
                            op("act", lambda e, i4=i4: e.activation(out=RB[rb0 + i4][:], in_=Fb[i4][:, :], func=AF.Relu),
                               reads=[tF[i4]], writes=[tRB[rb0 + i4]])
                    fb = 4 + (G % 2)
                    grp("pe", [(lambda e, i4=i4: e.matmul(Fb[fb][:, :], lhsT=SELB[sl][:, i4, :], rhs=RB[rb0 + i4][:], start=(i4 == 0), stop=(i4 == 3)))
                               for i4 in range(4)], reads=[tQL[sl]] + [tRB[rb0 + i] for i in range(4)], writes=[tF[fb]])
                    op("dve", lambda e: e.tensor_reduce(out=MNt[:, G:G + 1], in_=Fb[fb][:, :], axis=AX.X, op=ALU.min),
                       reads=[tF[fb]], writes=[tMN])
                    csl = slice(512 * G, 512 * G + 512)
                    if G == 0:
                        kb = KB0D if m == 0 else KB0
                        op("dve", lambda e, kb=kb: e.tensor_tensor(out=SCR[:, csl], in0=Fb[fb][:, :], in1=kb[:], op=ALU.add),
                           reads=[tF[fb], tKB0], writes=[tSCR])
                    elif G == m:
                        op("dve", lambda e: e.tensor_copy(out=SCR[:, 512 * G:512 * G + 384], in_=Fb[fb][:, 0:384]), reads=[tF[fb]], writes=[tSCR])
                        op("dve", lambda e: e.tensor_tensor(out=SCR[:, 512 * G + 384:512 * G + 512], in0=Fb[fb][:, 384:512], in1=TRIB[:], op=ALU.add),
                           reads=[tF[fb], tC], writes=[tSCR])
                    else:
                        op("act", lambda e: e.activation(out=SCR[:, csl], in_=Fb[fb][:, :], func=AF.Copy), reads=[tF[fb]], writes=[tSCR])
                    op("dve", lambda e: e.tensor_reduce(out=MX[:, G:G + 1], in_=SCR[:, csl], axis=AX.X, op=ALU.max), reads=[tSCR], writes=[tMX])
                op("dve", lambda e: e.tensor_reduce(out=HI[:], in_=MX[:, 0:ng], axis=AX.X, op=ALU.max), reads=[tMX], writes=[tBS])
                op("dve", lambda e: e.tensor_reduce(out=LO[:], in_=MNt[:, 0:ng], axis=AX.X, op=ALU.min), reads=[tMN, tBS], writes=[tBS])
                op("dve", lambda e: e.tensor_tensor(out=DD[:], in0=HI[:], in1=LO[:], op=ALU.subtract), reads=[tBS], writes=[tBS])
                op("dve", lambda e: e.scalar_tensor_tensor(out=HI[:], in0=DD[:], scalar=1e-3, in1=HI[:], op0=ALU.mult, op1=ALU.add), reads=[tBS], writes=[tBS])
                op("dve", lambda e: e.tensor_scalar(out=HI[:], in0=HI[:], scalar1=1e-6, scalar2=None, op0=ALU.add), reads=[tBS], writes=[tBS])
                op("dve", lambda e: e.memset(CH[:], 0.0), reads=[tBS], writes=[tBS])
                chunks = [(c0, min(2048, nk - c0)) for c0 in range(0, nk, 2048)]
                for it in range(NIT):
                    op("dve", lambda e: e.scalar_tensor_tensor(out=MID[:], in0=LO[:], scalar=HI[:, 0:1], in1=HALF[:], op0=ALU.add, op1=ALU.mult),
                       reads=[tBS, tKB0], writes=[tBS])
                    for ci, (c0, w) in enumerate(chunks):
                        cprev = CNT[(ci + 1) % 2]
                        ccur = CNT[ci % 2]
                        op("dve", lambda e, c0=c0, w=w, ci=ci, cprev=cprev, ccur=ccur: e.tensor_scalar(
                            out=JUNK[:, 0:w], in0=SCR[:, c0:c0 + w], scalar1=MID[:, 0:1], scalar2=(None if ci == 0 else cprev[:, 0:1]),
                            op0=ALU.is_ge, op1=ALU.add, accum_out=ccur[:, 0:1]), reads=[tSCR, tBS], writes=[tBS, tJUNK])
                    cfin = CNT[(len(chunks) - 1) % 2]
                    op("dve", lambda e: e.tensor_scalar(out=SELI[:], in0=cfin[:], scalar1=TOPK, scalar2=None, op0=ALU.is_ge), reads=[tBS], writes=[tBS])
                    op("dve", lambda e: e.tensor_scalar(out=NSEL[:], in0=cfin[:], scalar1=TOPK, scalar2=None, op0=ALU.is_lt), reads=[tBS], writes=[tBS])
                    op("dve", lambda e: e.copy_predicated(out=LO[:], mask=SELI[:], data=MID[:]), reads=[tBS], writes=[tBS])
                    op("dve", lambda e: e.copy_predicated(out=HI[:], mask=NSEL[:], data=MID[:]), reads=[tBS], writes=[tBS])
                    op("dve", lambda e: e.copy_predicated(out=CH[:], mask=NSEL[:], data=cfin[:]), reads=[tBS], writes=[tBS])
                op("dve", lambda e: e.tensor_scalar(out=MNEED[:], in0=CH[:], scalar1=-1.0, scalar2=TOPK, op0=ALU.mult, op1=ALU.add), reads=[tBS], writes=[tBS])

            def final_mask(m):
                ng = m + 1
                nk = 512 * ng
                chunks = [(c0, min(2048, nk - c0)) for c0 in range(0, nk, 2048)]
                for ci, (c0, w) in enumerate(chunks):
                    op("dve", lambda e: e.tensor_scalar(out=AB[0][:, 0:w], in0=SCR[:, c0:c0 + w], scalar1=LO[:, 0:1], scalar2=None, op0=ALU.is_ge),
                       reads=[tSCR, tBS], writes=[tAB[0]])
                    op("dve", lambda e: e.tensor_scalar(out=AB[1][:, 0:w], in0=SCR[:, c0:c0 + w], scalar1=HI[:, 0:1], scalar2=None, op0=ALU.is_ge),
                       reads=[tSCR, tBS], writes=[tAB[1]])
                    if ci > 0:
                        op("dve", lambda e: e.tensor_copy(out=CARRY[:], in_=PF[:, 2047:2048]), reads=[tPF, tBS], writes=[tBS])
                    op("dve", lambda e: e.tensor_tensor_scan(out=PF[:, 0:w], data0=AB[0][:, 0:w], data1=AB[1][:, 0:w],
                                                             initial=(0.0 if ci == 0 else CARRY[:, 0:1]), op0=ALU.add, op1=ALU.subtract),
                       reads=[tAB[0], tAB[1], tBS], writes=[tPF])
                    op("dve", lambda e: e.scalar_tensor_tensor(out=TM[:, 0:w], in0=PF[:, 0:w], scalar=MNEED[:, 0:1], in1=AB[1][:, 0:w],
                                                               op0=ALU.is_le, op1=ALU.max), reads=[tPF, tAB[1], tBS], writes=[tTM])
                    op("pool", lambda e: e.tensor_tensor(out=MK[:, 0:w], in0=TM[:, 0:w], in1=AB[0][:, 0:w], op=ALU.mult), reads=[tTM, tAB[0]], writes=[tMK])
                    op("pool", lambda e: e.tensor_scalar(out=MKB[:, 0:w], in0=MK[:, 0:w], scalar1=-1.0, scalar2=BIGM, op0=ALU.add, op1=ALU.mult),
                       reads=[tMK], writes=[tMKB])
                    nb_ = w // 128
                    for j0 in range(0, nb_, 8):
                        j1 = min(nb_, j0 + 8)
                        hb = (j0 // 8) % 2
                        grp("pe", [(lambda e, j=j: e.transpose(out=Hb[hb][:, 128 * (j - j0):128 * (j - j0) + 128], in_=MKB[:, 128 * j:128 * j + 128], identity=IDN[:]))
                                   for j in range(j0, j1)], reads=[tMKB, tC], writes=[tH[hb]])
                        kb0_ = c0 // 128 + j0
                        op("act", lambda e: e.activation(out=MT[:, kb0_:kb0_ + (j1 - j0), :],
                                                         in_=Hb[hb][:, 0:128 * (j1 - j0)].rearrange("p (a t) -> p a t", t=128), func=AF.Copy),
                           reads=[tH[hb]], writes=[tMT])

            def attention(m):
                nonlocal kchunk
                sl = m % 2
                nkb = 4 * (m + 1)
                first = [True, True]
                for k0 in range(0, nkb, 4):
                    cs = kchunk % 2
                    kchunk += 1
                    gi = k0 // 4
                    dma("sp", dKTB[cs], KTB[cs][:], KT_d[:, :, 512 * gi:512 * gi + 512], reads=[tKTd[gi]], writes=[tKTB[cs]])
                    dma("sp", dVB[cs], VB[cs][:], V_d[:, 4 * gi:4 * gi + 4, :], reads=[tVd[gi]], writes=[tVB[cs]])
                    for j in range(4):
                        kb = k0 + j
                        last_kb = (kb == nkb - 1)
                        for half in range(2):
                            bi = 2 * (kb % 2) + half
                            fns = []
                            for pp in range(2):
                                pr = 2 * half + pp
                                fns.append(lambda e, pp=pp, pr=pr: e.matmul(Fb[bi][:, 256 * pp:256 * pp + 256], lhsT=KTB[cs][:, pr, 128 * j:128 * j + 128],
                                                                            rhs=QBD[sl][:, pr, :], start=True, stop=False))
                                for hp in range(2):
                                    fns.append(lambda e, pp=pp, hp=hp: e.matmul(Fb[bi][:, 256 * pp + 128 * hp:256 * pp + 128 * hp + 128], lhsT=IDN[:],
                                                                                rhs=MT[:, kb, :], start=False, stop=(hp == 1), skip_group_check=True))
                            grp("pe", fns, reads=[tKTB[cs], tQBD[sl], tMT, tC], writes=[tF[bi]])
                            op("act", lambda e: e.activation(out=EX[bi][:].rearrange("p a t -> p (a t)"), in_=Fb[bi][:, :], func=AF.Exp, scale=0.125),
                               reads=[tF[bi]], writes=[tEX[bi]])
                            fns = []
                            for idx in range(4):
                                head = 4 * half + idx
                                st_ = first[half]
                                first[half] = False
                                sp_ = last_kb and idx == 3
                                fns.append(lambda e, idx=idx, head=head, st_=st_, sp_=sp_: e.matmul(
                                    Fb[4 + half][:, 65 * idx:65 * idx + 65], lhsT=EX[bi][:, idx, :],
                                    rhs=VB[cs][:, j, 65 * head:65 * head + 65], start=st_, stop=sp_, skip_group_check=True))
                            grp("pe", fns, reads=[tEX[bi], tVB[cs]], writes=[tF[4 + half]])
                for ob in range(2):
                    ov = Fb[4 + ob][:, 0:260].rearrange("p (h e) -> p h e", e=65)
                    op("dve", lambda e, ov=ov, ob=ob: e.reciprocal(out=RC[:, 4 * ob:4 * ob + 4], in_=ov[:, :, 64]), reads=[tF[4 + ob]], writes=[tRC])
                    op("dve", lambda e, ov=ov, ob=ob: e.tensor_tensor(out=ATo[:, 256 * ob:256 * ob + 256].rearrange("p (h d) -> p h d", d=64),
                                                                      in0=ov[:, :, 0:64], in1=RC[:, 4 * ob:4 * ob + 4].unsqueeze(2).broadcast_to([128, 4, 64]),
                                                                      op=ALU.mult), reads=[tF[4 + ob], tRC], writes=[tATo])
                grp("pe", [(lambda e, i=i: e.transpose(out=Hb[0][:, 128 * i:128 * i + 128], in_=ATo[:, 128 * i:128 * i + 128], identity=IDN[:]))
                           for i in range(4)], reads=[tATo, tC], writes=[tH[0]])
                op("act", lambda e: e.activation(out=ATS[sl][:], in_=Hb[0][:, 0:512].rearrange("p (a t) -> p a t", a=4), func=AF.Copy),
                   reads=[tH[0]], writes=[tATS[sl]])
                dma("sp", dATS[sl], ATT_d[:, :, 128 * m:128 * m + 128], ATS[sl][:], reads=[tATS[sl]], writes=[tATTd[m]])

            load_q(0)
            scores_bisect(0)
            final_mask(0)
            for m in range(NOWN):
                if m + 1 < NOWN:
                    load_q(m + 1)
                    scores_bisect(m + 1)
                attention(m)
                if m + 1 < NOWN:
                    final_mask(m + 1)
            p2_bufs = [tIKT, tSCR, tKB0, tMX, tMN, tBS, tJUNK, tPF, tTM, tMK, tMKB, tMT, tRC, tATo] + tQBD + tQL + tRB + tAB + tKTB + tVB + tEX + tPM + tATS
            for q in ("pe", "act", "dve", "pool", "sp"):
                sc.finish(q, p2_bufs)

        with ExitStack() as p3:
            WO = sb(p3, "WO", [128, 8, D], BF16)
            WU = sb(p3, "WU", [128, 8, DFF], BF16)
            WD = sb(p3, "WD", [128, 32, D], BF16)
            tWO, tWU, tWD = T(), T(), T()
            dW3 = [sc.dsem("dW3_%d" % i) for i in range(3)]
            wo_v = wo_d.rearrange("(c p) n -> p c n", p=128)
            wu_v = wup_d.rearrange("(c p) n -> p c n", p=128)
            wd_v = wdn_d.rearrange("(c p) n -> p c n", p=128)
            for c in range(8):
                dma("pool", dW3[0], WO[:, c, :], wo_v[:, c, :], writes=[tWO])
            for c in range(8):
                dma("pool", dW3[1], WU[:, c, :], wu_v[:, c, :], writes=[tWU])
            for c in range(32):
                dma("pool", dW3[2], WD[:, c, :], wd_v[:, c, :], writes=[tWD])
            LNP = [sb(p3, "LNP%d" % i, [128, D], F32) for i in range(4)]
            tLNP = T()
            dLNP = [sc.dsem("dLNP%d" % i) for i in range(4)]
            for i in range(4):
                dma("sp", dLNP[i], LNP[i][:], lnp_ds[i][0:1, :].partition_broadcast(128), writes=[tLNP])
            ATB = [sb(p3, "ATB%d" % i, [128, 4, 128], BF16) for i in range(2)]
            HGB = [sb(p3, "HGB%d" % i, [128, 4, 128], BF16) for i in range(2)]
            XO = [sb(p3, "XO%d" % i, [128, D], F32) for i in range(2)]
            tL3 = [T(), T()]
            dL3 = [sc.dsem("dL3_0"), sc.dsem("dL3_1")]
            Y = sb(p3, "Y", [128, D], F32); tY = T()
            STT = sb(p3, "STT", [128, 2, 6], F32); MV = sb(p3, "MV", [128, 2], F32); RSD = sb(p3, "RSD", [128, 1], F32); tLN = T()
            X1 = sb(p3, "X1", [128, D], F32); tX1 = T()
            X1B = sb(p3, "X1B", [128, D], BF16); tX1B = T()
            X1T = sb(p3, "X1T", [128, 8, 128], BF16); tX1T = T()
            RT = [sb(p3, "RT%d" % i, [128, 128], F32) for i in range(2)]; tRT = [T(), T()]
            HT = sb(p3, "HT", [128, 32, 128], BF16); tHT = T()
            OB = [sb(p3, "OB%d" % i, [128, D], F32) for i in range(2)]; tOB = [T(), T()]
            dOB = [sc.dsem("dOB0"), sc.dsem("dOB1")]

            def layer_norm(src, tsrc, gi, dst, tdst):
                for hh in range(2):
                    op("dve", lambda e, hh=hh: e.bn_stats(out=STT[:, hh, :], in_=src[:, 512 * hh:512 * hh + 512]), reads=[tsrc], writes=[tLN])
                op("dve", lambda e: e.bn_aggr(out=MV[:], in_=STT[:].rearrange("p a b -> p (a b)")), reads=[tLN], writes=[tLN])
                op("dve", lambda e: e.tensor_scalar(out=RSD[:], in0=MV[:, 1:2], scalar1=LN_EPS, scalar2=None, op0=ALU.add), reads=[tLN], writes=[tLN])
                op("act", lambda e: e.activation(out=RSD[:], in_=RSD[:], func=AF.Ln), reads=[tLN], writes=[tLN])
                op("act", lambda e: e.activation(out=RSD[:], in_=RSD[:], func=AF.Exp, scale=-0.5), reads=[tLN], writes=[tLN])
                op("dve", lambda e: e.tensor_scalar(out=dst[:], in0=src[:], scalar1=MV[:, 0:1], scalar2=RSD[:, 0:1], op0=ALU.subtract, op1=ALU.mult),
                   reads=[tsrc, tLN], writes=[tdst])
                op("pool", lambda e: e.tensor_tensor(out=dst[:], in0=dst[:], in1=LNP[gi][:], op=ALU.mult), reads=[tdst, tLNP], writes=[tdst])
                op("pool", lambda e: e.tensor_tensor(out=dst[:], in0=dst[:], in1=LNP[gi + 1][:], op=ALU.add), reads=[tdst, tLNP], writes=[tdst])

            for m in range(NOWN):
                sl = m % 2
                dma("sp", dL3[sl], ATB[sl][:], ATT_d[:, :, 128 * m:128 * m + 128], reads=[tATTd[m]], writes=[tL3[sl]])
                dma("sp", dL3[sl], HGB[sl][:], HGT_d[:, :, 128 * m:128 * m + 128], reads=[tHGTd[m]], writes=[tL3[sl]])
                dma("sp", dL3[sl], XO[sl][:], xo_d[128 * m:128 * m + 128, :], writes=[tL3[sl]])
                for hh in range(2):
                    grp("pe", [(lambda e, c=c: e.matmul(Fb[hh][:, :], lhsT=(ATB[sl][:, c, :] if c < 4 else HGB[sl][:, c - 4, :]),
                                                        rhs=WO[:, c, 512 * hh:512 * hh + 512], start=(c == 0), stop=(c == 7))) for c in range(8)],
                        reads=[tL3[sl], tWO], writes=[tF[hh]])
                    op("dve", lambda e, hh=hh: e.scalar_tensor_tensor(out=Y[:, 512 * hh:512 * hh + 512], in0=XO[sl][:, 512 * hh:512 * hh + 512], scalar=ALPHA,
                                                                      in1=Fb[hh][:, :], op0=ALU.mult, op1=ALU.add), reads=[tL3[sl], tF[hh]], writes=[tY])
                layer_norm(Y, tY, 0, X1, tX1)
                op("act", lambda e: e.activation(out=X1B[:], in_=X1[:], func=AF.Copy), reads=[tX1], writes=[tX1B])
                grp("pe", [(lambda e, i=i: e.transpose(out=Hb[0][:, 128 * i:128 * i + 128], in_=X1B[:, 128 * i:128 * i + 128], identity=IDN[:]))
                           for i in range(8)], reads=[tX1B, tC], writes=[tH[0]])
                op("act", lambda e: e.activation(out=X1T[:], in_=Hb[0][:, :].rearrange("p (a t) -> p a t", a=8), func=AF.Copy), reads=[tH[0]], writes=[tX1T])
                for fc in range(32):
                    bi = 2 + fc % 4
                    grp("pe", [(lambda e, c=c: e.matmul(Fb[bi][:, 0:128], lhsT=WU[:, c, 128 * fc:128 * fc + 128], rhs=X1T[:, c, :],
                                                        start=(c == 0), stop=(c == 7))) for c in range(8)], reads=[tWU, tX1T], writes=[tF[bi]])
                    op("act", lambda e: e.activation(out=RT[fc % 2][:], in_=Fb[bi][:, 0:128], func=AF.Relu), reads=[tF[bi]], writes=[tRT[fc % 2]])
                    op("pool", lambda e: e.tensor_tensor(out=HT[:, fc, :], in0=RT[fc % 2][:], in1=RT[fc % 2][:], op=ALU.mult), reads=[tRT[fc % 2]], writes=[tHT])
                for hh in range(2):
                    grp("pe", [(lambda e, fc=fc: e.matmul(Fb[hh][:, :], lhsT=HT[:, fc, :], rhs=WD[:, fc, 512 * hh:512 * hh + 512],
                                                          start=(fc == 0), stop=(fc == 31))) for fc in range(32)], reads=[tHT, tWD], writes=[tF[hh]])
                    op("dve", lambda e, hh=hh: e.scalar_tensor_tensor(out=Y[:, 512 * hh:512 * hh + 512], in0=X1[:, 512 * hh:512 * hh + 512], scalar=ALPHA,
                                                                      in1=Fb[hh][:, :], op0=ALU.mult, op1=ALU.add), reads=[tX1, tF[hh]], writes=[tY])
                layer_norm(Y, tY, 2, OB[sl], tOB[sl])
                dma("sp", dOB[sl], y_d[128 * m:128 * m + 128, :], OB[sl][:], reads=[tOB[sl]])
            p3_bufs = [tWO, tWU, tWD, tLNP, tY, tLN, tX1, tX1B, tX1T, tHT] + tL3 + tRT + tOB
            for q in ("pe", "act", "dve", "pool", "sp"):
                sc.finish(q, p3_bufs)
        for q in ("pe", "act", "dve", "pool", "sp"):
            sc.finish(q, [tC] + tF + tH + [tF2s])
    return nc


def make_inputs(x, w_in, w_o, lb_logits, hg_norm_g, ln1_g, ln1_b, w_up, w_down, ln2_g, ln2_b, S=SEQ):
    NB = S // 128
    NOWN = NB // 4
    x = np.asarray(x, np.float32)
    B = x.shape[0]
    in_maps = []
    shared = {
        "w_in": np.ascontiguousarray(np.asarray(w_in, np.float32)[0]),
        "w_o": np.ascontiguousarray(np.asarray(w_o, np.float32)[0]),
        "w_up": np.ascontiguousarray(np.asarray(w_up, np.float32)[0]),
        "w_down": np.ascontiguousarray(np.asarray(w_down, np.float32)[0]),
        "lb0": np.ascontiguousarray(np.asarray(lb_logits, np.float32).reshape(2, 512)[0:1]),
        "lb1": np.ascontiguousarray(np.asarray(lb_logits, np.float32).reshape(2, 512)[1:2]),
        "hgn": np.ascontiguousarray(np.asarray(hg_norm_g, np.float32).reshape(1, 512)),
        "lnp0": np.ascontiguousarray(np.asarray(ln1_g, np.float32).reshape(1, D)),
        "lnp1": np.ascontiguousarray(np.asarray(ln1_b, np.float32).reshape(1, D)),
        "lnp2": np.ascontiguousarray(np.asarray(ln2_g, np.float32).reshape(1, D)),
        "lnp3": np.ascontiguousarray(np.asarray(ln2_b, np.float32).reshape(1, D)),
    }
    for c in range(4 * B):
        b, j = divmod(c, 4)
        npad = 3 - j
        nreal = NB - npad
        xp = np.zeros((S, D), np.float32)
        xp[128 * npad:] = x[b, :128 * nreal]
        own_blocks = [4 * m + j for m in range(NOWN)]
        xo = np.concatenate([x[b, 128 * r:128 * r + 128] for r in own_blocks], 0)
        pidx = np.arange(S, dtype=np.float32).reshape(NB, 128).T - 128.0 * npad
        pos = np.maximum(pidx, 0.0).astype(np.float32)
        kb0 = np.where(pidx.T.reshape(-1)[:512] < 0, NEG, 0.0).astype(np.float32).reshape(1, 512)
        d = dict(shared)
        d.update({"xT": np.ascontiguousarray(xp.T), "xo": np.ascontiguousarray(xo), "pos": np.ascontiguousarray(pos), "kb0": kb0})
        in_maps.append(d)
    return in_maps


def assemble(results, B, S=SEQ):
    NB = S // 128
    NOWN = NB // 4
    out = np.zeros((B, S, D), np.float32)
    for c in range(4 * B):
        b, j = divmod(c, 4)
        y = results[c]["y"]
        for m in range(NOWN):
            r = 4 * m + j
            out[b, 128 * r:128 * r + 128] = y[128 * m:128 * m + 128]
    return out


def kernel(x, w_in, w_o, lb_logits, hg_norm_g, ln1_g, ln1_b, w_up, w_down, ln2_g, ln2_b):
    x = np.asarray(x)
    B, S, _ = x.shape
    nc = build_nc(S)
    in_maps = make_inputs(x, w_in, w_o, lb_logits, hg_norm_g, ln1_g, ln1_b, w_up, w_down, ln2_g, ln2_b, S)
    res = run_bass_kernel_spmd(nc, in_maps, core_ids=list(range(4 * B)))
    return assemble(res.results, B, S)
```

```python
import math
from contextlib import ExitStack
import numpy as np
import concourse.bass as bass
import concourse.mybir as mybir
from concourse.bass_utils import run_bass_kernel_spmd

F32 = mybir.dt.float32
BF16 = mybir.dt.bfloat16
I32 = mybir.dt.int32
AF = mybir.ActivationFunctionType
ALU = mybir.AluOpType
AX = mybir.AxisListType

D = 1024
SEQ = 16384
NCOL = 3908
DFF = 4096
ALPHA = 2.0 ** 0.25
LN_EPS = 1e-5
RMS_EPS = 1e-6
IDX_SCALE = (4 ** -0.5) * (64 ** -0.5)
TOPK = 256.0
NEG = -1.0e30
NIT = 16

C_Q, C_K, C_V, C_IQ, C_IK, C_IW, C_HQ, C_HF, C_HI, C_HG = 0, 512, 1024, 1536, 1792, 1856, 1860, 2372, 2884, 3396


class T:
    __slots__ = ("w", "r", "name", "excl")

    def __init__(self, name="", excl=False):
        self.w = None
        self.r = {}
        self.name = name
        self.excl = excl


class DSem:
    def __init__(self, h):
        self.h = h
        self.count = 0


class Sched:
    def __init__(self, nc, es):
        self.nc = nc
        self.es = es
        self.eng = {"pe": nc.tensor, "act": nc.scalar, "dve": nc.vector, "pool": nc.gpsimd, "sp": nc.sync}
        self.sem = {k: es.enter_context(nc.semaphore("s_" + k)) for k in ("pe", "act", "dve", "pool")}
        self.cnt = {k: 0 for k in self.sem}
        self.seen = {k: {} for k in self.eng}
        self.n_ins = 0

    def dsem(self, name):
        return DSem(self.es.enter_context(self.nc.semaphore(name)))

    def _deps(self, reads, writes, eng=None):
        deps = {}
        for b in reads:
            if b.w is not None:
                k, v = b.w
                if deps.get(k, 0) < v:
                    deps[k] = v
            if b.excl:
                for k, v in b.r.items():
                    if k != eng and deps.get(k, 0) < v:
                        deps[k] = v
        for b in writes:
            if b.w is not None:
                k, v = b.w
                if deps.get(k, 0) < v:
                    deps[k] = v
            for k, v in b.r.items():
                if deps.get(k, 0) < v:
                    deps[k] = v
        return deps

    def _wait(self, eng, deps):
        seen = self.seen[eng]
        e = self.eng[eng]
        for k, v in deps.items():
            if seen.get(k, 0) >= v:
                continue
            if k == "pe" and eng == "pe":
                continue
            h = self.sem[k] if isinstance(k, str) else k.h
            e.wait_ge(h, v)
            seen[k] = v
            self.n_ins += 1

    def op(self, eng, fn, reads=(), writes=()):
        self._wait(eng, self._deps(reads, writes, eng))
        ins = fn(self.eng[eng])
        self.cnt[eng] += 1
        c = self.cnt[eng]
        ins.then_inc(self.sem[eng], 1)
        self.n_ins += 1
        for b in reads:
            b.r[eng] = c
        for b in writes:
            b.w = (eng, c)
            b.r = {}
        return ins

    def group(self, eng, fns, reads=(), writes=()):
        self._wait(eng, self._deps(reads, writes, eng))
        e = self.eng[eng]
        ins = None
        for fn in fns:
            ins = fn(e)
            self.n_ins += 1
        self.cnt[eng] += 1
        c = self.cnt[eng]
        ins.then_inc(self.sem[eng], 1)
        for b in reads:
            b.r[eng] = c
        for b in writes:
            b.w = (eng, c)
            b.r = {}

    def dma(self, q, ds, out, in_, reads=(), writes=()):
        deps = self._deps(reads, writes)
        deps.pop(ds, None)
        self._wait(q, deps)
        ins = self.eng[q].dma_start(out=out, in_=in_)
        ds.count += 16
        ins.then_inc(ds.h, 16)
        self.n_ins += 1
        for b in reads:
            b.r[ds] = ds.count
        for b in writes:
            b.w = (ds, ds.count)
            b.r = {}

    def finish(self, q, bufs):
        self._wait(q, self._deps(bufs, bufs))


def build_nc(S=SEQ, dbg=False):
    NB = S // 128
    NOWN = NB // 4
    SO = NOWN * 128
    NG = NB // 4
    nc = bass.Bass("TRN2", target_bir_lowering=False)
    dram = lambda n, s, d, k: nc.dram_tensor(n, s, d, kind=k).ap()
    xT_d = dram("xT", [D, S], F32, "ExternalInput")
    xo_d = dram("xo", [SO, D], F32, "ExternalInput")
    pos_d = dram("pos", [128, NB], F32, "ExternalInput")
    kb0_d = dram("kb0", [1, 512], F32, "ExternalInput")
    win_d = dram("w_in", [D, NCOL], F32, "ExternalInput")
    wo_d = dram("w_o", [D, D], F32, "ExternalInput")
    wup_d = dram("w_up", [D, DFF], F32, "ExternalInput")
    wdn_d = dram("w_down", [DFF, D], F32, "ExternalInput")
    lb0_d = dram("lb0", [1, 512], F32, "ExternalInput")
    lb1_d = dram("lb1", [1, 512], F32, "ExternalInput")
    hgn_d = dram("hgn", [1, 512], F32, "ExternalInput")
    lnp_ds = [dram("lnp%d" % i, [1, D], F32, "ExternalInput") for i in range(4)]
    y_d = dram("y", [SO, D], F32, "ExternalOutput")
    SK = "ExternalOutput" if dbg else "Internal"
    KT_d = dram("KT_s", [128, 4, S], BF16, SK)
    V_d = dram("V_s", [128, NB, 520], BF16, SK)
    IKT_d = dram("IKT_s", [128, S], BF16, SK)
    QT_d = dram("QT_s", [128, 4, SO], BF16, SK)
    IQT_d = dram("IQT_s", [128, NOWN, 256], BF16, SK)
    SEL_d = dram("SEL_s", [128, NOWN, 512], BF16, SK)
    HGT_d = dram("HGT_s", [128, 4, SO], BF16, SK)
    ATT_d = dram("ATT_s", [128, 4, SO], BF16, SK)
    dbg_outs = {}

    with ExitStack() as es:
        sc = Sched(nc, es)
        op, grp, dma = sc.op, sc.group, sc.dma

        def sb(stack, name, shape, dt):
            return stack.enter_context(nc.sbuf_tensor(name, shape, dt))

        Fb = [es.enter_context(nc.psum_tensor("F%d" % i, [128, 512], F32)) for i in range(6)]
        Hb = [es.enter_context(nc.psum_tensor("H%d" % i, [128, 1024], BF16)) for i in range(2)]
        tF = [T("F%d" % i, excl=True) for i in range(6)]
        tH = [T("H%d" % i, excl=True) for i in range(2)]
        tF2s = tF[2]

        IDN = sb(es, "IDN", [128, 128], BF16)
        tC = T("consts")
        op("pool", lambda e: e.memset(IDN[:], 1.0), writes=[tC])
        op("pool", lambda e: e.affine_select(out=IDN[:], in_=IDN[:], pattern=[[-1, 128]], compare_op=ALU.is_equal,
                                             fill=0.0, base=0, channel_multiplier=1), writes=[tC])
        TRIB = sb(es, "TRIB", [128, 128], F32)
        op("pool", lambda e: e.memset(TRIB[:], 0.0), writes=[tC])
        op("pool", lambda e: e.affine_select(out=TRIB[:], in_=TRIB[:], pattern=[[-1, 128]], compare_op=ALU.is_ge,
                                             fill=NEG, base=0, channel_multiplier=1), writes=[tC])

        fin_list = []

        with ExitStack() as p1:
            Wb = sb(p1, "Wb", [128, 8, NCOL], BF16)
            tW = T("Wb")
            dW = sc.dsem("dW")
            win_v = win_d.rearrange("(c p) n -> p c n", p=128)
            for c in range(8):
                dma("pool", dW, Wb[:, c, :], win_v[:, c, :], writes=[tW])
            XG = [sb(p1, "XG%d" % i, [128, 8, 512], BF16) for i in range(2)]
            tXG = [T("XG0"), T("XG1")]
            dXG = [sc.dsem("dXG0"), sc.dsem("dXG1")]
            xT_v = xT_d.rearrange("(c p) t -> p c t", p=128)
            POS = sb(p1, "POS", [128, NB], F32)
            dMisc = sc.dsem("dMisc")
            dPOS = sc.dsem("dPOS")
            tPOS = T("POS")
            dma("sp", dPOS, POS[:], pos_d[:, :], writes=[tPOS])
            COS = sb(p1, "COS", [128, NB, 32], F32)
            SIN = sb(p1, "SIN", [128, NB, 32], F32)
            tTab = T("tab")
            with ExitStack() as pt:
                INV = sb(pt, "INV", [128, 32], F32)
                ANG = sb(pt, "ANG", [128, NB, 32], F32)
                KQ = sb(pt, "KQ", [128, NB, 32], F32)
                RR = sb(pt, "RR", [128, NB, 32], F32)
                tI, tA, tK, tR = T(), T(), T(), T()
                for dd_ in range(32):
                    op("pool", lambda e, dd_=dd_: e.memset(INV[:, dd_:dd_ + 1], float(np.float32(10000.0 ** (-dd_ / 32.0)))), writes=[tI])
                op("dve", lambda e: e.tensor_tensor(out=ANG[:], in0=POS[:].unsqueeze(2).broadcast_to([128, NB, 32]),
                                                    in1=INV[:].unsqueeze(1).broadcast_to([128, NB, 32]), op=ALU.mult),
                   reads=[tPOS, tI], writes=[tA])
                TWO_PI = 2.0 * math.pi
                C1 = 6.28125
                C2 = TWO_PI - C1
                MAGIC = 12582912.0
                for which, off, TAB in (("sin", 0.0, SIN), ("cos", 0.25, COS)):
                    op("dve", lambda e: e.tensor_scalar(out=KQ[:], in0=ANG[:], scalar1=1.0 / TWO_PI, scalar2=off,
                                                        op0=ALU.mult, op1=ALU.add), reads=[tA], writes=[tK])
                    op("dve", lambda e: e.tensor_scalar(out=KQ[:], in0=KQ[:], scalar1=MAGIC, scalar2=None, op0=ALU.add),
                       reads=[tK], writes=[tK])
                    op("dve", lambda e: e.tensor_scalar(out=KQ[:], in0=KQ[:], scalar1=-MAGIC, scalar2=None, op0=ALU.add),
                       reads=[tK], writes=[tK])
                    op("dve", lambda e: e.scalar_tensor_tensor(out=RR[:], in0=KQ[:], scalar=-C1, in1=ANG[:],
                                                               op0=ALU.mult, op1=ALU.add), reads=[tK, tA], writes=[tR])
                    op("dve", lambda e: e.scalar_tensor_tensor(out=RR[:], in0=KQ[:], scalar=-C2, in1=RR[:],
                                                               op0=ALU.mult, op1=ALU.add), reads=[tK, tR], writes=[tR])
                    if which == "cos":
                        op("dve", lambda e: e.tensor_scalar(out=RR[:], in0=RR[:], scalar1=math.pi / 2.0, scalar2=None,
                                                            op0=ALU.add), reads=[tR], writes=[tR])
                    op("dve", lambda e: e.tensor_scalar(out=RR[:], in0=RR[:], scalar1=3.14159, scalar2=-3.14159,
                                                        op0=ALU.min, op1=ALU.max), reads=[tR], writes=[tR])
                    op("act", lambda e, TAB=TAB: e.activation(out=TAB[:], in_=RR[:], func=AF.Sin), reads=[tR], writes=[tTab])
                for q in ("pe", "act", "dve", "pool", "sp"):
                    sc.finish(q, [tI, tA, tK, tR, tTab, tPOS])

            LB = sb(p1, "LB", [128, 512], F32)
            OML = sb(p1, "OML", [128, 512], F32)
            NGB = sb(p1, "NGB", [128, 512], F32)
            L1 = sb(p1, "L1", [128, 512], F32)
            tLB = T("LB")
            dma("sp", dMisc, LB[:], lb0_d[0:1, :].partition_broadcast(128), writes=[tLB])
            tL1 = T("L1")
            dL1 = sc.dsem("dL1")
            dma("sp", dL1, L1[:], lb1_d[0:1, :].partition_broadcast(128), writes=[tL1])
            tNG = T("NGB")
            dNG = sc.dsem("dNG")
            dma("sp", dNG, NGB[:], hgn_d[0:1, :].partition_broadcast(128), writes=[tNG])
            op("dve", lambda e: e.tensor_tensor(out=L1[:], in0=L1[:], in1=LB[:], op=ALU.subtract), reads=[tL1, tLB], writes=[tL1])
            op("act", lambda e: e.activation(out=L1[:], in_=L1[:], func=AF.Exp), reads=[tL1], writes=[tL1])
            op("dve", lambda e: e.tensor_scalar(out=L1[:], in0=L1[:], scalar1=1.0, scalar2=None, op0=ALU.add), reads=[tL1], writes=[tL1])
            op("dve", lambda e: e.reciprocal(out=LB[:], in_=L1[:]), reads=[tL1], writes=[tLB])
            op("dve", lambda e: e.tensor_scalar(out=OML[:], in0=LB[:], scalar1=-1.0, scalar2=1.0, op0=ALU.mult, op1=ALU.add),
               reads=[tLB], writes=[tLB])
            LT_U128 = sb(p1, "LT_U128", [128, 128], F32)
            LT_I64 = sb(p1, "LT_I64", [128, 128], F32)
            LT_U64 = sb(p1, "LT_U64", [128, 128], F32)
            IND2 = sb(p1, "IND2", [128, 2], F32)
            MASKBD = sb(p1, "MASKBD", [128, 128], F32)
            op("pool", lambda e: e.memset(LT_U128[:], 1.0), writes=[tC])
            op("pool", lambda e: e.affine_select(out=LT_U128[:], in_=LT_U128[:], pattern=[[-1, 128]], compare_op=ALU.is_gt,
                                                 fill=0.0, base=0, channel_multiplier=1), writes=[tC])
            op("pool", lambda e: e.memset(LT_I64[:], 0.0), writes=[tC])
            op("pool", lambda e: e.memset(LT_U64[:], 0.0), writes=[tC])
            for cblk in range(2):
                sl = slice(64 * cblk, 64 * cblk + 64)
                op("pool", lambda e, sl=sl: e.memset(LT_I64[sl, sl], 1.0), writes=[tC])
                op("pool", lambda e, sl=sl: e.affine_select(out=LT_I64[sl, sl], in_=LT_I64[sl, sl], pattern=[[1, 64]],
                                                            compare_op=ALU.is_ge, fill=0.0, base=0, channel_multiplier=-1), writes=[tC])
                op("pool", lambda e, sl=sl: e.memset(LT_U64[sl, sl], 1.0), writes=[tC])
                op("pool", lambda e, sl=sl: e.affine_select(out=LT_U64[sl, sl], in_=LT_U64[sl, sl], pattern=[[-1, 64]],
                                                            compare_op=ALU.is_gt, fill=0.0, base=0, channel_multiplier=1), writes=[tC])
            op("pool", lambda e: e.tensor_copy(out=MASKBD[:], in_=LT_I64[:]), writes=[tC])
            op("pool", lambda e: e.memset(IND2[:], 1.0), writes=[tC])
            op("pool", lambda e: e.memset(IND2[64:128, 0:1], 0.0), writes=[tC])
            E_u = [sb(p1, "E_u%d" % u, [128, 128], BF16) for u in range(2)]
            ETA = [sb(p1, "ETA%d" % u, [128, 128], BF16) for u in range(2)]
            ETB = [sb(p1, "ETB%d" % u, [128, 128], BF16) for u in range(2)]
            for u in range(2):
                op("pool", lambda e, u=u: e.memset(E_u[u][:], 1.0), writes=[tC])
                for hf_ in range(2):
                    ps = slice(64 * hf_, 64 * hf_ + 64)
                    op("pool", lambda e, u=u, ps=ps: e.affine_select(out=E_u[u][ps, :], in_=E_u[u][ps, :], pattern=[[1, 128]],
                                                                      compare_op=ALU.is_equal, fill=0.0, base=-64 * u,
                                                                      channel_multiplier=-1), writes=[tC])
                op("pool", lambda e, u=u: e.memset(ETA[u][:], 0.0), writes=[tC])
                op("pool", lambda e, u=u: e.memset(ETB[u][:], 0.0), writes=[tC])
                op("pool", lambda e, u=u: e.memset(ETA[u][:, 0:64], 1.0), writes=[tC])
                op("pool", lambda e, u=u: e.memset(ETB[u][:, 64:128], 1.0), writes=[tC])
                op("pool", lambda e, u=u: e.affine_select(out=ETA[u][:, 0:64], in_=ETA[u][:, 0:64], pattern=[[1, 64]],
                                                          compare_op=ALU.is_equal, fill=0.0, base=64 * u, channel_multiplier=-1), writes=[tC])
                op("pool", lambda e, u=u: e.affine_select(out=ETB[u][:, 64:128], in_=ETB[u][:, 64:128], pattern=[[1, 64]],
                                                          compare_op=ALU.is_equal, fill=0.0, base=64 * u, channel_multiplier=-1), writes=[tC])

            KTS = [sb(p1, "KTS%d" % i, [128, 4, 512], BF16) for i in range(2)]
            VS = [sb(p1, "VS%d" % i, [128, 4, 520], BF16) for i in range(2)]
            IKS = [sb(p1, "IKS%d" % i, [128, 512], BF16) for i in range(2)]
            tKTS = [T(), T()]; tVS = [T(), T()]; tIKS = [T(), T()]
            dKTS = [sc.dsem("dKTS0"), sc.dsem("dKTS1")]
            dVS = [sc.dsem("dVS0"), sc.dsem("dVS1")]
            dIKS = [sc.dsem("dIKS0"), sc.dsem("dIKS1")]
            for i in range(2):
                op("pool", lambda e, i=i: e.memset(VS[i][:], 1.0), writes=[tVS[i]])
            ST_ = sb(p1, "STATE", [128, 4, 128], F32)
            tST = T("state")
            op("pool", lambda e: e.memset(ST_[:], 0.0), writes=[tST])

            def tmp(name, shape, dt):
                return sb(p1, name, shape, dt), T(name)
            KR, tKR = tmp("KR", [128, 512], BF16)
            TA, tTA = tmp("TA", [128, 512], F32)
            TB_, tTB = tmp("TB", [128, 256], F32)
            TC_, tTC = tmp("TC", [128, 256], F32)
            XR, tXR = tmp("XR", [128, 320], F32)
            IKR, tIKR = tmp("IKR", [128, 128], BF16)
            EZ, tEZ = tmp("EZ", [128, 512], F32)
            FG, tFG = tmp("FG", [128, 512], F32)
            LF, tLF = tmp("LF", [128, 512], F32)
            KK, tKK = tmp("KK", [128, 512], F32)
            DP, tDP = tmp("DP", [128, 512], F32)
            DK, tDK = tmp("DK", [128, 512], BF16)
            VH, tVH = tmp("VH", [128, 512], BF16)
            DCOL, tDCOL = tmp("DCOL", [128, 8], F32)
            SB16, tSB16 = tmp("SB16", [128, 4, 128], BF16)
            AW, tAW = tmp("AW", [128, 4], F32)
            SGN, tSGN = tmp("SGN", [128, 4], BF16)
            IQS, tIQS = tmp("IQS", [128, 256], BF16)
            AQ, tAQ = tmp("AQ", [128, 512], F32)
            BN, tBN = tmp("BN", [128, 512], F32)
            D64, tD64 = tmp("D64", [128, 512], F32)
            QA, tQA = tmp("QA", [128, 512], BF16)
            KBm, tKBm = tmp("KBm", [128, 512], BF16)
            KD, tKD = tmp("KD", [128, 512], BF16)
            QAT, tQAT = tmp("QAT", [128, 4, 128], BF16)
            QAI, tQAI = tmp("QAI", [128, 4, 128], BF16)
            KBT, tKBT = tmp("KBT", [128, 4, 128], BF16)
            KDT, tKDT = tmp("KDT", [128, 4, 64], BF16)
            AT, tAT = tmp("AT", [128, 4, 128], BF16)
            MS, tMS = tmp("MS", [128, 4], F32)
            RS, tRS = tmp("RS", [128, 4], F32)
            JK, tJK = tmp("JK", [128, 128], F32)
            GS, tGS = tmp("GS", [128, 512], F32)
            OG, tOG = tmp("OG", [128, 512], F32)
            OGB, tOGB = tmp("OGB", [128, 512], BF16)
            QTS = [sb(p1, "QTS%d" % i, [128, 4, 128], BF16) for i in range(2)]
            IQTS = [sb(p1, "IQTS%d" % i, [128, 2, 2, 64], BF16) for i in range(2)]
            SELS = [sb(p1, "SELS%d" % i, [128, 4, 128], BF16) for i in range(2)]
            HGTS = [sb(p1, "HGTS%d" % i, [128, 4, 128], BF16) for i in range(2)]
            tQTS = [T(), T()]; tIQTS = [T(), T()]; tSELS = [T(), T()]; tHGTS = [T(), T()]
            dQTS = [sc.dsem("dQTS0"), sc.dsem("dQTS1")]
            dIQTS = [sc.dsem("dIQTS0"), sc.dsem("dIQTS1")]
            dSELS = [sc.dsem("dSELS0"), sc.dsem("dSELS1")]
            dHGTS = [sc.dsem("dHGTS0"), sc.dsem("dHGTS1")]
            tKTd = [T() for _ in range(NG)]
            tVd = [T() for _ in range(NG)]
            tIKTd = [T() for _ in range(NG)]
            tQTd = [T() for _ in range(NOWN)]
            tIQTd = [T() for _ in range(NOWN)]
            tSELd = [T() for _ in range(NOWN)]
            tHGTd = [T() for _ in range(NOWN)]
            tATTd = [T() for _ in range(NOWN)]

            def proj(bank, tb, xs, r, c0, w, extra_reads=()):
                grp("pe", [(lambda e, c=c: e.matmul(bank[:, 0:w], lhsT=xs[:, c, 128 * r:128 * r + 128], rhs=Wb[:, c, c0:c0 + w],
                                                     start=(c == 0), stop=(c == 7))) for c in range(8)],
                    reads=[tW] + list(extra_reads), writes=[tb])

            def rope(src, tsrc, nh, n, dst, tdst):
                cosb = COS[:, n, :].unsqueeze(1).broadcast_to([128, 2 * nh, 32])
                sinb = SIN[:, n, :].unsqueeze(1).broadcast_to([128, nh, 32])
                s4 = src.rearrange("p (h two d) -> p h two d", two=2, d=32)
                d4 = dst.rearrange("p (h two d) -> p h two d", two=2, d=32)
                ta = TA[:, 0:nh * 64]
                tb = TB_[:, 0:nh * 32].rearrange("p (h d) -> p h d", d=32)
                tc_ = TC_[:, 0:nh * 32].rearrange("p (h d) -> p h d", d=32)
                ta4 = ta.rearrange("p (h two d) -> p h two d", two=2, d=32)
                op("dve", lambda e: e.tensor_tensor(out=ta.rearrange("p (g d) -> p g d", d=32),
                                                    in0=src.rearrange("p (g d) -> p g d", d=32), in1=cosb, op=ALU.mult),
                   reads=[tsrc, tTab], writes=[tTA])
                op("dve", lambda e: e.tensor_tensor(out=tb, in0=s4[:, :, 1, :], in1=sinb, op=ALU.mult), reads=[tsrc, tTab], writes=[tTB])
                op("dve", lambda e: e.tensor_tensor(out=tc_, in0=s4[:, :, 0, :], in1=sinb, op=ALU.mult), reads=[tsrc, tTab], writes=[tTC])
                op("dve", lambda e: e.tensor_tensor(out=d4[:, :, 0, :], in0=ta4[:, :, 0, :], in1=tb, op=ALU.subtract),
                   reads=[tTA, tTB], writes=[tdst])
                op("dve", lambda e: e.tensor_tensor(out=d4[:, :, 1, :], in0=ta4[:, :, 1, :], in1=tc_, op=ALU.add),
                   reads=[tTA, tTC], writes=[tdst])

            def transposes(src, tsrc, nblk, hb, out_cols=128, in_rows=slice(0, 128)):
                grp("pe", [(lambda e, i=i: e.transpose(out=Hb[hb][:, i * out_cols:(i + 1) * out_cols],
                                                       in_=src[in_rows, 128 * i:128 * i + 128], identity=IDN[in_rows, in_rows]))
                           for i in range(nblk)], reads=[tsrc, tC], writes=[tH[hb]])

            for n in range(NB):
                g, r = divmod(n, 4)
                own = (r == 3)
                m = g
                xs = XG[g % 2]
                if r == 0:
                    dma("pool", dXG[g % 2], xs[:], xT_v[:, :, 512 * g:512 * g + 512], writes=[tXG[g % 2]])
                txs = tXG[g % 2]
                proj(Fb[3], tF[3], xs, r, C_HF, 512, [txs])
                op("act", lambda e: e.activation(out=EZ[:], in_=Fb[3][:, :], func=AF.Exp, scale=-1.0), reads=[tF[3]], writes=[tEZ])
                op("dve", lambda e: e.tensor_scalar(out=EZ[:], in0=EZ[:], scalar1=1.0, scalar2=None, op0=ALU.add), reads=[tEZ], writes=[tEZ])
                op("dve", lambda e: e.reciprocal(out=FG[:], in_=EZ[:]), reads=[tEZ], writes=[tFG])
                op("dve", lambda e: e.tensor_tensor(out=FG[:], in0=FG[:], in1=OML[:], op=ALU.mult), reads=[tFG, tLB], writes=[tFG])
                op("dve", lambda e: e.tensor_tensor(out=FG[:], in0=FG[:], in1=LB[:], op=ALU.add), reads=[tFG, tLB], writes=[tFG])
                op("act", lambda e: e.activation(out=LF[:], in_=FG[:], func=AF.Ln), reads=[tFG], writes=[tLF])
                op("pool", lambda e: e.tensor_scalar(out=KK[:], in0=FG[:], scalar1=-1.0, scalar2=1.0, op0=ALU.mult, op1=ALU.add),
                   reads=[tFG], writes=[tKK])
                proj(Fb[0], tF[0], xs, r, C_K, 512, [txs])
                proj(Fb[1], tF[1], xs, r, C_V, 512, [txs])
                proj(Fb[2], tF[2], xs, r, C_IQ, 324, [txs])
                proj(Fb[4], tF[4], xs, r, C_HI, 512, [txs])
                rope(Fb[0][:, :], tF[0], 8, n, KR[:, :], tKR)
                transposes(KR, tKR, 4, 0)
                op("act", lambda e: e.activation(out=KTS[g % 2][:, :, 128 * r:128 * r + 128],
                                                 in_=Hb[0][:, 0:512].rearrange("p (a t) -> p a t", a=4), func=AF.Copy),
                   reads=[tH[0]], writes=[tKTS[g % 2]])
                op("act", lambda e: e.activation(out=VS[g % 2][:, r, :].rearrange("p (h e) -> p h e", e=65)[:, :, 0:64],
                                                 in_=Fb[1][:, :].rearrange("p (h d) -> p h d", d=64), func=AF.Copy),
                   reads=[tF[1]], writes=[tVS[g % 2]])
                rope(Fb[2][:, 0:320], tF[2], 5, n, XR[:, :], tXR)
                op("dve", lambda e: e.tensor_copy(out=IKR[:, :].rearrange("p (a d) -> p a d", a=2),
                                                  in_=XR[:, 256:320].unsqueeze(1).broadcast_to([128, 2, 64])),
                   reads=[tXR], writes=[tIKR])
                transposes(IKR, tIKR, 1, 1)
                op("act", lambda e: e.activation(out=IKS[g % 2][:, 128 * r:128 * r + 128], in_=Hb[1][:, 0:128], func=AF.Copy),
                   reads=[tH[1]], writes=[tIKS[g % 2]])
                grp("pe", [lambda e: e.matmul(Fb[3][:, :], lhsT=LT_U128[:], rhs=LF[:], start=True, stop=True)],
                    reads=[tLF, tC], writes=[tF[3]])
                op("act", lambda e: e.activation(out=DP[:], in_=Fb[3][:, :], func=AF.Exp), reads=[tF[3]], writes=[tDP])
                op("pool", lambda e: e.tensor_tensor(out=DK[:], in0=KK[:], in1=DP[:], op=ALU.mult), reads=[tKK, tDP], writes=[tDK])
                op("act", lambda e: e.activation(out=VH[:], in_=Fb[4][:, :], func=AF.Copy), reads=[tF[4]], writes=[tVH])
                grp("pe", [(lambda e, h=h: e.matmul(Fb[2][:, 384 + 2 * h:386 + 2 * h], lhsT=LF[:, 128 * h:128 * h + 128], rhs=IND2[:],
                                                    start=True, stop=True)) for h in range(4)],
                    reads=[tLF, tC], writes=[tF2s])
                op("act", lambda e: e.activation(out=DCOL[:], in_=Fb[2][:, 384:392], func=AF.Exp), reads=[tF2s], writes=[tDCOL])
                if own:
                    op("act", lambda e: e.activation(out=SB16[:], in_=ST_[:], func=AF.Copy), reads=[tST], writes=[tSB16])
                grp("pe", [(lambda e, h=h: e.matmul(Fb[5][:, 128 * h:128 * h + 128], lhsT=DK[:, 128 * h:128 * h + 128],
                                                    rhs=VH[:, 128 * h:128 * h + 128], start=True, stop=True)) for h in range(4)],
                    reads=[tDK, tVH], writes=[tF[5]])
                op("dve", lambda e: e.tensor_tensor(out=ST_[:], in0=ST_[:],
                                                    in1=DCOL[:, :].rearrange("p (h c) -> p h c", c=2)[:, :, 1:2].broadcast_to([128, 4, 128]),
                                                    op=ALU.mult), reads=[tST, tDCOL], writes=[tST])
                op("dve", lambda e: e.tensor_tensor(out=ST_[:], in0=ST_[:], in1=Fb[5][:, :].rearrange("p (h v) -> p h v", h=4), op=ALU.add),
                   reads=[tST, tF[5]], writes=[tST])

                if own:
                    sl = m % 2
                    proj(Fb[0], tF[0], xs, r, C_Q, 512, [txs])
                    rope(Fb[0][:, :], tF[0], 8, n, KR[:, :], tKR)
                    transposes(KR, tKR, 4, 0)
                    op("act", lambda e: e.activation(out=QTS[sl][:], in_=Hb[0][:, 0:512].rearrange("p (a t) -> p a t", a=4), func=AF.Copy),
                       reads=[tH[0]], writes=[tQTS[sl]])
                    dma("sp", dQTS[sl], QT_d[:, :, 128 * m:128 * m + 128], QTS[sl][:], reads=[tQTS[sl]], writes=[tQTd[m]])
                    op("dve", lambda e: e.tensor_scalar(out=TC_[:, 0:4], in0=Fb[2][:, 320:324], scalar1=0.0, scalar2=2.0,
                                                        op0=ALU.is_ge, op1=ALU.mult), reads=[tF[2]], writes=[tTC])
                    op("dve", lambda e: e.tensor_scalar(out=TC_[:, 4:8], in0=TC_[:, 0:4], scalar1=-1.0, scalar2=None, op0=ALU.add),
                       reads=[tTC], writes=[tTC])
                    op("dve", lambda e: e.tensor_copy(out=SGN[:], in_=TC_[:, 4:8]), reads=[tTC], writes=[tSGN])
                    op("dve", lambda e: e.scalar_tensor_tensor(out=AW[:], in0=Fb[2][:, 320:324], scalar=IDX_SCALE, in1=TC_[:, 4:8],
                                                               op0=ALU.mult, op1=ALU.mult), reads=[tF[2], tTC], writes=[tAW])
                    op("dve", lambda e: e.tensor_tensor(out=IQS[:, :].rearrange("p (h d) -> p h d", d=64),
                                                        in0=XR[:, 0:256].rearrange("p (h d) -> p h d", d=64),
                                                        in1=AW[:, :].unsqueeze(2).broadcast_to([128, 4, 64]), op=ALU.mult),
                       reads=[tXR, tAW], writes=[tIQS])
                    transposes(IQS, tIQS, 2, 1)
                    op("act", lambda e: e.activation(out=IQTS[sl][:].rearrange("p u a t -> p a u t"), in_=Hb[1][:, 0:256].rearrange("p (a u t) -> p a u t", a=2, u=2), func=AF.Copy),
                       reads=[tH[1]], writes=[tIQTS[sl]])
                    dma("sp", dIQTS[sl], IQT_d[:, m, :], IQTS[sl][:].rearrange("p u a t -> p (u a t)"), reads=[tIQTS[sl]], writes=[tIQTd[m]])
                    fns = []
                    for u in range(2):
                        fns.append(lambda e, u=u: e.matmul(Fb[2][:, 400 + 2 * u:402 + 2 * u], lhsT=ETA[u][:], rhs=SGN[:, 0:2], start=True, stop=False))
                        fns.append(lambda e, u=u: e.matmul(Fb[2][:, 400 + 2 * u:402 + 2 * u], lhsT=ETB[u][:], rhs=SGN[:, 2:4], start=False, stop=True))
                    grp("pe", fns, reads=[tSGN, tC], writes=[tF2s])
                    for hp in range(2):
                        for u in range(2):
                            op("dve", lambda e, hp=hp, u=u: e.tensor_scalar(out=SELS[sl][:, 2 * hp + u, :], in0=E_u[u][:],
                                                                            scalar1=Fb[2][:, 400 + 2 * u + hp:401 + 2 * u + hp],
                                                                            scalar2=None, op0=ALU.mult),
                               reads=[tF2s, tC], writes=[tSELS[sl]])
                    dma("sp", dSELS[sl], SEL_d[:, m, :], SELS[sl][:].rearrange("p a t -> p (a t)"), reads=[tSELS[sl]], writes=[tSELd[m]])
                    proj(Fb[1], tF[1], xs, r, C_HQ, 512, [txs])
                    proj(Fb[4], tF[4], xs, r, C_HG, 512, [txs])
                    grp("pe", [lambda e: e.matmul(Fb[3][:, :], lhsT=LT_I64[:], rhs=LF[:], start=True, stop=True)], reads=[tLF, tC], writes=[tF[3]])
                    op("act", lambda e: e.activation(out=AQ[:], in_=Fb[3][:, :], func=AF.Exp), reads=[tF[3]], writes=[tAQ])
                    op("act", lambda e: e.activation(out=BN[:], in_=Fb[3][:, :], func=AF.Exp, scale=-1.0), reads=[tF[3]], writes=[tBN])
                    grp("pe", [lambda e: e.matmul(Fb[3][:, :], lhsT=LT_U64[:], rhs=LF[:], start=True, stop=True)], reads=[tLF, tC], writes=[tF[3]])
                    op("act", lambda e: e.activation(out=D64[:], in_=Fb[3][:, :], func=AF.Exp), reads=[tF[3]], writes=[tD64])
                    op("dve", lambda e: e.tensor_tensor(out=QA[:], in0=Fb[1][:, :], in1=AQ[:], op=ALU.mult), reads=[tF[1], tAQ], writes=[tQA])
                    op("pool", lambda e: e.tensor_tensor(out=KBm[:], in0=KK[:], in1=BN[:], op=ALU.mult), reads=[tKK, tBN], writes=[tKBm])
                    op("pool", lambda e: e.tensor_tensor(out=KD[:], in0=KK[:], in1=D64[:], op=ALU.mult), reads=[tKK, tD64], writes=[tKD])
                    transposes(QA, tQA, 4, 0)
                    h0v = Hb[0][:, 0:512].rearrange("p (a t) -> p a t", a=4)
                    op("act", lambda e: e.activation(out=QAT[:], in_=h0v, func=AF.Copy), reads=[tH[0]], writes=[tQAT])
                    op("act", lambda e: e.activation(out=QAI[:, :, 0:64], in_=h0v[:, :, 0:64], func=AF.Copy), reads=[tH[0]], writes=[tQAI])
                    op("dve", lambda e: e.tensor_tensor(out=QAI[:, :, 64:128], in0=h0v[:, :, 64:128],
                                                        in1=DCOL[:, :].rearrange("p (h c) -> p h c", c=2)[:, :, 0:1].broadcast_to([128, 4, 64]),
                                                        op=ALU.mult), reads=[tH[0], tDCOL], writes=[tQAI])
                    transposes(KBm, tKBm, 4, 1)
                    op("act", lambda e: e.activation(out=KBT[:], in_=Hb[1][:, 0:512].rearrange("p (a t) -> p a t", a=4), func=AF.Copy),
                       reads=[tH[1]], writes=[tKBT])
                    transposes(KD, tKD, 4, 0, out_cols=64, in_rows=slice(0, 64))
                    op("act", lambda e: e.activation(out=KDT[:], in_=Hb[0][:, 0:256].rearrange("p (a t) -> p a t", a=4), func=AF.Copy),
                       reads=[tH[0]], writes=[tKDT])
                    grp("pe", [(lambda e, h=h: e.matmul(Fb[5][:, 128 * h:128 * h + 128], lhsT=KBT[:, h, :], rhs=QAT[:, h, :], start=True, stop=True))
                               for h in range(4)], reads=[tKBT, tQAT], writes=[tF[5]])
                    op("dve", lambda e: e.tensor_tensor(out=AT[:], in0=Fb[5][:, :].rearrange("p (h t) -> p h t", h=4),
                                                        in1=MASKBD[:, :].unsqueeze(1).broadcast_to([128, 4, 128]), op=ALU.mult),
                       reads=[tF[5], tC], writes=[tAT])
                    grp("pe", [(lambda e, h=h: e.matmul(Fb[0][0:64, 64 * h:64 * h + 64], lhsT=KDT[:, h, :], rhs=QAT[:, h, 64:128], start=True, stop=True))
                               for h in range(4)], reads=[tKDT, tQAT], writes=[tF[0]])
                    op("dve", lambda e: e.tensor_copy(out=AT[0:64, :, 64:128], in_=Fb[0][0:64, 0:256].rearrange("p (h t) -> p h t", h=4)),
                       reads=[tF[0]], writes=[tAT])
                    fns = []
                    for h in range(4):
                        fns.append(lambda e, h=h: e.matmul(Fb[5][:, 128 * h:128 * h + 128], lhsT=AT[:, h, :], rhs=VH[:, 128 * h:128 * h + 128], start=True, stop=False))
                        fns.append(lambda e, h=h: e.matmul(Fb[5][:, 128 * h:128 * h + 128], lhsT=QAI[:, h, :], rhs=SB16[:, h, :], start=False, stop=True))
                    grp("pe", fns, reads=[tAT, tVH, tQAI, tSB16], writes=[tF[5]])
                    for h in range(4):
                        op("act", lambda e, h=h: e.activation(out=JK[:], in_=Fb[5][:, 128 * h:128 * h + 128], func=AF.Square, accum_out=MS[:, h:h + 1]),
                           reads=[tF[5]], writes=[tMS, tJK])
                    op("dve", lambda e: e.tensor_scalar(out=MS[:], in0=MS[:], scalar1=1.0 / 128.0, scalar2=RMS_EPS, op0=ALU.mult, op1=ALU.add),
                       reads=[tMS], writes=[tMS])
                    op("act", lambda e: e.activation(out=RS[:], in_=MS[:], func=AF.Ln), reads=[tMS], writes=[tRS])
                    op("act", lambda e: e.activation(out=RS[:], in_=RS[:], func=AF.Exp, scale=-0.5), reads=[tRS], writes=[tRS])
                    op("act", lambda e: e.activation(out=GS[:], in_=Fb[4][:, :], func=AF.Exp, scale=-1.0), reads=[tF[4]], writes=[tGS])
                    op("pool", lambda e: e.tensor_scalar(out=GS[:], in0=GS[:], scalar1=1.0, scalar2=None, op0=ALU.add), reads=[tGS], writes=[tGS])
                    op("dve", lambda e: e.reciprocal(out=GS[:], in_=GS[:]), reads=[tGS], writes=[tGS])
                    op("dve", lambda e: e.tensor_tensor(out=GS[:], in0=Fb[4][:, :], in1=GS[:], op=ALU.mult), reads=[tF[4], tGS], writes=[tGS])
                    op("dve", lambda e: e.tensor_tensor(out=OG[:, :].rearrange("p (h v) -> p h v", h=4),
                                                        in0=Fb[5][:, :].rearrange("p (h v) -> p h v", h=4),
                                                        in1=RS[:, :].unsqueeze(2).broadcast_to([128, 4, 128]), op=ALU.mult),
                       reads=[tF[5], tRS], writes=[tOG])
                    op("pool", lambda e: e.tensor_tensor(out=OG[:], in0=OG[:], in1=NGB[:], op=ALU.mult), reads=[tOG, tNG], writes=[tOG])
                    op("pool", lambda e: e.tensor_tensor(out=OGB[:], in0=OG[:], in1=GS[:], op=ALU.mult), reads=[tOG, tGS], writes=[tOGB])
                    transposes(OGB, tOGB, 4, 1)
                    op("act", lambda e: e.activation(out=HGTS[sl][:], in_=Hb[1][:, 0:512].rearrange("p (a t) -> p a t", a=4), func=AF.Copy),
                       reads=[tH[1]], writes=[tHGTS[sl]])
                    dma("sp", dHGTS[sl], HGT_d[:, :, 128 * m:128 * m + 128], HGTS[sl][:], reads=[tHGTS[sl]], writes=[tHGTd[m]])
                    dma("sp", dKTS[g % 2], KT_d[:, :, 512 * g:512 * g + 512], KTS[g % 2][:], reads=[tKTS[g % 2]], writes=[tKTd[g]])
                    dma("sp", dVS[g % 2], V_d[:, 4 * g:4 * g + 4, :], VS[g % 2][:], reads=[tVS[g % 2]], writes=[tVd[g]])
                    dma("sp", dIKS[g % 2], IKT_d[:, 512 * g:512 * g + 512], IKS[g % 2][:], reads=[tIKS[g % 2]], writes=[tIKTd[g]])
            if dbg:
                for nm, tl, tt in [("FG", FG, tFG), ("LF", LF, tLF), ("KK", KK, tKK), ("DP", DP, tDP), ("DCOL", DCOL, tDCOL),
                                   ("ST", ST_, tST), ("AQ", AQ, tAQ), ("BN", BN, tBN), ("D64", D64, tD64), ("QAT", QAT, tQAT),
                                   ("AT", AT, tAT), ("OG", OG, tOG), ("GS", GS, tGS), ("RS", RS, tRS), ("SB16", SB16, tSB16),
                                   ("COS", COS, tTab), ("SIN", SIN, tTab), ("VH", VH, tVH), ("QAI", QAI, tQAI), ("KBT", KBT, tKBT)]:
                    shp = list(tl[:].shape)
                    od = dram("dbg_" + nm, shp, tl[:].dtype, "ExternalOutput")
                    dsx = sc.dsem("dd_" + nm)
                    dma("sp", dsx, od, tl[:], reads=[tt])
                    sc._wait("sp", {dsx: dsx.count})
            p1_bufs = [tW, tXG[0], tXG[1], tTab, tLB, tL1, tNG, tC, tST, tKR, tTA, tTB, tTC, tXR, tIKR, tEZ, tFG, tLF, tKK, tDP, tDK, tVH,
                       tDCOL, tSB16, tAW, tSGN, tIQS, tAQ, tBN, tD64, tQA, tKBm, tKD, tQAT, tQAI, tKBT, tKDT, tAT, tMS, tRS, tJK, tGS, tOG,
                       tOGB, tPOS] + tKTS + tVS + tIKS + tQTS + tIQTS + tSELS + tHGTS
            for q in ("pe", "act", "dve", "pool", "sp"):
                sc.finish(q, p1_bufs)

        with ExitStack() as p2:
            IKT = sb(p2, "IKT", [128, S], BF16)
            tIKT = T("IKT")
            dIKT = sc.dsem("dIKT")
            for g in range(NG):
                dma("sp", dIKT, IKT[:, 512 * g:512 * g + 512], IKT_d[:, 512 * g:512 * g + 512], reads=[tIKTd[g]], writes=[tIKT])
            SCR = sb(p2, "SCR", [128, S], F32)
            tSCR = T("SCR")
            KB0 = sb(p2, "KB0", [128, 512], F32)
            KB0D = sb(p2, "KB0D", [128, 512], F32)
            tKB0 = T("KB0")
            dKB0 = sc.dsem("dKB0")
            dma("sp", dKB0, KB0[:], kb0_d[0:1, :].partition_broadcast(128), writes=[tKB0])
            op("dve", lambda e: e.tensor_copy(out=KB0D[:], in_=KB0[:]), reads=[tKB0], writes=[tKB0])
            op("dve", lambda e: e.tensor_tensor(out=KB0D[:, 384:512], in0=KB0D[:, 384:512], in1=TRIB[:], op=ALU.add), reads=[tKB0, tC], writes=[tKB0])
            HALF = sb(p2, "HALF", [128, 1], F32)
            op("dve", lambda e: e.memset(HALF[:], 0.5), writes=[tKB0])
            QTB = [sb(p2, "QTB%d" % i, [128, 4, 128], BF16) for i in range(2)]
            IQTB = [sb(p2, "IQTB%d" % i, [128, 256], BF16) for i in range(2)]
            SELB = [sb(p2, "SELB%d" % i, [128, 4, 128], BF16) for i in range(2)]
            tQL = [T(), T()]
            dQL = [sc.dsem("dQL0"), sc.dsem("dQL1")]
            RB = [sb(p2, "RB%d" % i, [128, 512], BF16) for i in range(8)]
            tRB = [T() for _ in range(8)]
            MX = sb(p2, "MX", [128, 32], F32); tMX = T()
            MNt = sb(p2, "MN", [128, 32], F32); tMN = T()
            HI = sb(p2, "HI", [128, 1], F32); LO = sb(p2, "LO", [128, 1], F32); MID = sb(p2, "MID", [128, 1], F32)
            CH = sb(p2, "CH", [128, 1], F32); CNT = [sb(p2, "CNT%d" % i, [128, 1], F32) for i in range(2)]
            DD = sb(p2, "DD", [128, 1], F32); MNEED = sb(p2, "MNEED", [128, 1], F32); CARRY = sb(p2, "CARRY", [128, 1], F32)
            SELI = sb(p2, "SELI", [128, 1], I32); NSEL = sb(p2, "NSEL", [128, 1], I32)
            tBS = T("bisect")
            JUNK = sb(p2, "JUNK", [128, 2048], BF16); tJUNK = T()
            AB = [sb(p2, "AB%d" % i, [128, 2048], BF16) for i in range(2)]; tAB = [T(), T()]
            PF = sb(p2, "PF", [128, 2048], F32); tPF = T()
            TM = sb(p2, "TM", [128, 2048], BF16); tTM = T()
            MK = sb(p2, "MK", [128, 2048], BF16); tMK = T()
            MT = sb(p2, "MT", [128, NB, 128], BF16); tMT = T()
            KTB = [sb(p2, "KTB%d" % i, [128, 4, 512], BF16) for i in range(2)]
            VB = [sb(p2, "VB%d" % i, [128, 4, 520], BF16) for i in range(2)]
            tKTB = [T(), T()]; tVB = [T(), T()]
            dKTB = [sc.dsem("dKTB0"), sc.dsem("dKTB1")]; dVB = [sc.dsem("dVB0"), sc.dsem("dVB1")]
            EX = [sb(p2, "EX%d" % i, [128, 4, 128], BF16) for i in range(4)]; tEX = [T() for _ in range(4)]
            tPM = []
            RC = sb(p2, "RC", [128, 8], F32); tRC = T()
            ATo = sb(p2, "ATo", [128, 512], BF16); tATo = T()
            ATS = [sb(p2, "ATS%d" % i, [128, 4, 128], BF16) for i in range(2)]; tATS = [T(), T()]
            dATS = [sc.dsem("dATS0"), sc.dsem("dATS1")]
            kchunk = 0

            QBD = [sb(p2, "QBD%d" % i, [128, 4, 256], BF16) for i in range(2)]
            tQBD = [T(), T()]
            for i in range(2):
                op("pool", lambda e, i=i: e.memset(QBD[i][:], 0.0), writes=[tQBD[i]])
            BIGM = 30000.0
            MKB = sb(p2, "MKB", [128, 2048], BF16); tMKB = T()

            def load_q(m):
                sl = m % 2
                dma("sp", dQL[sl], QTB[sl][:], QT_d[:, :, 128 * m:128 * m + 128], reads=[tQTd[m]], writes=[tQL[sl]])
                dma("sp", dQL[sl], IQTB[sl][:], IQT_d[:, m, :], reads=[tIQTd[m]], writes=[tQL[sl]])
                dma("sp", dQL[sl], SELB[sl][:].rearrange("p a t -> p (a t)"), SEL_d[:, m, :], reads=[tSELd[m]], writes=[tQL[sl]])
                op("pool", lambda e: e.tensor_copy(out=QBD[sl][0:64, :, 0:128], in_=QTB[sl][0:64, :, :]), reads=[tQL[sl]], writes=[tQBD[sl]])
                op("pool", lambda e: e.tensor_copy(out=QBD[sl][64:128, :, 128:256], in_=QTB[sl][64:128, :, :]), reads=[tQL[sl]], writes=[tQBD[sl]])

            def scores_bisect(m):
                sl = m % 2
                ng = m + 1
                nk = 512 * ng
                for G in range(ng):
                    rb0 = 4 * (G % 2)
                    for hp in range(2):
                        for u in range(2):
                            i4 = 2 * hp + u
                            ps = slice(64 * hp, 64 * hp + 64)
                            grp("pe", [lambda e, ps=ps, u=u, i4=i4: e.matmul(Fb[i4][:, :], lhsT=IQTB[sl][ps, 128 * u:128 * u + 128],
                                                                             rhs=IKT[ps, 512 * G:512 * G + 512], start=True, stop=True)],
                                reads=[tQL[sl], tIKT], writes=[tF[i4]])
                            op("act", lambda e, i4=i4: e.activation(out=RB[rb0 + i4][:], in_=Fb[i4][:, :], func=AF.Relu),
                               reads=[tF[i4]], writes=[tRB[rb0 + i4]])
                    fb = 4 + (G % 2)
                    grp("pe", [(lambda e, i4=i4: e.matmul(Fb[fb][:, :], lhsT=SELB[sl][:, i4, :], rhs=RB[rb0 + i4][:], start=(i4 == 0), stop=(i4 == 3)))
                               for i4 in range(4)], reads=[tQL[sl]] + [tRB[rb0 + i] for i in range(4)], writes=[tF[fb]])
                    op("dve", lambda e: e.tensor_reduce(out=MNt[:, G:G + 1], in_=Fb[fb][:, :], axis=AX.X, op=ALU.min),
                       reads=[tF[fb]], writes=[tMN])
                    csl = slice(512 * G, 512 * G + 512)
                    if G == 0:
                        kb = KB0D if m == 0 else KB0
                        op("dve", lambda e, kb=kb: e.tensor_tensor(out=SCR[:, csl], in0=Fb[fb][:, :], in1=kb[:], op=ALU.add),
                           reads=[tF[fb], tKB0], writes=[tSCR])
                    elif G == m:
                        op("dve", lambda e: e.tensor_copy(out=SCR[:, 512 * G:512 * G + 384], in_=Fb[fb][:, 0:384]), reads=[tF[fb]], writes=[tSCR])
                        op("dve", lambda e: e.tensor_tensor(out=SCR[:, 512 * G + 384:512 * G + 512], in0=Fb[fb][:, 384:512], in1=TRIB[:], op=ALU.add),
                           reads=[tF[fb], tC], writes=[tSCR])
                    else:
                        op("act", lambda e: e.activation(out=SCR[:, csl], in_=Fb[fb][:, :], func=AF.Copy), reads=[tF[fb]], writes=[tSCR])
                    op("dve", lambda e: e.tensor_reduce(out=MX[:, G:G + 1], in_=SCR[:, csl], axis=AX.X, op=ALU.max), reads=[tSCR], writes=[tMX])
                op("dve", lambda e: e.tensor_reduce(out=HI[:], in_=MX[:, 0:ng], axis=AX.X, op=ALU.max), reads=[tMX], writes=[tBS])
                op("dve", lambda e: e.tensor_reduce(out=LO[:], in_=MNt[:, 0:ng], axis=AX.X, op=ALU.min), reads=[tMN, tBS], writes=[tBS])
                op("dve", lambda e: e.tensor_tensor(out=DD[:], in0=HI[:], in1=LO[:], op=ALU.subtract), reads=[tBS], writes=[tBS])
                op("dve", lambda e: e.scalar_tensor_tensor(out=HI[:], in0=DD[:], scalar=1e-3, in1=HI[:], op0=ALU.mult, op1=ALU.add), reads=[tBS], writes=[tBS])
                op("dve", lambda e: e.tensor_scalar(out=HI[:], in0=HI[:], scalar1=1e-6, scalar2=None, op0=ALU.add), reads=[tBS], writes=[tBS])
                op("dve", lambda e: e.memset(CH[:], 0.0), reads=[tBS], writes=[tBS])
                chunks = [(c0, min(2048, nk - c0)) for c0 in range(0, nk, 2048)]
                for it in range(NIT):
                    op("dve", lambda e: e.scalar_tensor_tensor(out=MID[:], in0=LO[:], scalar=HI[:, 0:1], in1=HALF[:], op0=ALU.add, op1=ALU.mult),
                       reads=[tBS, tKB0], writes=[tBS])
                    for ci, (c0, w) in enumerate(chunks):
                        cprev = CNT[(ci + 1) % 2]
                        ccur = CNT[ci % 2]
                        op("dve", lambda e, c0=c0, w=w, ci=ci, cprev=cprev, ccur=ccur: e.tensor_scalar(
                            out=JUNK[:, 0:w], in0=SCR[:, c0:c0 + w], scalar1=MID[:, 0:1], scalar2=(None if ci == 0 else cprev[:, 0:1]),
                            op0=ALU.is_ge, op1=ALU.add, accum_out=ccur[:, 0:1]), reads=[tSCR, tBS], writes=[tBS, tJUNK])
                    cfin = CNT[(len(chunks) - 1) % 2]
                    op("dve", lambda e: e.tensor_scalar(out=SELI[:], in0=cfin[:], scalar1=TOPK, scalar2=None, op0=ALU.is_ge), reads=[tBS], writes=[tBS])
                    op("dve", lambda e: e.tensor_scalar(out=NSEL[:], in0=cfin[:], scalar1=TOPK, scalar2=None, op0=ALU.is_lt), reads=[tBS], writes=[tBS])
                    op("dve", lambda e: e.copy_predicated(out=LO[:], mask=SELI[:], data=MID[:]), reads=[tBS], writes=[tBS])
                    op("dve", lambda e: e.copy_predicated(out=HI[:], mask=NSEL[:], data=MID[:]), reads=[tBS], writes=[tBS])
                    op("dve", lambda e: e.copy_predicated(out=CH[:], mask=NSEL[:], data=cfin[:]), reads=[tBS], writes=[tBS])
                op("dve", lambda e: e.tensor_scalar(out=MNEED[:], in0=CH[:], scalar1=-1.0, scalar2=TOPK, op0=ALU.mult, op1=ALU.add), reads=[tBS], writes=[tBS])

            def final_mask(m):
                ng = m + 1
                nk = 512 * ng
                chunks = [(c0, min(2048, nk - c0)) for c0 in range(0, nk, 2048)]
                for ci, (c0, w) in enumerate(chunks):
                    op("dve", lambda e: e.tensor_scalar(out=AB[0][:, 0:w], in0=SCR[:, c0:c0 + w], scalar1=LO[:, 0:1], scalar2=None, op0=ALU.is_ge),
                       reads=[tSCR, tBS], writes=[tAB[0]])
                    op("dve", lambda e: e.tensor_scalar(out=AB[1][:, 0:w], in0=SCR[:, c0:c0 + w], scalar1=HI[:, 0:1], scalar2=None, op0=ALU.is_ge),
                       reads=[tSCR, tBS], writes=[tAB[1]])
                    if ci > 0:
                        op("dve", lambda e: e.tensor_copy(out=CARRY[:], in_=PF[:, 2047:2048]), reads=[tPF, tBS], writes=[tBS])
                    op("dve", lambda e: e.tensor_tensor_scan(out=PF[:, 0:w], data0=AB[0][:, 0:w], data1=AB[1][:, 0:w],
                                                             initial=(0.0 if ci == 0 else CARRY[:, 0:1]), op0=ALU.add, op1=ALU.subtract),
                       reads=[tAB[0], tAB[1], tBS], writes=[tPF])
                    op("dve", lambda e: e.scalar_tensor_tensor(out=TM[:, 0:w], in0=PF[:, 0:w], scalar=MNEED[:, 0:1], in1=AB[1][:, 0:w],
                                                               op0=ALU.is_le, op1=ALU.max), reads=[tPF, tAB[1], tBS], writes=[tTM])
                    op("pool", lambda e: e.tensor_tensor(out=MK[:, 0:w], in0=TM[:, 0:w], in1=AB[0][:, 0:w], op=ALU.mult), reads=[tTM, tAB[0]], writes=[tMK])
                    op("pool", lambda e: e.tensor_scalar(out=MKB[:, 0:w], in0=MK[:, 0:w], scalar1=-1.0, scalar2=BIGM, op0=ALU.add, op1=ALU.mult),
                       reads=[tMK], writes=[tMKB])
                    nb_ = w // 128
                    for j0 in range(0, nb_, 8):
                        j1 = min(nb_, j0 + 8)
                        hb = (j0 // 8) % 2
                        grp("pe", [(lambda e, j=j: e.transpose(out=Hb[hb][:, 128 * (j - j0):128 * (j - j0) + 128], in_=MKB[:, 128 * j:128 * j + 128], identity=IDN[:]))
                                   for j in range(j0, j1)], reads=[tMKB, tC], writes=[tH[hb]])
                        kb0_ = c0 // 128 + j0
                        op("act", lambda e: e.activation(out=MT[:, kb0_:kb0_ + (j1 - j0), :],
                                                         in_=Hb[hb][:, 0:128 * (j1 - j0)].rearrange("p (a t) -> p a t", t=128), func=AF.Copy),
                           reads=[tH[hb]], writes=[tMT])

            def attention(m):
                nonlocal kchunk
                sl = m % 2
                nkb = 4 * (m + 1)
                first = [True, True]
                for k0 in range(0, nkb, 4):
                    cs = kchunk % 2
                    kchunk += 1
                    gi = k0 // 4
                    dma("sp", dKTB[cs], KTB[cs][:], KT_d[:, :, 512 * gi:512 * gi + 512], reads=[tKTd[gi]], writes=[tKTB[cs]])
                    dma("sp", dVB[cs], VB[cs][:], V_d[:, 4 * gi:4 * gi + 4, :], reads=[tVd[gi]], writes=[tVB[cs]])
                    for j in range(4):
                        kb = k0 + j
                        last_kb = (kb == nkb - 1)
                        for half in range(2):
                            bi = 2 * (kb % 2) + half
                            fns = []
                            for pp in range(2):
                                pr = 2 * half + pp
                                fns.append(lambda e, pp=pp, pr=pr: e.matmul(Fb[bi][:, 256 * pp:256 * pp + 256], lhsT=KTB[cs][:, pr, 128 * j:128 * j + 128],
                                                                            rhs=QBD[sl][:, pr, :], start=True, stop=False))
                                for hp in range(2):
                                    fns.append(lambda e, pp=pp, hp=hp: e.matmul(Fb[bi][:, 256 * pp + 128 * hp:256 * pp + 128 * hp + 128], lhsT=IDN[:],
                                                                                rhs=MT[:, kb, :], start=False, stop=(hp == 1), skip_group_check=True))
                            grp("pe", fns, reads=[tKTB[cs], tQBD[sl], tMT, tC], writes=[tF[bi]])
                            op("act", lambda e: e.activation(out=EX[bi][:].rearrange("p a t -> p (a t)"), in_=Fb[bi][:, :], func=AF.Exp, scale=0.125),
                               reads=[tF[bi]], writes=[tEX[bi]])
                            fns = []
                            for idx in range(4):
                                head = 4 * half + idx
                                st_ = first[half]
                                first[half] = False
                                sp_ = last_kb and idx == 3
                                fns.append(lambda e, idx=idx, head=head, st_=st_, sp_=sp_: e.matmul(
                                    Fb[4 + half][:, 65 * idx:65 * idx + 65], lhsT=EX[bi][:, idx, :],
                                    rhs=VB[cs][:, j, 65 * head:65 * head + 65], start=st_, stop=sp_, skip_group_check=True))
                            grp("pe", fns, reads=[tEX[bi], tVB[cs]], writes=[tF[4 + half]])
                for ob in range(2):
                    ov = Fb[4 + ob][:, 0:260].rearrange("p (h e) -> p h e", e=65)
                    op("dve", lambda e, ov=ov, ob=ob: e.reciprocal(out=RC[:, 4 * ob:4 * ob + 4], in_=ov[:, :, 64]), reads=[tF[4 + ob]], writes=[tRC])
                    op("dve", lambda e, ov=ov, ob=ob: e.tensor_tensor(out=ATo[:, 256 * ob:256 * ob + 256].rearrange("p (h d) -> p h d", d=64),
                                                                      in0=ov[:, :, 0:64], in1=RC[:, 4 * ob:4 * ob + 4].unsqueeze(2).broadcast_to([128, 4, 64]),
                                                                      op=ALU.mult), reads=[tF[4 + ob], tRC], writes=[tATo])
                grp("pe", [(lambda e, i=i: e.transpose(out=Hb[0][:, 128 * i:128 * i + 128], in_=ATo[:, 128 * i:128 * i + 128], identity=IDN[:]))
                           for i in range(4)], reads=[tATo, tC], writes=[tH[0]])
                op("act", lambda e: e.activation(out=ATS[sl][:], in_=Hb[0][:, 0:512].rearrange("p (a t) -> p a t", a=4), func=AF.Copy),
                   reads=[tH[0]], writes=[tATS[sl]])
                dma("sp", dATS[sl], ATT_d[:, :, 128 * m:128 * m + 128], ATS[sl][:], reads=[tATS[sl]], writes=[tATTd[m]])

            load_q(0)
            scores_bisect(0)
            final_mask(0)
            for m in range(NOWN):
                if m + 1 < NOWN:
                    load_q(m + 1)
                    scores_bisect(m + 1)
                attention(m)
                if m + 1 < NOWN:
                    final_mask(m + 1)
            p2_bufs = [tIKT, tSCR, tKB0, tMX, tMN, tBS, tJUNK, tPF, tTM, tMK, tMKB, tMT, tRC, tATo] + tQBD + tQL + tRB + tAB + tKTB + tVB + tEX + tPM + tATS
            for q in ("pe", "act", "dve", "pool", "sp"):
                sc.finish(q, p2_bufs)

        with ExitStack() as p3:
            WO = sb(p3, "WO", [128, 8, D], BF16)
            WU = sb(p3, "WU", [128, 8, DFF], BF16)
            WD = sb(p3, "WD", [128, 32, D], BF16)
            tWO, tWU, tWD = T(), T(), T()
            dW3 = [sc.dsem("dW3_%d" % i) for i in range(3)]
            wo_v = wo_d.rearrange("(c p) n -> p c n", p=128)
            wu_v = wup_d.rearrange("(c p) n -> p c n", p=128)
            wd_v = wdn_d.rearrange("(c p) n -> p c n", p=128)
            for c in range(8):
                dma("pool", dW3[0], WO[:, c, :], wo_v[:, c, :], writes=[tWO])
            for c in range(8):
                dma("pool", dW3[1], WU[:, c, :], wu_v[:, c, :], writes=[tWU])
            for c in range(32):
                dma("pool", dW3[2], WD[:, c, :], wd_v[:, c, :], writes=[tWD])
            LNP = [sb(p3, "LNP%d" % i, [128, D], F32) for i in range(4)]
            tLNP = T()
            dLNP = [sc.dsem("dLNP%d" % i) for i in range(4)]
            for i in range(4):
                dma("sp", dLNP[i], LNP[i][:], lnp_ds[i][0:1, :].partition_broadcast(128), writes=[tLNP])
            ATB = [sb(p3, "ATB%d" % i, [128, 4, 128], BF16) for i in range(2)]
            HGB = [sb(p3, "HGB%d" % i, [128, 4, 128], BF16) for i in range(2)]
            XO = [sb(p3, "XO%d" % i, [128, D], F32) for i in range(2)]
            tL3 = [T(), T()]
            dL3 = [sc.dsem("dL3_0"), sc.dsem("dL3_1")]
            Y = sb(p3, "Y", [128, D], F32); tY = T()
            STT = sb(p3, "STT", [128, 2, 6], F32); MV = sb(p3, "MV", [128, 2], F32); RSD = sb(p3, "RSD", [128, 1], F32); tLN = T()
            X1 = sb(p3, "X1", [128, D], F32); tX1 = T()
            X1B = sb(p3, "X1B", [128, D], BF16); tX1B = T()
            X1T = sb(p3, "X1T", [128, 8, 128], BF16); tX1T = T()
            RT = [sb(p3, "RT%d" % i, [128, 128], F32) for i in range(2)]; tRT = [T(), T()]
            HT = sb(p3, "HT", [128, 32, 128], BF16); tHT = T()
            OB = [sb(p3, "OB%d" % i, [128, D], F32) for i in range(2)]; tOB = [T(), T()]
            dOB = [sc.dsem("dOB0"), sc.dsem("dOB1")]

            def layer_norm(src, tsrc, gi, dst, tdst):
                for hh in range(2):
                    op("dve", lambda e, hh=hh: e.bn_stats(out=STT[:, hh, :], in_=src[:, 512 * hh:512 * hh + 512]), reads=[tsrc], writes=[tLN])
                op("dve", lambda e: e.bn_aggr(out=MV[:], in_=STT[:].rearrange("p a b -> p (a b)")), reads=[tLN], writes=[tLN])
                op("dve", lambda e: e.tensor_scalar(out=RSD[:], in0=MV[:, 1:2], scalar1=LN_EPS, scalar2=None, op0=ALU.add), reads=[tLN], writes=[tLN])
                op("act", lambda e: e.activation(out=RSD[:], in_=RSD[:], func=AF.Ln), reads=[tLN], writes=[tLN])
                op("act", lambda e: e.activation(out=RSD[:], in_=RSD[:], func=AF.Exp, scale=-0.5), reads=[tLN], writes=[tLN])
                op("dve", lambda e: e.tensor_scalar(out=dst[:], in0=src[:], scalar1=MV[:, 0:1], scalar2=RSD[:, 0:1], op0=ALU.subtract, op1=ALU.mult),
                   reads=[tsrc, tLN], writes=[tdst])
                op("pool", lambda e: e.tensor_tensor(out=dst[:], in0=dst[:], in1=LNP[gi][:], op=ALU.mult), reads=[tdst, tLNP], writes=[tdst])
                op("pool", lambda e: e.tensor_tensor(out=dst[:], in0=dst[:], in1=LNP[gi + 1][:], op=ALU.add), reads=[tdst, tLNP], writes=[tdst])

            for m in range(NOWN):
                sl = m % 2
                dma("sp", dL3[sl], ATB[sl][:], ATT_d[:, :, 128 * m:128 * m + 128], reads=[tATTd[m]], writes=[tL3[sl]])
                dma("sp", dL3[sl], HGB[sl][:], HGT_d[:, :, 128 * m:128 * m + 128], reads=[tHGTd[m]], writes=[tL3[sl]])
                dma("sp", dL3[sl], XO[sl][:], xo_d[128 * m:128 * m + 128, :], writes=[tL3[sl]])
                for hh in range(2):
                    grp("pe", [(lambda e, c=c: e.matmul(Fb[hh][:, :], lhsT=(ATB[sl][:, c, :] if c < 4 else HGB[sl][:, c - 4, :]),
                                                        rhs=WO[:, c, 512 * hh:512 * hh + 512], start=(c == 0), stop=(c == 7))) for c in range(8)],
                        reads=[tL3[sl], tWO], writes=[tF[hh]])
                    op("dve", lambda e, hh=hh: e.scalar_tensor_tensor(out=Y[:, 512 * hh:512 * hh + 512], in0=XO[sl][:, 512 * hh:512 * hh + 512], scalar=ALPHA,
                                                                      in1=Fb[hh][:, :], op0=ALU.mult, op1=ALU.add), reads=[tL3[sl], tF[hh]], writes=[tY])
                layer_norm(Y, tY, 0, X1, tX1)
                op("act", lambda e: e.activation(out=X1B[:], in_=X1[:], func=AF.Copy), reads=[tX1], writes=[tX1B])
                grp("pe", [(lambda e, i=i: e.transpose(out=Hb[0][:, 128 * i:128 * i + 128], in_=X1B[:, 128 * i:128 * i + 128], identity=IDN[:]))
                           for i in range(8)], reads=[tX1B, tC], writes=[tH[0]])
                op("act", lambda e: e.activation(out=X1T[:], in_=Hb[0][:, :].rearrange("p (a t) -> p a t", a=8), func=AF.Copy), reads=[tH[0]], writes=[tX1T])
                for fc in range(32):
                    bi = 2 + fc % 4
                    grp("pe", [(lambda e, c=c: e.matmul(Fb[bi][:, 0:128], lhsT=WU[:, c, 128 * fc:128 * fc + 128], rhs=X1T[:, c, :],
                                                        start=(c == 0), stop=(c == 7))) for c in range(8)], reads=[tWU, tX1T], writes=[tF[bi]])
                    op("act", lambda e: e.activation(out=RT[fc % 2][:], in_=Fb[bi][:, 0:128], func=AF.Relu), reads=[tF[bi]], writes=[tRT[fc % 2]])
                    op("pool", lambda e: e.tensor_tensor(out=HT[:, fc, :], in0=RT[fc % 2][:], in1=RT[fc % 2][:], op=ALU.mult), reads=[tRT[fc % 2]], writes=[tHT])
                for hh in range(2):
                    grp("pe", [(lambda e, fc=fc: e.matmul(Fb[hh][:, :], lhsT=HT[:, fc, :], rhs=WD[:, fc, 512 * hh:512 * hh + 512],
                                                          start=(fc == 0), stop=(fc == 31))) for fc in range(32)], reads=[tHT, tWD], writes=[tF[hh]])
                    op("dve", lambda e, hh=hh: e.scalar_tensor_tensor(out=Y[:, 512 * hh:512 * hh + 512], in0=X1[:, 512 * hh:512 * hh + 512], scalar=ALPHA,
                                                                      in1=Fb[hh][:, :], op0=ALU.mult, op1=ALU.add), reads=[tX1, tF[hh]], writes=[tY])
                layer_norm(Y, tY, 2, OB[sl], tOB[sl])
                dma("sp", dOB[sl], y_d[128 * m:128 * m + 128, :], OB[sl][:], reads=[tOB[sl]])
            p3_bufs = [tWO, tWU, tWD, tLNP, tY, tLN, tX1, tX1B, tX1T, tHT] + tL3 + tRT + tOB
            for q in ("pe", "act", "dve", "pool", "sp"):
                sc.finish(q, p3_bufs)
        for q in ("pe", "act", "dve", "pool", "sp"):
            sc.finish(q, [tC] + tF + tH + [tF2s])
    return nc


def make_inputs(x, w_in, w_o, lb_logits, hg_norm_g, ln1_g, ln1_b, w_up, w_down, ln2_g, ln2_b, S=SEQ):
    NB = S // 128
    NOWN = NB // 4
    x = np.asarray(x, np.float32)
    B = x.shape[0]
    in_maps = []
    shared = {
        "w_in": np.ascontiguousarray(np.asarray(w_in, np.float32)[0]),
        "w_o": np.ascontiguousarray(np.asarray(w_o, np.float32)[0]),
        "w_up": np.ascontiguousarray(np.asarray(w_up, np.float32)[0]),
        "w_down": np.ascontiguousarray(np.asarray(w_down, np.float32)[0]),
        "lb0": np.ascontiguousarray(np.asarray(lb_logits, np.float32).reshape(2, 512)[0:1]),
        "lb1": np.ascontiguousarray(np.asarray(lb_logits, np.float32).reshape(2, 512)[1:2]),
        "hgn": np.ascontiguousarray(np.asarray(hg_norm_g, np.float32).reshape(1, 512)),
        "lnp0": np.ascontiguousarray(np.asarray(ln1_g, np.float32).reshape(1, D)),
        "lnp1": np.ascontiguousarray(np.asarray(ln1_b, np.float32).reshape(1, D)),
        "lnp2": np.ascontiguousarray(np.asarray(ln2_g, np.float32).reshape(1, D)),
        "lnp3": np.ascontiguousarray(np.asarray(ln2_b, np.float32).reshape(1, D)),
    }
    for c in range(4 * B):
        b, j = divmod(c, 4)
        npad = 3 - j
        nreal = NB - npad
        xp = np.zeros((S, D), np.float32)
        xp[128 * npad:] = x[b, :128 * nreal]
        own_blocks = [4 * m + j for m in range(NOWN)]
        xo = np.concatenate([x[b, 128 * r:128 * r + 128] for r in own_blocks], 0)
        pidx = np.arange(S, dtype=np.float32).reshape(NB, 128).T - 128.0 * npad
        pos = np.maximum(pidx, 0.0).astype(np.float32)
        kb0 = np.where(pidx.T.reshape(-1)[:512] < 0, NEG, 0.0).astype(np.float32).reshape(1, 512)
        d = dict(shared)
        d.update({"xT": np.ascontiguousarray(xp.T), "xo": np.ascontiguousarray(xo), "pos": np.ascontiguousarray(pos), "kb0": kb0})
        in_maps.append(d)
    return in_maps


def assemble(results, B, S=SEQ):
    NB = S // 128
    NOWN = NB // 4
    out = np.zeros((B, S, D), np.float32)
    for c in range(4 * B):
        b, j = divmod(c, 4)
        y = results[c]["y"]
        for m in range(NOWN):
            r = 4 * m + j
            out[b, 128 * r:128 * r + 128] = y[128 * m:128 * m + 128]
    return out


def kernel(x, w_in, w_o, lb_logits, hg_norm_g, ln1_g, ln1_b, w_up, w_down, ln2_g, ln2_b):
    x = np.asarray(x)
    B, S, _ = x.shape
    nc = build_nc(S)
    in_maps = make_inputs(x, w_in, w_o, lb_logits, hg_norm_g, ln1_g, ln1_b, w_up, w_down, ln2_g, ln2_b, S)
    res = run_bass_kernel_spmd(nc, in_maps, core_ids=list(range(4 * B)))
    return assemble(res.results, B, S)
```
